# Optimizing a Trainium2 kernel written in Bass

```python
import jax, jax.numpy as jnp
from jax import lax
import numpy as np

D_MODEL = 1024
BATCH = 8
SEQ = 2048
DEPTH = 2

HEAD_DIM = 64
N_BRANCHES = 4
BRANCH_WIDTH = D_MODEL // N_BRANCHES
BRANCH_HEADS = BRANCH_WIDTH // HEAD_DIM
MOBA_BLOCK = 256
MOBA_TOPK = 3
MOBA_QCHUNK = 32
GLA_KEY_DIM = HEAD_DIM // 2
GLA_GATE_RANK = 16
GLA_GATE_TEMP = 16.0
LINEAR_CHUNK = 64
RET_DECAY_BASE = 5.0
SWA_KV_HEADS = 2
WINDOW = 128
N_ALIBI_HEADS = 2 * BRANCH_HEADS
N_EXPERTS = 32
TOP_K = 4
D_EXPERT = D_MODEL
SWIGLU_ALPHA = 1.702
SWIGLU_LIMIT = 7.0
NORM_EPS = 1e-5

GLA_QK_WIDTH = BRANCH_HEADS * GLA_KEY_DIM
SWA_KV_WIDTH = SWA_KV_HEADS * HEAD_DIM
IN_SIZES = (
    BRANCH_WIDTH, BRANCH_WIDTH, BRANCH_WIDTH,
    GLA_QK_WIDTH, GLA_QK_WIDTH, BRANCH_WIDTH, GLA_GATE_RANK, BRANCH_WIDTH,
    BRANCH_WIDTH, BRANCH_WIDTH, BRANCH_WIDTH, BRANCH_WIDTH,
    BRANCH_WIDTH, SWA_KV_WIDTH, SWA_KV_WIDTH,
    N_BRANCHES * D_MODEL,
)
D_IN = sum(IN_SIZES)

kernel_name = "hybrid_moba_gla_retnet_swa_moe_block"


def rms_norm(x, g):
    xf = x.astype(jnp.float32)
    y = xf * lax.rsqrt(jnp.mean(xf * xf, axis=-1, keepdims=True) + NORM_EPS)
    return (y * g.astype(jnp.float32)).astype(x.dtype)


def split_cols(z, sizes):
    offs = [int(o) for o in np.cumsum(sizes)[:-1]]
    return jnp.split(z, offs, axis=-1)


def to_heads(t, n_heads):
    b, s, _ = t.shape
    return t.reshape(b, s, n_heads, -1).transpose(0, 2, 1, 3)


def from_heads(o):
    b, h, s, d = o.shape
    return o.transpose(0, 2, 1, 3).reshape(b, s, h * d)


def head_rms_norm(o, g):
    h, d = o.shape[1], o.shape[3]
    of = o.astype(jnp.float32)
    y = of * lax.rsqrt(jnp.mean(of * of, axis=-1, keepdims=True) + NORM_EPS)
    return y * g.astype(jnp.float32).reshape(1, h, 1, d)


def head_group_norm(o, g):
    h, d = o.shape[1], o.shape[3]
    of = o.astype(jnp.float32)
    mu = jnp.mean(of, axis=-1, keepdims=True)
    var = jnp.mean(jnp.square(of - mu), axis=-1, keepdims=True)
    return (of - mu) * lax.rsqrt(var + NORM_EPS) * g.astype(jnp.float32).reshape(1, h, 1, d)


def moba_attention(q, k, v, slopes):
    B, H, S, dh = q.shape
    n_blocks = -(-S // MOBA_BLOCK)
    s_pad = n_blocks * MOBA_BLOCK
    pad = ((0, 0), (0, 0), (0, s_pad - S), (0, 0))
    q, k, v = jnp.pad(q, pad), jnp.pad(k, pad), jnp.pad(v, pad)
    kb = k.reshape(B, H, n_blocks, MOBA_BLOCK, dh)
    vb = v.reshape(B, H, n_blocks, MOBA_BLOCK, dh)
    k_mean = jnp.mean(kb.astype(jnp.float32), axis=3)
    k_sel = min(MOBA_TOPK, n_blocks)
    scale = dh ** -0.5
    b_idx = jnp.arange(B)[:, None, None, None]
    h_idx = jnp.arange(H)[None, :, None, None]
    sl = slopes.astype(jnp.float32)
    blk_offs = jnp.arange(MOBA_BLOCK)

    def chunk_fn(ci):
        start = ci * MOBA_QCHUNK
        q_c = lax.dynamic_slice_in_dim(q, start, MOBA_QCHUNK, axis=2)
        t = (start + jnp.arange(MOBA_QCHUNK)).astype(jnp.float32)
        blk = start // MOBA_BLOCK
        gate = jnp.einsum('bhqd,bhnd->bhqn', q_c.astype(jnp.float32), k_mean)
        gate = jnp.where(jnp.arange(n_blocks) < blk, gate, -jnp.inf)
        _, idx = lax.top_k(gate, k_sel)
        valid = idx < blk
        k_g = kb[b_idx, h_idx, idx]
        v_g = vb[b_idx, h_idx, idx]
        s_g = jnp.einsum('bhqd,bhqjsd->bhqjs', q_c, k_g).astype(jnp.float32) * scale
        pos_g = (idx[..., None] * MOBA_BLOCK + blk_offs).astype(jnp.float32)
        dist_g = t[None, None, :, None, None] - pos_g
        s_g = jnp.where(valid[..., None], s_g - sl[None, :, None, None, None] * dist_g, -jnp.inf)
        k_o = lax.dynamic_index_in_dim(kb, blk, axis=2, keepdims=False)
        v_o = lax.dynamic_index_in_dim(vb, blk, axis=2, keepdims=False)
        s_o = jnp.einsum('bhqd,bhsd->bhqs', q_c, k_o).astype(jnp.float32) * scale
        dist_o = t[:, None] - (blk * MOBA_BLOCK + blk_offs).astype(jnp.float32)[None, :]
        s_o = jnp.where(dist_o >= 0, s_o - sl[None, :, None, None] * dist_o, -jnp.inf)
        logits = jnp.concatenate([s_g.reshape(B, H, MOBA_QCHUNK, k_sel * MOBA_BLOCK), s_o], axis=-1)
        p = jax.nn.softmax(logits, axis=-1)
        p_g = p[..., :k_sel * MOBA_BLOCK].reshape(B, H, MOBA_QCHUNK, k_sel, MOBA_BLOCK).astype(v.dtype)
        p_o = p[..., k_sel * MOBA_BLOCK:].astype(v.dtype)
        return (jnp.einsum('bhqjs,bhqjsd->bhqd', p_g, v_g)
                + jnp.einsum('bhqs,bhsd->bhqd', p_o, v_o))

    out = lax.map(chunk_fn, jnp.arange(s_pad // MOBA_QCHUNK))
    out = out.transpose(1, 2, 0, 3, 4).reshape(B, H, s_pad, dh)
    return out[:, :, :S]


def chunked_decay_linear_attention(q, k, v, log_a):
    B, H, S, dk = q.shape
    dv = v.shape[-1]
    L = LINEAR_CHUNK
    N = S // L
    q, k, v, log_a = (t.astype(jnp.float32).reshape(B, H, N, L, t.shape[-1]) for t in (q, k, v, log_a))
    b = jnp.cumsum(log_a, axis=3)
    b_end = b[:, :, :, -1:, :]
    q_dec = q * jnp.exp(b)
    k_in = k * jnp.exp(-b)
    k_end = k * jnp.exp(b_end - b)
    causal = jnp.tril(jnp.ones((L, L), dtype=bool))
    a = jnp.where(causal, jnp.einsum('bhnld,bhnmd->bhnlm', q_dec, k_in), 0.0)
    o_intra = jnp.einsum('bhnlm,bhnmv->bhnlv', a, v)
    kv = jnp.einsum('bhnld,bhnlv->nbhdv', k_end, v)
    decay = jnp.exp(jnp.moveaxis(b_end[:, :, :, 0, :], 2, 0))

    def step(state, inp):
        kv_n, d_n = inp
        return d_n[..., None] * state + kv_n, state

    _, s_prev = lax.scan(step, jnp.zeros((B, H, dk, dv), jnp.float32), (kv, decay))
    o_inter = jnp.einsum('bhnld,nbhdv->bhnlv', q_dec, s_prev)
    return (o_intra + o_inter).reshape(B, H, S, dv)


def sliding_window_attention(q, k, v, sinks, slopes):
    B, Hq, S, dh = q.shape
    Hkv = k.shape[1]
    G = Hq // Hkv
    W = WINDOW
    nq = S // W
    qb = q.reshape(B, Hkv, G, nq, W, dh)

    def band(t):
        tp = jnp.pad(t, ((0, 0), (0, 0), (W, 0), (0, 0))).reshape(B, Hkv, nq + 1, W, dh)
        return jnp.concatenate([tp[:, :, :-1], tp[:, :, 1:]], axis=3)

    kb, vb = band(k), band(v)
    s = jnp.einsum('bkgnqd,bknsd->bkgnqs', qb, kb).astype(jnp.float32) * dh ** -0.5
    dist = (W + jnp.arange(W)[:, None] - jnp.arange(2 * W)[None, :])
    key_pos = jnp.arange(nq)[:, None] * W - W + jnp.arange(2 * W)[None, :]
    allowed = (dist >= 0)[None] & (dist < W)[None] & (key_pos >= 0)[:, None, :]
    sl = slopes.astype(jnp.float32).reshape(Hkv, G)[None, :, :, None, None, None]
    s = jnp.where(allowed, s - sl * dist.astype(jnp.float32), -jnp.inf)
    sink = jnp.broadcast_to(sinks.astype(jnp.float32).reshape(Hkv, G)[None, :, :, None, None, None],
                            s.shape[:-1] + (1,))
    p = jax.nn.softmax(jnp.concatenate([s, sink], axis=-1), axis=-1)[..., :-1]
    o = jnp.einsum('bkgnqs,bknsd->bkgnqd', p.astype(v.dtype), vb)
    return o.reshape(B, Hq, S, dh)


def moe_ffn(h, w_router, b_router, w_gate_up, b_gate_up, w_down, b_down):
    logits = (h @ w_router + b_router).astype(jnp.float32)
    top_vals, top_idx = lax.top_k(logits, TOP_K)
    weights = jax.nn.softmax(top_vals, axis=-1)
    combine = jnp.einsum('tk,tke->te', weights,
                         jax.nn.one_hot(top_idx, N_EXPERTS, dtype=jnp.float32)).astype(h.dtype)
    y = jnp.zeros_like(h)
    for e in range(N_EXPERTS):
        gu = h @ w_gate_up[e] + b_gate_up[e]
        x_glu, x_lin = jnp.split(gu, 2, axis=-1)
        x_glu = jnp.minimum(x_glu, SWIGLU_LIMIT)
        x_lin = jnp.clip(x_lin, -SWIGLU_LIMIT, SWIGLU_LIMIT)
        act = x_glu * jax.nn.sigmoid(SWIGLU_ALPHA * x_glu) * (x_lin + 1.0)
        y = y + combine[:, e:e + 1] * (act @ w_down[e] + b_down[e])
    return y


def setup_inputs(seed: int = 0) -> dict:
    key = jax.random.key(seed)
    ks = jax.random.split(key, 22)
    f32 = jnp.float32
    nrm = lambda k, shape, s: jax.random.normal(k, shape, f32) * s
    return {
        "x": nrm(ks[0], (BATCH, SEQ, D_MODEL), 1.0),
        "c": nrm(ks[1], (BATCH, D_MODEL), 1.0),
        "w_ada": nrm(ks[2], (DEPTH, D_MODEL, 6 * D_MODEL), 0.5 * D_MODEL ** -0.5),
        "b_ada": nrm(ks[3], (DEPTH, 6 * D_MODEL), 0.01),
        "g_norm_mix": 1.0 + nrm(ks[4], (DEPTH, D_MODEL), 0.01),
        "w_in": nrm(ks[5], (DEPTH, D_MODEL, D_IN), D_MODEL ** -0.5),
        "w_gla_gate": nrm(ks[6], (DEPTH, GLA_GATE_RANK, GLA_QK_WIDTH), GLA_GATE_RANK ** -0.5),
        "b_gla_gate": nrm(ks[7], (DEPTH, GLA_QK_WIDTH), 0.1),
        "g_gla_norm": 1.0 + nrm(ks[8], (DEPTH, BRANCH_WIDTH), 0.01),
        "g_ret_norm": 1.0 + nrm(ks[9], (DEPTH, BRANCH_WIDTH), 0.01),
        "attn_sinks": nrm(ks[10], (DEPTH, BRANCH_HEADS), 1.0),
        "w_branch": nrm(ks[11], (DEPTH, N_BRANCHES, BRANCH_WIDTH, D_MODEL), BRANCH_WIDTH ** -0.5),
        "w_out": nrm(ks[12], (DEPTH, D_MODEL, D_MODEL), D_MODEL ** -0.5),
        "g_norm_ffn": 1.0 + nrm(ks[13], (DEPTH, D_MODEL), 0.01),
        "w_router": nrm(ks[14], (DEPTH, D_MODEL, N_EXPERTS), D_MODEL ** -0.5),
        "b_router": nrm(ks[15], (DEPTH, N_EXPERTS), 0.01),
        "w_gate_up": nrm(ks[16], (DEPTH, N_EXPERTS, D_MODEL, 2 * D_EXPERT), D_MODEL ** -0.5),
        "b_gate_up": nrm(ks[17], (DEPTH, N_EXPERTS, 2 * D_EXPERT), 0.01),
        "w_down": nrm(ks[18], (DEPTH, N_EXPERTS, D_EXPERT, D_MODEL), D_EXPERT ** -0.5),
        "b_down": nrm(ks[19], (DEPTH, N_EXPERTS, D_MODEL), 0.01),
        "g_final": 1.0 + nrm(ks[20], (D_MODEL,), 0.01),
    }


def reference(x, c, w_ada, b_ada, g_norm_mix, w_in, w_gla_gate, b_gla_gate, g_gla_norm, g_ret_norm,
              attn_sinks, w_branch, w_out, g_norm_ffn, w_router, b_router, w_gate_up, b_gate_up,
              w_down, b_down, g_final):
    B, S, D = x.shape
    H = BRANCH_HEADS
    slopes = 2.0 ** (-(jnp.arange(N_ALIBI_HEADS, dtype=jnp.float32) + 1.0) * (8.0 / N_ALIBI_HEADS))
    swa_slopes, moba_slopes = slopes[:H], slopes[H:]
    log_gamma = jnp.log(1.0 - 2.0 ** (-RET_DECAY_BASE - jnp.arange(H, dtype=jnp.float32)))
    c_act = jax.nn.silu(c)

    for l in range(DEPTH):
        mod = c_act @ w_ada[l] + b_ada[l]
        shift1, scale1, gate1, shift2, scale2, gate2 = (m[:, None, :] for m in jnp.split(mod, 6, axis=-1))

        h = rms_norm(x, g_norm_mix[l]) * (1.0 + scale1) + shift1
        z = h @ w_in[l]
        (mq, mk, mv, gq, gk, gv, ga, gr, rq, rk, rv, rg, sq, sk, sv, merge_logits) = split_cols(z, IN_SIZES)

        y_moba = from_heads(moba_attention(to_heads(mq, H), to_heads(mk, H), to_heads(mv, H), moba_slopes))

        gate_logit = ga @ w_gla_gate[l] + b_gla_gate[l]
        log_a = jax.nn.log_sigmoid(gate_logit.astype(jnp.float32)) / GLA_GATE_TEMP
        o_gla = chunked_decay_linear_attention(to_heads(gq, H) * GLA_KEY_DIM ** -0.5, to_heads(gk, H),
                                               to_heads(gv, H), to_heads(log_a, H))
        y_gla = (from_heads(head_rms_norm(o_gla, g_gla_norm[l])) * jax.nn.silu(gr.astype(jnp.float32))).astype(x.dtype)

        ret_log_a = jnp.broadcast_to(log_gamma[None, :, None, None], (B, H, S, HEAD_DIM))
        o_ret = chunked_decay_linear_attention(to_heads(rq, H), to_heads(rk, H) * HEAD_DIM ** -0.5,
                                               to_heads(rv, H), ret_log_a)
        y_ret = (from_heads(head_group_norm(o_ret, g_ret_norm[l])) * jax.nn.silu(rg.astype(jnp.float32))).astype(x.dtype)

        y_swa = from_heads(sliding_window_attention(to_heads(sq, H), to_heads(sk, SWA_KV_HEADS),
                                                    to_heads(sv, SWA_KV_HEADS), attn_sinks[l], swa_slopes))

        ys = jnp.stack([y_moba, y_gla, y_ret, y_swa], axis=2)
        up = jnp.einsum('bsnc,ncd->bsnd', ys, w_branch[l])
        gates = jax.nn.sigmoid(merge_logits.reshape(B, S, N_BRANCHES, D))
        mixed = jnp.sum(gates * up, axis=2) @ w_out[l]
        x = x + gate1 * mixed

        h2 = rms_norm(x, g_norm_ffn[l]) * (1.0 + scale2) + shift2
        y_ffn = moe_ffn(h2.reshape(B * S, D), w_router[l], b_router[l], w_gate_up[l], b_gate_up[l],
                        w_down[l], b_down[l]).reshape(B, S, D)
        x = x + gate2 * y_ffn

    return rms_norm(x, g_final)
```

```python
import numpy as np
import ml_dtypes
from contextlib import ExitStack
import concourse.bass as bass
import concourse.mybir as mybir
from concourse.bass_utils import run_bass_kernel_spmd

F32 = mybir.dt.float32
BF16 = mybir.dt.bfloat16
AF = mybir.ActivationFunctionType
ALU = mybir.AluOpType
AX = mybir.AxisListType

D = 1024
S = 2048
NT = 16
DEPTH = 2
NE = 32
EPS = 1e-5
D_IN = 7184
O_MQ, O_MK, O_MV = 0, 256, 512
O_GQ, O_GK, O_GV, O_GA, O_GR = 768, 896, 1024, 1280, 1296
O_RQ, O_RK, O_RV, O_RG = 1552, 1808, 2064, 2320
O_SQ, O_SK, O_SV = 2576, 2832, 2960
O_MG = 3088
NEG = -30000.0
SKIP = {"ret"}
RETCUT = 99
RETTILES = 16
RETV = 0


class Tk:
    __slots__ = ("w", "r", "name")

    def __init__(self, name=""):
        self.w = None
        self.r = {}
        self.name = name


def tks(n, name=""):
    return [Tk(f"{name}{i}") for i in range(n)]


class Ctx:
    ENG = ("pe", "act", "dve", "pool", "sp")

    def __init__(self, nc, es, n_dsem=24):
        self.nc = nc
        self.E = {"pe": nc.tensor, "act": nc.scalar, "dve": nc.vector, "pool": nc.gpsimd, "sp": nc.sync}
        self.sem = {e: es.enter_context(nc.semaphore("s_" + e)) for e in self.ENG}
        self.cnt = {e: 0 for e in self.ENG}
        self.seen = {e: {} for e in self.ENG}
        self.dsem = [es.enter_context(nc.semaphore(f"s_d{i}")) for i in range(n_dsem)]
        self.dcnt = [0] * n_dsem
        half = n_dsem // 2
        self.dpool = {"sp": list(range(half)), "pool": list(range(half, n_dsem))}
        self.dnext = {"sp": 0, "pool": 0}
        self.nins = 0

    def _need(self, eng, reads, writes):
        need = {}

        def add(key, val):
            if need.get(key, 0) < val:
                need[key] = val

        for t in reads:
            if t.w is not None:
                for k, v in t.w.items():
                    add(k, v)
        for t in writes:
            if t.w is not None:
                for k, v in t.w.items():
                    add(k, v)
            for k, v in t.r.items():
                add(k, v)
        for key, val in need.items():
            if key == ("e", "pe") and eng == "pe":
                continue
            if self.seen[eng].get(key, 0) >= val:
                continue
            sem = self.sem[key[1]] if key[0] == "e" else self.dsem[key[1]]
            self.E[eng].wait_ge(sem, val)
            self.seen[eng][key] = val
            self.nins += 1

    def _mark(self, key, val, reads, writes):
        for t in reads:
            if t.r.get(key, 0) < val:
                t.r[key] = val
        for t in writes:
            if key[0] == "d" and t.w is not None:
                t.w[key] = val
            else:
                t.w = {key: val}
            t.r = {}

    def op(self, eng, fn, reads=(), writes=(), inc=True):
        self._need(eng, reads, writes)
        ins = fn(self.E[eng])
        self.nins += 1
        if inc:
            ins.then_inc(self.sem[eng], 1)
            self.cnt[eng] += 1
            val = self.cnt[eng]
        else:
            assert eng == "pe"
            val = self.cnt[eng] + 1
        self._mark(("e", eng), val, reads, writes)
        return ins

    def dma(self, q, out, in_, reads=(), writes=(), **kw):
        self._need(q, reads, writes)
        i = self.dpool[q][self.dnext[q]]
        self.dnext[q] = (self.dnext[q] + 1) % len(self.dpool[q])
        key = ("d", i)
        if self.dcnt[i] > 0 and self.seen[q].get(key, 0) < self.dcnt[i] * 16:
            self.E[q].wait_ge(self.dsem[i], self.dcnt[i] * 16)
            self.seen[q][key] = self.dcnt[i] * 16
        ins = self.E[q].dma_start(out=out, in_=in_, **kw)
        ins.then_inc(self.dsem[i], 16)
        self.nins += 1
        self.dcnt[i] += 1
        self._mark(key, self.dcnt[i] * 16, reads, writes)
        return ins

    def barrier(self):
        for i in range(len(self.dsem)):
            if self.dcnt[i] > 0 and self.seen["sp"].get(("d", i), 0) < self.dcnt[i] * 16:
                self.E["sp"].wait_ge(self.dsem[i], self.dcnt[i] * 16)
                self.seen["sp"][("d", i)] = self.dcnt[i] * 16
        ins = self.E["sp"].nop() if False else None
        for e in self.ENG:
            for f in self.ENG:
                if e == f:
                    continue
                key = ("e", f)
                if self.cnt[f] > 0 and self.seen[e].get(key, 0) < self.cnt[f]:
                    self.E[e].wait_ge(self.sem[f], self.cnt[f])
                    self.seen[e][key] = self.cnt[f]
            for i in range(len(self.dsem)):
                if self.dcnt[i] > 0 and self.seen[e].get(("d", i), 0) < self.dcnt[i] * 16:
                    self.E[e].wait_ge(self.dsem[i], self.dcnt[i] * 16)
                    self.seen[e][("d", i)] = self.dcnt[i] * 16


def make_consts():
    bf = ml_dtypes.bfloat16
    c = {}
    c["ident_bf"] = np.eye(128, dtype=np.float32).astype(bf)
    c["ident_f"] = np.eye(128, dtype=np.float32)
    k = np.arange(128)[:, None]
    q = np.arange(128)[None, :]
    c["tri_bf"] = (k <= q).astype(np.float32).astype(bf)
    slopes = 2.0 ** (-(np.arange(8, dtype=np.float64) + 1.0))
    swa = np.zeros((128, 4, 2, 128), np.float64)
    for h in range(4):
        sl = slopes[h]
        dist_prev = 128 + q - k
        swa[:, h, 0, :] = np.where(k > q, np.exp(-sl * dist_prev), 0.0)
        dist_own = q - k
        swa[:, h, 1, :] = np.where(k <= q, np.exp(-sl * dist_own), 0.0)
    c["swa_mask"] = swa.astype(np.float32).astype(bf)
    t = np.arange(S)
    a = (t // 128).astype(np.float64)
    r = (t % 128).astype(np.float64)
    qa = np.zeros((4, 12, S), np.float64)
    ka = np.zeros((4, 12, S), np.float64)
    for h in range(4):
        sl = slopes[4 + h] * 8.0
        for j in range(8):
            ka[h, j] = (t // 256 == j)
        qa[h, 8] = -sl * 128.0 * a
        ka[h, 8] = 1.0
        qa[h, 9] = -sl * r
        ka[h, 9] = 1.0
        qa[h, 10] = 1.0
        ka[h, 10] = sl * 128.0 * a
        qa[h, 11] = 1.0
        ka[h, 11] = sl * r
    c["moba_qa"] = qa.astype(np.float32).astype(bf)
    c["moba_ka"] = ka.astype(np.float32).astype(bf)
    m = np.arange(128)[:, None]
    l = np.arange(128)[None, :]
    c["cum_incl"] = ((m <= l) * (-1.0 / 16.0)).astype(np.float32)
    c["cum_after"] = ((m > l) * (-1.0 / 16.0)).astype(np.float32)
    gam = 1.0 - 2.0 ** (-5.0 - np.arange(4, dtype=np.float64))
    pos = np.arange(128, dtype=np.float64)
    rq = np.zeros((128, 2, 128)); rk = np.zeros((128, 2, 128))
    rke = np.zeros((128, 256)); rdec = np.zeros((128, 2))
    for h in range(4):
        j, o = h // 2, (h % 2) * 64
        rq[o:o + 64, j, :] = gam[h] ** (pos + 1.0)
        rk[o:o + 64, j, :] = gam[h] ** (-(pos + 1.0)) * 64 ** -0.5
        rke[:, h * 64:(h + 1) * 64] = (gam[h] ** (127.0 - pos))[:, None] * 64 ** -0.5
        rdec[o:o + 64, j] = gam[h] ** 128.0
    tt_ = np.arange(16)[:, None]; nn_ = np.arange(8)[None, :]
    c["moba_past"] = np.where(nn_ < tt_ // 2, 0.0, -1e30).astype(np.float32).reshape(1, 128)
    c["moba_notown"] = np.where(nn_ == tt_ // 2, 0.0, 1.0).astype(np.float32).reshape(1, 128)
    bd = np.zeros((128, 256), np.float32)
    for h in range(4):
        bd[32 * h:32 * h + 32, 64 * h:64 * h + 64] = 1.0
    c["bd_gla"] = bd
    bd = np.zeros((128, 128), np.float32)
    for h in range(2):
        bd[64 * h:64 * h + 64, 64 * h:64 * h + 64] = 1.0
    c["bd_ret"] = bd
    tpos = np.arange(S, dtype=np.float64) - 1024.0
    c["ret_qd"] = np.stack([gam[h] ** tpos for h in range(4)]).astype(np.float32)
    c["ret_kd"] = np.stack([gam[h] ** (-tpos) for h in range(4)]).astype(np.float32)
    c["ret_q"] = rq.astype(np.float32)
    c["ret_k"] = rk.astype(np.float32)
    c["ret_kend"] = rke.astype(np.float32)
    c["ret_dec"] = rdec.astype(np.float32)
    return c


CONST_SPECS = {
    "ident_bf": ([128, 128], BF16), "ident_f": ([128, 128], F32), "tri_bf": ([128, 128], BF16),
    "swa_mask": ([128, 4, 2, 128], BF16), "moba_qa": ([4, 12, S], BF16), "moba_ka": ([4, 12, S], BF16),
    "cum_incl": ([128, 128], F32), "cum_after": ([128, 128], F32),
    "ret_q": ([128, 2, 128], F32), "ret_k": ([128, 2, 128], F32), "ret_kend": ([128, 256], F32),
    "ret_dec": ([128, 2], F32), "moba_past": ([1, 128], F32), "ret_qd": ([4, S], F32), "ret_kd": ([4, S], F32), "bd_gla": ([128, 256], F32), "bd_ret": ([128, 128], F32), "moba_notown": ([1, 128], F32),
}

IN_SPECS = {
    "x": [S, D], "c": [1, D], "w_ada": [DEPTH, D, 6 * D], "b_ada": [DEPTH, 6 * D], "g_norm_mix": [DEPTH, D],
    "w_in": [DEPTH, D, D_IN], "w_gla_gate": [DEPTH, 16, 128], "b_gla_gate": [DEPTH, 128],
    "g_gla_norm": [DEPTH, 256], "g_ret_norm": [DEPTH, 256], "attn_sinks": [DEPTH, 4],
    "w_branch": [DEPTH, 4, 256, D], "w_out": [DEPTH, D, D], "g_norm_ffn": [DEPTH, D],
    "w_router": [DEPTH, D, NE], "b_router": [DEPTH, NE], "w_gate_up": [DEPTH, NE, D, 2 * D],
    "b_gate_up": [DEPTH, NE, 2 * D], "w_down": [DEPTH, NE, D, D], "b_down": [DEPTH, NE, D],
    "g_final": [1, D],
}


def build(n_layers=DEPTH, stop_after=None, taps=()):
    nc = bass.Bass("TRN2", target_bir_lowering=False)
    small = stop_after is not None and not stop_after.startswith(("moe", "router"))
    dr = {k: nc.dram_tensor(k, shp, F32, kind="ExternalInput").ap() for k, shp in IN_SPECS.items()
          if not (small and k in ("w_gate_up", "w_down"))}
    cst = {k: nc.dram_tensor("k_" + k, shp, dt, kind="ExternalInput").ap() for k, (shp, dt) in CONST_SPECS.items()}
    out_d = nc.dram_tensor("out", [S, D], F32, kind="ExternalOutput").ap()
    tap_d = {}
    es = ExitStack()
    with es:
        C = Ctx(nc, es)

        uniq = [0]

        def sb(name, shape, dt, stack=es):
            uniq[0] += 1
            return stack.enter_context(nc.sbuf_tensor(f"{name}_{uniq[0]}", shape, dt))

        X = sb("X", [128, NT, D], F32)
        XT = tks(NT, "X")
        HT = sb("HT", [128, 8, S], BF16)
        HTk = [[Tk(f"HT{f}_{g}") for g in range(4)] for f in range(8)]
        ident_bf = sb("ident_bf", [128, 128], BF16)
        ident_f = sb("ident_f", [128, 128], F32)
        K_id = Tk("ident")
        PSB = [es.enter_context(nc.psum_tensor(f"ps{i}", [128, 512], F32)) for i in range(8)]
        PSk = tks(8, "ps")
        colsT = sb("colsT", [128, 64], F32)
        K_cols = Tk("colsT")
        modT = sb("modT", [128, 48], F32)
        K_mod = Tk("modT")
        AB = sb("AB", [128, 4, 8], F32)
        K_AB = Tk("AB")
        G12 = sb("G12", [128, 2, D], F32)
        K_G = tks(2, "G")
        cactT = sb("cactT", [128, 8], BF16)
        cactB = sb("cactB", [128, 8, 128], BF16)
        K_cact = Tk("cact")
        ss = sb("ss", [128, NT], F32)
        rstd = sb("rstd", [128, NT], F32)
        K_ss = tks(4, "ss")
        K_rstd = tks(4, "rstd")
        junk = sb("junk", [128, 2, D], BF16)
        K_junk = tks(2, "junk")

        def tap(name, ap, reads, shape, dt=F32):
            if name not in taps:
                return
            d = nc.dram_tensor("tap_" + name, list(shape), dt, kind="ExternalOutput").ap()
            tap_d[name] = d
            C.dma("sp", d, ap, reads=reads)

        def wload(dst, dst_tk, src2d, q="pool"):
            n = src2d.shape[1]
            sv = src2d.rearrange("(kt p) c -> p kt c", p=128)
            for c0 in range(0, n, 512):
                c1 = min(n, c0 + 512)
                C.dma(q, dst[:, :, c0:c1], sv[:, :, c0:c1], writes=[dst_tk])

        def ps_bf(i):
            return PSB[i][:].bitcast(BF16)

        C.dma("sp", ident_bf[:], cst["ident_bf"], writes=[K_id])
        C.dma("sp", ident_f[:], cst["ident_f"], writes=[K_id])
        xv = dr["x"].rearrange("(t p) d -> p t d", p=128)
        for g in range(4):
            C.dma("sp", X[:, 4 * g:4 * g + 4, :], xv[:, 4 * g:4 * g + 4, :], writes=XT[4 * g:4 * g + 4])

        with ExitStack() as ph:
            crow = sb("crow", [8, 128], F32, ph)
            K_crow = Tk()
            ccol = sb("ccol", [128, 8], F32, ph)
            K_ccol = Tk()
            C.dma("sp", crow[:], dr["c"].rearrange("o (kt p) -> (o kt) p", p=128), writes=[K_crow])
            C.op("pe", lambda e: e.transpose(PSB[0][:, 0:8], crow[:], ident_f[0:8, 0:8]),
                 reads=[K_crow, K_id], writes=[PSk[0]])
            C.op("act", lambda e: e.activation(out=ccol[:], in_=PSB[0][:, 0:8], func=AF.Silu),
                 reads=[PSk[0]], writes=[K_ccol])
            C.op("dve", lambda e: e.tensor_copy(out=cactT[:], in_=ccol[:]), reads=[K_ccol], writes=[K_cact])
            C.op("dve", lambda e: e.tensor_copy(out=cactB[:], in_=ccol[:].unsqueeze(2).to_broadcast([128, 8, 128])),
                 reads=[K_ccol], writes=[K_cact])
            C.barrier()

        def adaln(l):
            with ExitStack() as ph:
                rows = sb("rows", [64, 128], F32, ph)
                K_rows = Tk()
                C.dma("sp", rows[0:48, :], dr["b_ada"][l].rearrange("(r p) -> r p", p=128), writes=[K_rows])
                C.dma("sp", rows[48:56, :], dr["g_norm_mix"][l].rearrange("(r p) -> r p", p=128), writes=[K_rows])
                C.dma("sp", rows[56:64, :], dr["g_norm_ffn"][l].rearrange("(r p) -> r p", p=128), writes=[K_rows])
                C.op("pe", lambda e: e.transpose(PSB[0][:, 0:64], rows[:], ident_f[0:64, 0:64]),
                     reads=[K_rows, K_id], writes=[PSk[0]])
                C.op("dve", lambda e: e.tensor_copy(out=colsT[:], in_=PSB[0][:, 0:64]), reads=[PSk[0]], writes=[K_cols])
                wa = [sb(f"wa{i}", [128, 8, 512], BF16, ph) for i in range(2)]
                K_wa = tks(2, "wa")
                bbc = sb("bbc", [128, D], F32, ph)
                K_bbc = Tk()
                ci = 0
                for j in range(6):
                    for half in range(2):
                        w = ci % 2
                        ci += 1
                        c0 = j * D + half * 512
                        wload(wa[w][:], K_wa[w], dr["w_ada"][l][:, c0:c0 + 512])
                        if j in (2, 5):
                            gi = 0 if j == 2 else 1
                            pb = 2 + half
                            for kt in range(8):
                                C.op("pe", lambda e: e.matmul(PSB[pb][:], cactB[:, kt, :], wa[w][:, kt, :],
                                                              start=(kt == 0), stop=(kt == 7)),
                                     reads=[K_cact, K_wa[w]], writes=[PSk[pb]], inc=(kt == 7))
                            if half == 0:
                                C.dma("sp", bbc[:], dr["b_ada"][l][j * D:(j + 1) * D].partition_broadcast(128),
                                      writes=[K_bbc])
                            C.op("dve", lambda e: e.tensor_tensor(out=G12[:, gi, half * 512:(half + 1) * 512],
                                                                  in0=PSB[pb][:], in1=bbc[:, half * 512:(half + 1) * 512],
                                                                  op=ALU.add),
                                 reads=[PSk[pb], K_bbc], writes=[K_G[gi]])
                        else:
                            for fl in range(4):
                                col = j * 8 + half * 4 + fl
                                for kt in range(8):
                                    C.op("pe", lambda e: e.matmul(PSB[1][:, col:col + 1], wa[w][:, kt, fl * 128:(fl + 1) * 128],
                                                                  cactT[:, kt:kt + 1], start=(kt == 0), stop=(kt == 7)),
                                         reads=[K_cact, K_wa[w]], writes=[PSk[1]], inc=(kt == 7))
                for j in (0, 1, 3, 4):
                    C.op("dve", lambda e: e.tensor_tensor(out=modT[:, j * 8:(j + 1) * 8], in0=PSB[1][:, j * 8:(j + 1) * 8],
                                                          in1=colsT[:, j * 8:(j + 1) * 8], op=ALU.add),
                         reads=[PSk[1], K_cols], writes=[K_mod])
                C.op("dve", lambda e: e.scalar_tensor_tensor(out=AB[:, 0, :], in0=modT[:, 8:16], scalar=1.0,
                                                             in1=colsT[:, 48:56], op0=ALU.add, op1=ALU.mult),
                     reads=[K_mod, K_cols], writes=[K_AB])
                C.op("dve", lambda e: e.tensor_copy(out=AB[:, 1, :], in_=modT[:, 0:8]), reads=[K_mod], writes=[K_AB])
                C.op("dve", lambda e: e.scalar_tensor_tensor(out=AB[:, 2, :], in0=modT[:, 32:40], scalar=1.0,
                                                             in1=colsT[:, 56:64], op0=ALU.add, op1=ALU.mult),
                     reads=[K_mod, K_cols], writes=[K_AB])
                C.op("dve", lambda e: e.tensor_copy(out=AB[:, 3, :], in_=modT[:, 24:32]), reads=[K_mod], writes=[K_AB])
                C.barrier()

        def norm_to_HT(ai):
            with ExitStack() as ph:
                xn = sb("xn", [128, 4, D], BF16, ph)
                K_xn = tks(4, "xn")
                ev = 0
                for g in range(4):
                    for i in range(4):
                        tt = 4 * g + i
                        C.op("act", lambda e: e.activation(out=junk[:, i % 2, :], in_=X[:, tt, :], func=AF.Square,
                                                           accum_out=ss[:, tt:tt + 1]),
                             reads=[XT[tt]], writes=[K_junk[i % 2], K_ss[g]])
                    C.op("dve", lambda e: e.tensor_scalar(out=rstd[:, 4 * g:4 * g + 4], in0=ss[:, 4 * g:4 * g + 4],
                                                          scalar1=1.0 / D, scalar2=EPS, op0=ALU.mult, op1=ALU.add),
                         reads=[K_ss[g]], writes=[K_rstd[g]])
                    C.op("act", lambda e: e.activation(out=rstd[:, 4 * g:4 * g + 4], in_=rstd[:, 4 * g:4 * g + 4], func=AF.Sqrt),
                         reads=[K_rstd[g]], writes=[K_rstd[g]])
                    C.op("dve", lambda e: e.reciprocal(out=rstd[:, 4 * g:4 * g + 4], in_=rstd[:, 4 * g:4 * g + 4]),
                         reads=[K_rstd[g]], writes=[K_rstd[g]])
                    for i in range(4):
                        tt = 4 * g + i
                        C.op("dve", lambda e: e.tensor_scalar(out=xn[:, i, :], in0=X[:, tt, :], scalar1=rstd[:, tt:tt + 1],
                                                              scalar2=None, op0=ALU.mult),
                             reads=[XT[tt], K_rstd[g]], writes=[K_xn[i]])
                    for ft in range(8):
                        pb = ft % 4
                        for i in range(4):
                            C.op("pe", lambda e: e.transpose(ps_bf(pb)[:, i * 128:(i + 1) * 128],
                                                             xn[:, i, ft * 128:(ft + 1) * 128], ident_bf[:]),
                                 reads=[K_xn[i], K_id], writes=[PSk[pb]], inc=(i == 3))
                        dst = HT[:, ft, g * 512:(g + 1) * 512]
                        if ev % 2 == 0:
                            C.op("act", lambda e: e.activation(out=dst, in_=ps_bf(pb)[:, 0:512], func=AF.Identity,
                                                               bias=AB[:, ai + 1, ft:ft + 1], scale=AB[:, ai, ft:ft + 1]),
                                 reads=[PSk[pb], K_AB], writes=[HTk[ft][g]])
                        else:
                            C.op("dve", lambda e: e.tensor_scalar(out=dst, in0=ps_bf(pb)[:, 0:512],
                                                                  scalar1=AB[:, ai, ft:ft + 1], scalar2=AB[:, ai + 1, ft:ft + 1],
                                                                  op0=ALU.mult, op1=ALU.add),
                                 reads=[PSk[pb], K_AB], writes=[HTk[ft][g]])
                        ev += 1
                C.barrier()

        for l in range(n_layers):
            adaln(l)
            tap(f"modT{l}", modT[:], [K_mod], [128, 48])
            tap(f"G{l}", G12[:], K_G, [128, 2, D])
            norm_to_HT(0)
            tap(f"HT{l}", HT[:], [t for r in HTk for t in r], [128, 8, S], BF16)
            if stop_after == f"norm{l}":
                break

            with ExitStack() as mx:
                YT = sb("YT", [128, 8, S], BF16, mx)
                YTk = [tks(NT, f"YT{c}_") for c in range(8)]
                tri = sb("tri", [128, 128], BF16, mx)
                K_tri = Tk()
                C.dma("sp", tri[:], cst["tri_bf"], writes=[K_tri])
                ytile = sb("ytile", [128, 2, 256], BF16, mx)
                K_yt = tks(2, "yt")
                WIN = dr["w_in"][l]

                def emit_y(n, tt, par):
                    pb = 7
                    for ci in range(2):
                        C.op("pe", lambda e: e.transpose(ps_bf(pb)[:, ci * 128:(ci + 1) * 128], ytile[:, par, ci * 128:(ci + 1) * 128], ident_bf[:]),
                             reads=[K_yt[par], K_id], writes=[PSk[pb]], inc=(ci == 1))
                    C.op("act", lambda e: e.copy(out=YT[:, 2 * n:2 * n + 2, tt * 128:(tt + 1) * 128],
                                                 in_=ps_bf(pb)[:, 0:256].rearrange("p (c x) -> p c x", c=2)),
                         reads=[PSk[pb]], writes=[YTk[2 * n][tt], YTk[2 * n + 1][tt]])

                def HTr(g):
                    return [HTk[f][g] for f in range(8)]

                def proj_fm(dst_fn, w, K_w, c0, M, evi=[0]):
                    for g in range(4):
                        pb = evi[0] % 2
                        for kt in range(8):
                            C.op("pe", lambda e: e.matmul(PSB[pb][0:M, :], w[:, kt, c0:c0 + M], HT[:, kt, g * 512:(g + 1) * 512],
                                                          start=(kt == 0), stop=(kt == 7)),
                                 reads=[K_w] + HTr(g), writes=[PSk[pb]], inc=(kt == 7))
                        dst, K_dst = dst_fn(g)
                        if evi[0] % 2 == 0:
                            C.op("act", lambda e: e.copy(out=dst, in_=PSB[pb][0:M, :]), reads=[PSk[pb]], writes=[K_dst])
                        else:
                            C.op("dve", lambda e: e.tensor_copy(out=dst, in_=PSB[pb][0:M, :]), reads=[PSk[pb]], writes=[K_dst])
                        evi[0] += 1

                def proj_tm(dst_fn, w, K_w, c0, N, evi=[0]):
                    for tt in range(NT):
                        pb = 2 + evi[0] % 2
                        evi[0] += 1
                        for kt in range(8):
                            C.op("pe", lambda e: e.matmul(PSB[pb][:, 0:N], HT[:, kt, tt * 128:(tt + 1) * 128], w[:, kt, c0:c0 + N],
                                                          start=(kt == 0), stop=(kt == 7)),
                                 reads=[K_w] + HTr(tt // 4), writes=[PSk[pb]], inc=(kt == 7))
                        dst_fn(tt, pb)

                if "swa" not in SKIP:
                    with ExitStack() as ph:
                        QS = [sb(f"QS{h}", [64, S], BF16, ph) for h in range(4)]
                        K_QS = [tks(4, f"QS{h}_") for h in range(4)]
                        KS = [sb(f"KS{g}", [64, S], BF16, ph) for g in range(2)]
                        K_KS = [tks(4, f"KS{g}_") for g in range(2)]
                        VS = sb("VS", [128, NT, 2, 65], BF16, ph)
                        K_VS = tks(NT, "VS")
                        wq = sb("swq", [128, 8, 256], BF16, ph)
                        wkv = sb("swkv", [128, 8, 256], BF16, ph)
                        K_wq, K_wkv = Tk(), Tk()
                        msk = sb("smask", [128, 4, 2, 128], BF16, ph)
                        K_msk = Tk()
                        esink = sb("esink", [128, 4], F32, ph)
                        K_es = Tk()
                        wload(wq[:], K_wq, WIN[:, O_SQ:O_SQ + 256])
                        wload(wkv[:], K_wkv, WIN[:, O_SK:O_SK + 256])
                        C.dma("sp", msk[:], cst["swa_mask"], writes=[K_msk])
                        C.dma("sp", esink[:], dr["attn_sinks"][l].partition_broadcast(128), writes=[K_es])
                        C.op("act", lambda e: e.activation(out=esink[:], in_=esink[:], func=AF.Exp), reads=[K_es], writes=[K_es])
                        C.op("dve", lambda e: e.memset(VS[:, :, :, 64:65], 1.0), writes=K_VS)
                        for h in range(4):
                            proj_fm(lambda g: (QS[h][:, g * 512:(g + 1) * 512], K_QS[h][g]), wq, K_wq, h * 64, 64)
                        for g2 in range(2):
                            proj_fm(lambda g: (KS[g2][:, g * 512:(g + 1) * 512], K_KS[g2][g]), wkv, K_wkv, g2 * 64, 64)

                        def v_ev(tt, pb):
                            C.op("act", lambda e: e.copy(out=VS[:, tt, :, 0:64], in_=PSB[pb][:, 0:128].rearrange("p (g d) -> p g d", g=2)),
                                 reads=[PSk[pb]], writes=[K_VS[tt]])
                        proj_tm(v_ev, wkv, K_wkv, 128, 128)
                        EX = sb("sEX", [128, 2, 512], BF16, ph)
                        PT = sb("sPT", [128, 2, 512], BF16, ph)
                        K_EX, K_PT = tks(2), tks(2)
                        den = sb("sden", [128, 2, 4], F32, ph)
                        K_den = tks(2)
                        it = 0
                        for tt in range(NT):
                            par = tt % 2
                            po = 4 + par
                            for g2 in range(2):
                                pb = it % 2
                                bi = it % 2
                                it += 1
                                pvs = (1,) if tt == 0 else (0, 1)
                                for hh in range(2):
                                    for pv in pvs:
                                        kt = tt - 1 + pv
                                        o = (hh * 2 + pv) * 128
                                        C.op("pe", lambda e: e.matmul(PSB[pb][:, o:o + 128], KS[g2][:, kt * 128:(kt + 1) * 128],
                                                                      QS[2 * g2 + hh][:, tt * 128:(tt + 1) * 128], start=True, stop=True),
                                             reads=[K_KS[g2][kt // 4], K_QS[2 * g2 + hh][tt // 4]], writes=[PSk[pb]],
                                             inc=(hh == 1 and pv == 1))
                                C.op("act", lambda e: e.activation(out=EX[:, bi, :], in_=PSB[pb][:], func=AF.Exp, scale=0.125),
                                     reads=[PSk[pb]], writes=[K_EX[bi]])
                                C.op("dve", lambda e: e.tensor_tensor(out=PT[:, bi, :], in0=EX[:, bi, :],
                                                                      in1=msk[:, 2 * g2:2 * g2 + 2, :, :].rearrange("p a b c -> p (a b c)"),
                                                                      op=ALU.mult),
                                     reads=[K_EX[bi], K_msk], writes=[K_PT[bi]])
                                for hh in range(2):
                                    h = 2 * g2 + hh
                                    for pv in pvs:
                                        kt = tt - 1 + pv
                                        o = (hh * 2 + pv) * 128
                                        C.op("pe", lambda e: e.matmul(PSB[po][:, h * 65:(h + 1) * 65], PT[:, bi, o:o + 128], VS[:, kt, g2, :],
                                                                      start=(pv == pvs[0]), stop=(pv == 1)),
                                             reads=[K_PT[bi], K_VS[kt]], writes=[PSk[po]], inc=(pv == 1))
                            pov = PSB[po][:, 0:260].rearrange("p (h d) -> p h d", h=4)
                            C.op("dve", lambda e: e.tensor_tensor(out=den[:, par, :], in0=pov[:, :, 64], in1=esink[:], op=ALU.add),
                                 reads=[PSk[po], K_es], writes=[K_den[par]])
                            C.op("dve", lambda e: e.reciprocal(out=den[:, par, :], in_=den[:, par, :]), reads=[K_den[par]], writes=[K_den[par]])
                            C.op("dve", lambda e: e.tensor_tensor(out=ytile[:, par, :].rearrange("p (h d) -> p h d", h=4), in0=pov[:, :, 0:64],
                                                                  in1=den[:, par, :].unsqueeze(2).to_broadcast([128, 4, 64]), op=ALU.mult),
                                 reads=[PSk[po], K_den[par]], writes=[K_yt[par]])
                            emit_y(3, tt, par)
                        C.barrier()
                if stop_after == f"swa{l}":
                    tap(f"YT{l}", YT[:], [t for r in YTk for t in r], [128, 8, S], BF16)
                    break

                if "moba" not in SKIP:
                    with ExitStack() as ph:
                        QA = [sb(f"QA{h}", [76, S], BF16, ph) for h in range(4)]
                        K_QA = [tks(4, f"QA{h}_") for h in range(4)]
                        K_QAs = [tks(4, f"QAs{h}_") for h in range(4)]
                        KA = [sb(f"KA{h}", [76, S], BF16, ph) for h in range(4)]
                        K_KA = [tks(4, f"KA{h}_") for h in range(4)]
                        VM = sb("VM", [128, NT, 4, 65], BF16, ph)
                        K_VM = tks(NT, "VM")
                        past = sb("mpast", [128, 128], F32, ph)
                        notown = sb("mnotown", [128, 128], F32, ph)
                        phA = ExitStack()
                        wq = sb("mwq", [128, 8, 256], BF16, phA)
                        wk = sb("mwk", [128, 8, 256], BF16, phA)
                        wv = sb("mwv", [128, 8, 256], BF16, phA)
                        K_wq, K_wk, K_wv = Tk(), Tk(), Tk()
                        wload(wq[:], K_wq, WIN[:, O_MQ:O_MQ + 256])
                        wload(wk[:], K_wk, WIN[:, O_MK:O_MK + 256])
                        wload(wv[:], K_wv, WIN[:, O_MV:O_MV + 256])
                        K_aug = Tk()
                        for h in range(4):
                            C.dma("sp", QA[h][64:76, :], cst["moba_qa"][h], writes=[K_aug])
                            C.dma("sp", KA[h][64:76, :], cst["moba_ka"][h], writes=[K_aug])
                        K_pm = Tk()
                        C.dma("sp", past[:], cst["moba_past"][0].partition_broadcast(128), writes=[K_pm])
                        C.dma("sp", notown[:], cst["moba_notown"][0].partition_broadcast(128), writes=[K_pm])
                        C.op("dve", lambda e: e.memset(VM[:, :, :, 64:65], 1.0), writes=K_VM)
                        for h in range(4):
                            proj_fm(lambda g: (QA[h][0:64, g * 512:(g + 1) * 512], K_QA[h][g]), wq, K_wq, h * 64, 64)
                            proj_fm(lambda g: (KA[h][0:64, g * 512:(g + 1) * 512], K_KA[h][g]), wk, K_wk, h * 64, 64)

                        def vm_ev(tt, pb):
                            C.op("act", lambda e: e.copy(out=VM[:, tt, :, 0:64], in_=PSB[pb][:, 0:256].rearrange("p (g d) -> p g d", g=4)),
                                 reads=[PSk[pb]], writes=[K_VM[tt]])
                        proj_tm(vm_ev, wv, K_wv, 0, 256)
                        C.barrier()
                        phA.close()
                        phB = ExitStack()
                        kms = sb("kms", [64, 4, 8], F32, phB)
                        kmb = sb("kmb", [64, 4, 8], BF16, phB)
                        K_km = Tk()
                        for h in range(4):
                            C.op("dve", lambda e: e.tensor_reduce(out=kms[:, h, :], in_=KA[h][0:64, :].rearrange("p (n s) -> p n s", n=8),
                                                                  axis=AX.X, op=ALU.add),
                                 reads=K_KA[h], writes=[K_km])
                        C.op("dve", lambda e: e.tensor_copy(out=kmb[:], in_=kms[:]), reads=[K_km], writes=[K_km])
                        for h in range(4):
                            for tt in range(NT):
                                o = (h * NT + tt) * 8
                                C.op("pe", lambda e: e.matmul(PSB[0][:, o:o + 8], QA[h][0:64, tt * 128:(tt + 1) * 128], kmb[:, h, :],
                                                              start=True, stop=True),
                                     reads=[K_QA[h][tt // 4], K_km], writes=[PSk[0]], inc=(h == 3 and tt == NT - 1))
                        gm = sb("mgm", [128, 4, 128], F32, phB)
                        m8 = sb("mm8", [128, 64, 8], F32, phB)
                        selb = sb("mselb", [128, 4, 128], F32, phB)
                        SP = sb("mSP", [128, 64, 72], BF16, phB)
                        K_gm, K_m8, K_selb, K_SP = Tk(), Tk(), Tk(), Tk()
                        C.op("dve", lambda e: e.tensor_tensor(out=gm[:], in0=PSB[0][:].rearrange("p (h x) -> p h x", h=4),
                                                              in1=past[:].unsqueeze(1).to_broadcast([128, 4, 128]), op=ALU.add),
                             reads=[PSk[0], K_pm], writes=[K_gm])
                        gmv = gm[:].rearrange("p h (t n) -> p (h t) n", n=8)
                        for gi in range(64):
                            C.op("dve", lambda e: e.max(out=m8[:, gi, :], in_=gmv[:, gi, :]), reads=[K_gm], writes=[K_m8])
                        C.op("dve", lambda e: e.tensor_tensor(out=selb[:].rearrange("p h (t n) -> p (h t) n", n=8), in0=gmv,
                                                              in1=m8[:, :, 2:3].to_broadcast([128, 64, 8]), op=ALU.is_ge),
                             reads=[K_gm, K_m8], writes=[K_selb])
                        C.op("dve", lambda e: e.tensor_scalar(out=selb[:], in0=selb[:], scalar1=-1.0, scalar2=-NEG, op0=ALU.add, op1=ALU.mult),
                             reads=[K_selb], writes=[K_selb])
                        C.op("dve", lambda e: e.tensor_tensor(out=selb[:], in0=selb[:], in1=notown[:].unsqueeze(1).to_broadcast([128, 4, 128]),
                                                              op=ALU.mult),
                             reads=[K_selb, K_pm], writes=[K_selb])
                        C.op("dve", lambda e: e.memset(SP[:], 0.0), writes=[K_SP])
                        C.op("dve", lambda e: e.tensor_copy(out=SP[:, :, 64:72], in_=selb[:].rearrange("p h (t n) -> p (h t) n", n=8)),
                             reads=[K_selb], writes=[K_SP])
                        for h in range(4):
                            for g in range(4):
                                pb = 1 + (h * 4 + g) % 2
                                for i in range(4):
                                    tt = 4 * g + i
                                    C.op("pe", lambda e: e.matmul(PSB[pb][0:72, i * 128:(i + 1) * 128], SP[:, h * NT + tt, :], ident_bf[:],
                                                                  start=True, stop=True),
                                         reads=[K_SP, K_id], writes=[PSk[pb]], inc=(i == 3))
                                C.op("act", lambda e: e.copy(out=QA[h][64:72, g * 512:(g + 1) * 512], in_=PSB[pb][64:72, :]),
                                     reads=[PSk[pb], K_aug], writes=[K_QAs[h][g]])
                        C.barrier()
                        phB.close()
                        PTp = sb("mPTp", [128, 2, 8, 512], BF16, ph)
                        PTd = sb("mPTd", [128, 2, 384], BF16, ph)
                        K_PTp = [tks(8), tks(8)]
                        K_PTd = tks(2)
                        rd = sb("mrd", [128, 2, 4], F32, ph)
                        K_rd = tks(2)
                        it = 0
                        sc = 0
                        for b in range(8):
                            for h in range(4):
                                bi = it % 2
                                it += 1
                                qrd = [K_QA[h][b // 2], K_QAs[h][b // 2]]
                                for pr in range(b):
                                    pb = sc % 2
                                    sc += 1
                                    for j in range(2):
                                        kt = 2 * pr + j
                                        C.op("pe", lambda e: e.matmul(PSB[pb][:, j * 256:(j + 1) * 256], KA[h][:, kt * 128:(kt + 1) * 128],
                                                                      QA[h][:, b * 256:(b + 1) * 256], start=True, stop=True),
                                             reads=[K_KA[h][kt // 4], K_aug] + qrd, writes=[PSk[pb]], inc=(j == 1))
                                    C.op("act", lambda e: e.activation(out=PTp[:, bi, pr, :], in_=PSB[pb][:], func=AF.Exp, scale=0.125),
                                         reads=[PSk[pb]], writes=[K_PTp[bi][pr]])
                                pb = sc % 2
                                sc += 1
                                kt = 2 * b
                                C.op("pe", lambda e: e.matmul(PSB[pb][:, 0:256], KA[h][:, kt * 128:(kt + 1) * 128],
                                                              QA[h][:, b * 256:(b + 1) * 256], start=True, stop=True),
                                     reads=[K_KA[h][kt // 4], K_aug] + qrd, writes=[PSk[pb]], inc=False)
                                kt = 2 * b + 1
                                C.op("pe", lambda e: e.matmul(PSB[pb][:, 256:384], KA[h][:, kt * 128:(kt + 1) * 128],
                                                              QA[h][:, b * 256 + 128:(b + 1) * 256], start=True, stop=True),
                                     reads=[K_KA[h][kt // 4], K_aug] + qrd, writes=[PSk[pb]])
                                C.op("act", lambda e: e.activation(out=PTd[:, bi, :], in_=PSB[pb][:, 0:384], func=AF.Exp, scale=0.125),
                                     reads=[PSk[pb]], writes=[K_PTd[bi]])
                                C.op("dve", lambda e: e.tensor_tensor(out=PTd[:, bi, 0:128], in0=PTd[:, bi, 0:128], in1=tri[:], op=ALU.mult),
                                     reads=[K_PTd[bi], K_tri], writes=[K_PTd[bi]])
                                C.op("dve", lambda e: e.tensor_tensor(out=PTd[:, bi, 256:384], in0=PTd[:, bi, 256:384], in1=tri[:], op=ALU.mult),
                                     reads=[K_PTd[bi], K_tri], writes=[K_PTd[bi]])
                                for qi in range(2):
                                    po = 4 + qi
                                    for pr in range(b):
                                        for j in range(2):
                                            kt = 2 * pr + j
                                            C.op("pe", lambda e: e.matmul(PSB[po][:, h * 65:(h + 1) * 65],
                                                                          PTp[:, bi, pr, j * 256 + qi * 128:j * 256 + (qi + 1) * 128],
                                                                          VM[:, kt, h, :], start=(kt == 0), stop=False),
                                                 reads=[K_PTp[bi][pr], K_VM[kt]], writes=[PSk[po]], inc=False)
                                    C.op("pe", lambda e: e.matmul(PSB[po][:, h * 65:(h + 1) * 65], PTd[:, bi, qi * 128:(qi + 1) * 128],
                                                                  VM[:, 2 * b, h, :], start=(b == 0), stop=(qi == 0)),
                                         reads=[K_PTd[bi], K_VM[2 * b]], writes=[PSk[po]], inc=(qi == 0))
                                    if qi == 1:
                                        C.op("pe", lambda e: e.matmul(PSB[po][:, h * 65:(h + 1) * 65], PTd[:, bi, 256:384],
                                                                      VM[:, 2 * b + 1, h, :], start=False, stop=True),
                                             reads=[K_PTd[bi], K_VM[2 * b + 1]], writes=[PSk[po]])
                            for qi in range(2):
                                tt = 2 * b + qi
                                po = 4 + qi
                                par = qi
                                pov = PSB[po][:, 0:260].rearrange("p (h d) -> p h d", h=4)
                                C.op("dve", lambda e: e.reciprocal(out=rd[:, par, :], in_=pov[:, :, 64]), reads=[PSk[po]], writes=[K_rd[par]])
                                C.op("dve", lambda e: e.tensor_tensor(out=ytile[:, par, :].rearrange("p (h d) -> p h d", h=4), in0=pov[:, :, 0:64],
                                                                      in1=rd[:, par, :].unsqueeze(2).to_broadcast([128, 4, 64]), op=ALU.mult),
                                     reads=[PSk[po], K_rd[par]], writes=[K_yt[par]])
                                emit_y(0, tt, par)
                        C.barrier()
                if stop_after == f"moba{l}":
                    tap(f"YT{l}", YT[:], [t for r in YTk for t in r], [128, 8, S], BF16)
                    break

                for br in (("ret",) if stop_after in (f"retonly{l}", f"retall{l}") else tuple(b_ for b_ in ("gla", "ret") if b_ not in SKIP)):
                    with ExitStack() as ph:
                        gla = br == "gla"
                        ncol = 784 if gla else 1024
                        wg = sb("lw", [128, 8, ncol], BF16, ph)
                        K_wg = Tk()
                        wload(wg[:], K_wg, WIN[:, (O_GQ if gla else O_RQ):(O_GQ if gla else O_RQ) + ncol])
                        gbc = sb("lgbc", [128, 256], F32, ph)
                        K_gbc = Tk()
                        C.dma("sp", gbc[:], dr["g_gla_norm" if gla else "g_ret_norm"][l].partition_broadcast(128), writes=[K_gbc])
                        K_cn = Tk()
                        if gla:
                            wgg = sb("lwgg", [32, 128], BF16, ph)
                            C.dma("pool", wgg[0:16, :], dr["w_gla_gate"][l], writes=[K_cn])
                            C.dma("pool", wgg[16:17, :], dr["b_gla_gate"][l:l + 1, :], writes=[K_cn])
                            cinc = sb("lcinc", [128, 128], F32, ph)
                            caft = sb("lcaft", [128, 128], F32, ph)
                            C.dma("sp", cinc[:], cst["cum_incl"], writes=[K_cn])
                            C.dma("sp", caft[:], cst["cum_after"], writes=[K_cn])
                            gaT = sb("lgaT", [32, 2, 128], BF16, ph)
                            K_gaT = tks(2)
                            C.op("dve", lambda e: e.memset(gaT[:], 1.0), writes=K_gaT)
                            LA = sb("lLA", [128, 2, 128], F32, ph)
                            K_LA = tks(2)
                            EB = sb("lEB", [128, 2, 3, 128], F32, ph)
                            K_EB = tks(2)
                            dec = sb("ldec", [128, 2], F32, ph)
                            nft = 1
                            kd = 32
                        else:
                            rqc = sb("lrqc", [128, 2, 128], F32, ph)
                            rkc = sb("lrkc", [128, 2, 128], F32, ph)
                            rkend = sb("lrkend", [128, 256], F32, ph)
                            rdec = sb("lrdec", [128, 2], F32, ph)
                            C.dma("sp", rqc[:], cst["ret_q"], writes=[K_cn])
                            C.dma("sp", rkc[:], cst["ret_k"], writes=[K_cn])
                            C.dma("sp", rkend[:], cst["ret_kend"], writes=[K_cn])
                            K_rd_ = Tk()
                            for h_ in range(4):
                                gv_ = float((1.0 - 2.0 ** (-5.0 - h_)) ** 128.0)
                                o_ = (h_ % 2) * 64
                                C.op("dve", lambda e: e.memset(rdec[o_:o_ + 64, h_ // 2:h_ // 2 + 1], gv_), writes=[K_rd_])
                            nft = 2
                            kd = 64
                        qd = sb("lqd", [128, 2, nft, 128], BF16, ph)
                        kin = sb("lkin", [128, 2, 4, 128], BF16, ph)
                        K_kin = tks(2)
                        C.op("dve", lambda e: e.memset(kin[:], 0.0), writes=K_kin)
                        SW = 256 if gla else 128
                        bdm = sb("lbdm", [128, SW], F32, ph)
                        C.dma("sp", bdm[:], cst["bd_gla" if gla else "bd_ret"], writes=[K_cn])
                        kvt = sb("lkvt", [128, 2, SW], F32, ph)
                        K_kvt = tks(2)
                        kend = sb("lkend", [128, 2, nft * 128], BF16, ph)
                        vv = sb("lvv", [128, 2, 256], BF16, ph)
                        ggr = sb("lggr", [128, 2, 256], F32, ph)
                        atm = sb("latm", [128, 2, 4, 128], BF16, ph)
                        K_qd, K_kend, K_vv, K_ggr, K_atm = tks(2), tks(2), tks(2), tks(2), tks(2)
                        Sf = sb("lSf", [128, 2, nft, SW], F32, ph)
                        Sb = sb("lSb", [128, 2, nft, SW], BF16, ph)
                        K_Sf, K_Sb = tks(2), tks(2)
                        C.op("dve", lambda e: e.memset(Sf[:], 0.0), writes=K_Sf)
                        sq = sb("lsq", [128, 2, 256], F32, ph)
                        st = sb("lst", [128, 2, 3, 4], F32, ph)
                        K_sq, K_st = tks(2), tks(2)
                        t1 = sb("lt1", [128, 2, 256], F32, ph)
                        K_t1 = tks(2)
                        for tt in range((NT if gla else RETTILES) if stop_after != f"retonly{l}" else 1):
                            if RETV in (32, 33) and tt > 0 and not gla:
                                C.barrier()
                            _rc = RETCUT
                            if RETV in (20, 22, 32, 30):
                                _rc = 3 if tt == 0 else 2
                            if RETV == 21:
                                _rc = 3 if tt == 0 else 1
                            p = tt % 2 if RETV != 4 else 0
                            q = 1 - p
                            hr = HTr(tt // 4)
                            tok = slice(tt * 128, (tt + 1) * 128)

                            def mmK(out, lhs, rhs, wr, inc8=True):
                                for kt in range(8):
                                    C.op("pe", lambda e: e.matmul(out, lhs(kt), rhs(kt), start=(kt == 0), stop=(kt == 7)),
                                         reads=[K_wg] + hr, writes=[wr], inc=(kt == 7))
                            for j in range(nft):
                                mmK(PSB[0][:, j * 128:(j + 1) * 128], lambda kt: wg[:, kt, j * 128:(j + 1) * 128], lambda kt: HT[:, kt, tok], PSk[0])
                                ko = 128 if gla else 256
                                mmK(PSB[0][:, (nft + j) * 128:(nft + j + 1) * 128], lambda kt: wg[:, kt, ko + j * 128:ko + (j + 1) * 128],
                                    lambda kt: HT[:, kt, tok], PSk[0])
                            if gla:
                                mmK(PSB[1][0:16, 0:128], lambda kt: wg[:, kt, 512:528], lambda kt: HT[:, kt, tok], PSk[1])
                                C.op("act", lambda e: e.copy(out=gaT[0:16, p, :], in_=PSB[1][0:16, 0:128]), reads=[PSk[1]], writes=[K_gaT[p]])
                                C.op("pe", lambda e: e.matmul(PSB[1][:, 128:256], gaT[0:17, p, :], wgg[0:17, :], start=True, stop=True),
                                     reads=[K_gaT[p], K_cn], writes=[PSk[1]])
                                C.op("act", lambda e: e.activation(out=LA[:, p, :], in_=PSB[1][:, 128:256], func=AF.Exp, scale=-1.0),
                                     reads=[PSk[1]], writes=[K_LA[p]])
                                C.op("act", lambda e: e.activation(out=LA[:, p, :], in_=LA[:, p, :], func=AF.Ln, bias=1.0),
                                     reads=[K_LA[p]], writes=[K_LA[p]])
                                C.op("pe", lambda e: e.matmul(PSB[1][:, 256:384], LA[:, p, :], cinc[:], start=True, stop=True),
                                     reads=[K_LA[p], K_cn], writes=[PSk[1]])
                                C.op("pe", lambda e: e.matmul(PSB[1][:, 384:512], caft[:], LA[:, p, :], start=True, stop=True),
                                     reads=[K_LA[p], K_cn], writes=[PSk[1]])
                                C.op("act", lambda e: e.activation(out=EB[:, p, 0, :], in_=PSB[1][:, 256:384], func=AF.Exp), reads=[PSk[1]], writes=[K_EB[p]])
                                C.op("act", lambda e: e.activation(out=EB[:, p, 1, :], in_=PSB[1][:, 256:384], func=AF.Exp, scale=-1.0), reads=[PSk[1]], writes=[K_EB[p]])
                                C.op("act", lambda e: e.activation(out=EB[:, p, 2, :], in_=PSB[1][:, 384:512], func=AF.Exp), reads=[PSk[1]], writes=[K_EB[p]])
                                C.op("dve", lambda e: e.scalar_tensor_tensor(out=qd[:, p, 0, :], in0=PSB[0][:, 0:128], scalar=32.0 ** -0.5, in1=EB[:, p, 0, :],
                                                                             op0=ALU.mult, op1=ALU.mult), reads=[PSk[0], K_EB[p]], writes=[K_qd[p]])
                                for h in range(4):
                                    o = 32 * h
                                    C.op("dve", lambda e: e.tensor_tensor(out=kin[o:o + 32, p, h, :], in0=PSB[0][o:o + 32, 128:256], in1=EB[o:o + 32, p, 1, :], op=ALU.mult),
                                         reads=[PSk[0], K_EB[p]], writes=[K_kin[p]])
                            else:
                                if RETV in (10, 12):
                                    mmK(PSB[1][0:16, 256:384], lambda kt: wg[:, kt, 0:16], lambda kt: HT[:, kt, tok], PSk[1])
                                if RETV in (11, 12):
                                    C.op("pe", lambda e: e.matmul(PSB[1][:, 384:512], rqc[:, 0, :], rkc[:, 0, :], start=True, stop=True),
                                         reads=[K_cn], writes=[PSk[1]])
                                C.op("dve", lambda e: e.tensor_tensor(out=qd[:, p, :, :], in0=PSB[0][:, 0:256].rearrange("p (j x) -> p j x", j=2),
                                                                      in1=rqc[:], op=ALU.mult), reads=[PSk[0], K_cn], writes=[K_qd[p]])
                                for h in range(4):
                                    j, o = h // 2, (h % 2) * 64
                                    C.op("dve", lambda e: e.tensor_tensor(out=kin[o:o + 64, p, h, :], in0=PSB[0][o:o + 64, 256 + j * 128:256 + (j + 1) * 128],
                                                                          in1=rkc[o:o + 64, j, :], op=ALU.mult), reads=[PSk[0], K_cn], writes=[K_kin[p]])
                            if _rc <= 1:
                                continue
                            nk = nft * 128
                            ko = 128 if gla else 256
                            if RETV == 30:
                                mmK(PSB[2][:, 0:nk], lambda kt: HT[:, kt, tok], lambda kt: wg[:, kt, ko:ko + nk], PSk[2])
                                mmK(PSB[2][:, nk:nk + 256], lambda kt: HT[:, kt, tok], lambda kt: wg[:, kt, ko + nk:ko + nk + 256], PSk[2])
                            else:
                                mmK(PSB[2][:, 0:nk + 256], lambda kt: HT[:, kt, tok], lambda kt: wg[:, kt, ko:ko + nk + 256], PSk[2])
                            go = 528 if gla else 768
                            mmK(PSB[3][:, 0:256], lambda kt: HT[:, kt, tok], lambda kt: wg[:, kt, go:go + 256], PSk[3])
                            if gla:
                                C.op("dve", lambda e: e.tensor_tensor(out=kend[:, p, :], in0=PSB[2][:, 0:128], in1=EB[:, p, 2, :], op=ALU.mult),
                                     reads=[PSk[2], K_EB[p]], writes=[K_kend[p]])
                            else:
                                C.op("dve", lambda e: e.tensor_tensor(out=kend[:, p, :], in0=PSB[2][:, 0:256], in1=rkend[:], op=ALU.mult),
                                     reads=[PSk[2], K_cn], writes=[K_kend[p]])
                            C.op("act", lambda e: e.copy(out=vv[:, p, :], in_=PSB[2][:, nk:nk + 256]), reads=[PSk[2]], writes=[K_vv[p]])
                            C.op("act", lambda e: e.activation(out=ggr[:, p, :], in_=PSB[3][:, 0:256], func=AF.Silu), reads=[PSk[3]], writes=[K_ggr[p]])
                            C.op("dve", lambda e: e.tensor_tensor(out=ggr[:, p, :], in0=ggr[:, p, :], in1=gbc[:], op=ALU.mult),
                                 reads=[K_ggr[p], K_gbc], writes=[K_ggr[p]])
                            if _rc <= 2:
                                continue
                            for h in range(4 if not (RETV == 9 and tt == 1) else 0):
                                j = 0 if gla else h // 2
                                C.op("pe", lambda e: e.matmul(PSB[4][:, h * 128:(h + 1) * 128], kin[:, p, h, :], qd[:, p, j, :],
                                                              start=True, stop=True),
                                     reads=[K_kin[p], K_qd[p]], writes=[PSk[4]], inc=(h == 3))
                            if not (RETV in (9, 13) and tt == 1):
                                C.op("dve", lambda e: e.tensor_tensor(out=atm[:, p, :, :], in0=PSB[4][:].rearrange("p (h x) -> p h x", h=4),
                                                                      in1=tri[:].unsqueeze(1).to_broadcast([128, 4, 128]), op=ALU.mult),
                                     reads=[PSk[4], K_tri], writes=[K_atm[p]])
                            obank = [5, 6]
                            if tt > 0 and RETV not in (1, 5, 6, 7, 8):
                                for j in range(nft if RETV != 2 else 1):
                                    C.op("pe", lambda e: e.matmul(PSB[obank[j]][:, 0:SW], qd[:, p, j, :], Sb[:, q, j, :], start=True, stop=False),
                                         reads=[K_qd[p], K_Sb[q]], writes=[PSk[obank[j]]], inc=(RETV == 3))
                            for h in range(4):
                                if (tt == 1 and RETV in (5, 13)) or (tt == 0 and RETV == 22) or (tt == 1 and False) or (tt == 1 and (False or (RETV == 6 and h >= 2) or (RETV == 8 and h < 2))):
                                    continue
                                j, oc = (0, h * 64) if gla else (h // 2, (h % 2) * 64)
                                C.op("pe", lambda e: e.matmul(PSB[obank[j]][:, oc:oc + 64], atm[:, p, h, :], vv[:, p, h * 64:(h + 1) * 64],
                                                              start=(tt == 0 or RETV in (1, 5, 6, 7, 8, 9, 13) or (RETV == 2 and h >= 2)), stop=(tt == 0 or h == 3 or (not gla and h == 1))),
                                     reads=[K_atm[p], K_vv[p]], writes=[PSk[obank[j]]], inc=True)
                            if _rc <= 3:
                                continue
                            if tt < NT - 1:
                                kvb = 1 if not gla else 3
                                for j in range(nft):
                                    C.op("pe", lambda e: e.matmul(PSB[kvb][:, 256:256 + SW] if gla else PSB[kvb][:, j * 128:(j + 1) * 128],
                                                                  kend[:, p, j * 128:(j + 1) * 128], vv[:, p, j * SW:(j + 1) * SW] if not gla else vv[:, p, :],
                                                                  start=True, stop=True),
                                         reads=[K_kend[p], K_vv[p]], writes=[PSk[kvb]])
                                    src = PSB[kvb][:, 256:256 + SW] if gla else PSB[kvb][:, j * 128:(j + 1) * 128]
                                    C.op("dve", lambda e: e.tensor_tensor(out=kvt[:, j if not gla else 0, :], in0=src, in1=bdm[:], op=ALU.mult),
                                         reads=[PSk[kvb], K_cn], writes=[K_kvt[j]])
                                    if gla:
                                        C.op("act", lambda e: e.copy(out=dec[:, p:p + 1], in_=EB[:, p, 0, 127:128]), reads=[K_EB[p]], writes=[K_EB[p]])
                                        dsc = dec[:, p:p + 1]
                                    else:
                                        dsc = rdec[:, j:j + 1]
                                    C.op("dve", lambda e: e.scalar_tensor_tensor(out=Sf[:, p, j, :], in0=Sf[:, q, j, :], scalar=dsc,
                                                                                 in1=kvt[:, j if not gla else 0, :], op0=ALU.mult, op1=ALU.add),
                                         reads=[K_Sf[q], K_kvt[j], K_EB[p] if gla else K_cn], writes=[K_Sf[p]])
                                C.op("act", lambda e: e.copy(out=Sb[:, p, :, :], in_=Sf[:, p, :, :]), reads=[K_Sf[p]], writes=[K_Sb[p]])
                            if _rc <= 4:
                                continue
                            osb = t1
                            for j in range(nft):
                                C.op("act", lambda e: e.copy(out=t1[:, p, j * SW:(j + 1) * SW], in_=PSB[obank[j]][:, 0:SW]), reads=[PSk[obank[j]]], writes=[K_t1[p]])
                            ov = t1[:, p, :].rearrange("p (h d) -> p h d", h=4)
                            C.op("act", lambda e: e.activation(out=sq[:, p, :], in_=t1[:, p, :], func=AF.Square), reads=[K_t1[p]], writes=[K_sq[p]])
                            C.op("dve", lambda e: e.tensor_reduce(out=st[:, p, 0, :], in_=sq[:, p, :].rearrange("p (h d) -> p h d", h=4), axis=AX.X, op=ALU.add),
                                 reads=[K_sq[p]], writes=[K_st[p]])
                            if _rc <= 4.2:
                                continue
                            if gla:
                                C.op("dve", lambda e: e.tensor_scalar(out=st[:, p, 0, :], in0=st[:, p, 0, :], scalar1=1.0 / 64, scalar2=EPS, op0=ALU.mult, op1=ALU.add),
                                     reads=[K_st[p]], writes=[K_st[p]])
                            else:
                                C.op("dve", lambda e: e.tensor_reduce(out=st[:, p, 1, :], in_=ov, axis=AX.X, op=ALU.add), reads=[K_t1[p]], writes=[K_st[p]])
                                C.op("dve", lambda e: e.tensor_scalar(out=st[:, p, 1, :], in0=st[:, p, 1, :], scalar1=-1.0 / 64, scalar2=None, op0=ALU.mult),
                                     reads=[K_st[p]], writes=[K_st[p]])
                                C.op("dve", lambda e: e.tensor_tensor(out=st[:, p, 2, :], in0=st[:, p, 1, :], in1=st[:, p, 1, :], op=ALU.mult),
                                     reads=[K_st[p]], writes=[K_st[p]])
                                C.op("dve", lambda e: e.scalar_tensor_tensor(out=st[:, p, 0, :], in0=st[:, p, 0, :], scalar=1.0 / 64, in1=st[:, p, 2, :],
                                                                             op0=ALU.mult, op1=ALU.subtract), reads=[K_st[p]], writes=[K_st[p]])
                                C.op("dve", lambda e: e.tensor_scalar(out=st[:, p, 0, :], in0=st[:, p, 0, :], scalar1=EPS, scalar2=None, op0=ALU.add),
                                     reads=[K_st[p]], writes=[K_st[p]])
                            if _rc <= 4.4:
                                continue
                            C.op("act", lambda e: e.activation(out=st[:, p, 0, :], in_=st[:, p, 0, :], func=AF.Sqrt), reads=[K_st[p]], writes=[K_st[p]])
                            C.op("dve", lambda e: e.reciprocal(out=st[:, p, 0, :], in_=st[:, p, 0, :]), reads=[K_st[p]], writes=[K_st[p]])
                            if _rc <= 4.6:
                                continue
                            t1v = t1[:, p, :].rearrange("p (h d) -> p h d", h=4)
                            if gla:
                                C.op("dve", lambda e: e.tensor_tensor(out=t1v, in0=ov, in1=st[:, p, 0, :].unsqueeze(2).to_broadcast([128, 4, 64]), op=ALU.mult),
                                     reads=[K_t1[p], K_st[p]], writes=[K_t1[p]])
                            else:
                                C.op("dve", lambda e: e.tensor_tensor(out=t1v, in0=ov, in1=st[:, p, 1, :].unsqueeze(2).to_broadcast([128, 4, 64]), op=ALU.add),
                                     reads=[K_t1[p], K_st[p]], writes=[K_t1[p]])
                                C.op("dve", lambda e: e.tensor_tensor(out=t1v, in0=t1v, in1=st[:, p, 0, :].unsqueeze(2).to_broadcast([128, 4, 64]), op=ALU.mult),
                                     reads=[K_t1[p], K_st[p]], writes=[K_t1[p]])
                            if _rc <= 4.8:
                                continue
                            C.op("dve", lambda e: e.tensor_tensor(out=ytile[:, p, :], in0=t1[:, p, :], in1=ggr[:, p, :], op=ALU.mult),
                                 reads=[K_t1[p], K_ggr[p]], writes=[K_yt[p]])
                            if _rc <= 5:
                                continue
                            emit_y(1 if gla else 2, tt, p)
                        C.barrier()
                    if stop_after == f"{br}{l}":
                        break
                    if stop_after == f"retall{l}" and "memsetYT" not in SKIP:
                        pass
                if "retq" not in SKIP:
                    with ExitStack() as ph:
                        VR = sb("VR", [128, NT, 4, 64], BF16, ph)
                        K_VR = tks(NT, "VR")
                        GG = sb("GG", [128, NT, 256], BF16, ph)
                        K_GG = tks(NT, "GG")
                        gbc = sb("rgbc", [128, 256], F32, ph)
                        K_gbc = Tk()
                        C.dma("sp", gbc[:], dr["g_ret_norm"][l].partition_broadcast(128), writes=[K_gbc])
                        gtmp = sb("rgtmp", [128, 2, 256], F32, ph)
                        K_gtmp = tks(2)
                        PTp = sb("rPTp", [128, 8, 512], BF16, ph)
                        PTd = sb("rPTd", [128, 2, 384], BF16, ph)
                        K_PTp = tks(8)
                        K_PTd = tks(2)
                        t1 = sb("rt1", [128, 2, 128], F32, ph)
                        sq = sb("rsq", [128, 2, 128], F32, ph)
                        st = sb("rst", [128, 2, 3, 2], F32, ph)
                        K_t1, K_sq, K_st = tks(2), tks(2), tks(2)
                        QR = [sb(f"QR{i}", [64, S], BF16, ph) for i in range(2)]
                        KR = [sb(f"KR{i}", [64, S], BF16, ph) for i in range(2)]
                        K_QR = [tks(4), tks(4)]
                        K_KR = [tks(4), tks(4)]
                        with ExitStack() as phA:
                            wvg = sb("rwvg", [128, 8, 512], BF16, phA)
                            K_wvg = Tk()
                            wload(wvg[:], K_wvg, WIN[:, O_RV:O_RV + 512])

                            def vr_ev(tt, pb):
                                C.op("act", lambda e: e.copy(out=VR[:, tt, :, :], in_=PSB[pb][:, 0:256].rearrange("p (g d) -> p g d", g=4)),
                                     reads=[PSk[pb]], writes=[K_VR[tt]])
                            proj_tm(vr_ev, wvg, K_wvg, 0, 256)

                            def gg_ev(tt, pb):
                                bi = tt % 2
                                C.op("act", lambda e: e.activation(out=gtmp[:, bi, :], in_=PSB[pb][:, 0:256], func=AF.Silu), reads=[PSk[pb]], writes=[K_gtmp[bi]])
                                C.op("dve", lambda e: e.tensor_tensor(out=GG[:, tt, :], in0=gtmp[:, bi, :], in1=gbc[:], op=ALU.mult),
                                     reads=[K_gtmp[bi], K_gbc], writes=[K_GG[tt]])
                            proj_tm(gg_ev, wvg, K_wvg, 256, 256)
                            C.barrier()
                        for pair in range(2):
                            with ExitStack() as phB:
                                wqk = sb("rwqk", [128, 8, 256], BF16, phB)
                                K_wqk = Tk()
                                wload(wqk[:, :, 0:128], K_wqk, WIN[:, O_RQ + pair * 128:O_RQ + (pair + 1) * 128])
                                wload(wqk[:, :, 128:256], K_wqk, WIN[:, O_RK + pair * 128:O_RK + (pair + 1) * 128])
                                dq = sb("rdq", [64, 1, S], BF16, phB)
                                dk = sb("rdk", [64, 1, S], BF16, phB)
                                K_dqk = Tk()
                                ev = 0
                                for hh in range(2):
                                    C.dma("pool", dq[:, 0, :], cst["ret_qd"][2 * pair + hh].partition_broadcast(64), writes=[K_dqk])
                                    C.dma("pool", dk[:, 0, :], cst["ret_kd"][2 * pair + hh].partition_broadcast(64), writes=[K_dqk])
                                    for which in range(2):
                                        for g in range(4):
                                            pb = ev % 2
                                            ev += 1
                                            c0 = which * 128 + hh * 64
                                            for kt in range(8):
                                                C.op("pe", lambda e: e.matmul(PSB[pb][0:64, :], wqk[:, kt, c0:c0 + 64], HT[:, kt, g * 512:(g + 1) * 512],
                                                                              start=(kt == 0), stop=(kt == 7)),
                                                     reads=[K_wqk] + HTr(g), writes=[PSk[pb]], inc=(kt == 7))
                                            dst = (QR if which == 0 else KR)[hh][:, g * 512:(g + 1) * 512]
                                            dtk = (K_QR if which == 0 else K_KR)[hh][g]
                                            dec_ = (dq if which == 0 else dk)[:, 0, g * 512:(g + 1) * 512]
                                            C.op("dve", lambda e: e.tensor_tensor(out=dst, in0=PSB[pb][0:64, :], in1=dec_, op=ALU.mult),
                                                 reads=[PSk[pb], K_dqk], writes=[dtk])
                                C.barrier()
                            sc = 0
                            for b in range(8):
                                for hh in range(2):
                                    h = 2 * pair + hh
                                    for pr in range(b):
                                        pb = sc % 2
                                        sc += 1
                                        for j in range(2):
                                            kt = 2 * pr + j
                                            C.op("pe", lambda e: e.matmul(PSB[pb][:, j * 256:(j + 1) * 256], KR[hh][:, kt * 128:(kt + 1) * 128],
                                                                          QR[hh][:, b * 256:(b + 1) * 256], start=True, stop=True),
                                                 reads=[K_KR[hh][kt // 4], K_QR[hh][b // 2]], writes=[PSk[pb]], inc=(j == 1))
                                        C.op("act", lambda e: e.activation(out=PTp[:, pr, :], in_=PSB[pb][:], func=AF.Identity, scale=0.125),
                                             reads=[PSk[pb]], writes=[K_PTp[pr]])
                                    pb = sc % 2
                                    sc += 1
                                    bi = hh
                                    kt = 2 * b
                                    C.op("pe", lambda e: e.matmul(PSB[pb][:, 0:256], KR[hh][:, kt * 128:(kt + 1) * 128],
                                                                  QR[hh][:, b * 256:(b + 1) * 256], start=True, stop=True),
                                         reads=[K_KR[hh][kt // 4], K_QR[hh][b // 2]], writes=[PSk[pb]], inc=False)
                                    kt = 2 * b + 1
                                    C.op("pe", lambda e: e.matmul(PSB[pb][:, 256:384], KR[hh][:, kt * 128:(kt + 1) * 128],
                                                                  QR[hh][:, b * 256 + 128:(b + 1) * 256], start=True, stop=True),
                                         reads=[K_KR[hh][kt // 4], K_QR[hh][b // 2]], writes=[PSk[pb]])
                                    C.op("act", lambda e: e.activation(out=PTd[:, bi, :], in_=PSB[pb][:, 0:384], func=AF.Identity, scale=0.125),
                                         reads=[PSk[pb]], writes=[K_PTd[bi]])
                                    C.op("dve", lambda e: e.tensor_tensor(out=PTd[:, bi, 0:128], in0=PTd[:, bi, 0:128], in1=tri[:], op=ALU.mult),
                                         reads=[K_PTd[bi], K_tri], writes=[K_PTd[bi]])
                                    C.op("dve", lambda e: e.tensor_tensor(out=PTd[:, bi, 256:384], in0=PTd[:, bi, 256:384], in1=tri[:], op=ALU.mult),
                                         reads=[K_PTd[bi], K_tri], writes=[K_PTd[bi]])
                                    for qi in range(2):
                                        po = 4 + qi
                                        for pr in range(b):
                                            for j in range(2):
                                                kt = 2 * pr + j
                                                C.op("pe", lambda e: e.matmul(PSB[po][:, hh * 64:(hh + 1) * 64],
                                                                              PTp[:, pr, j * 256 + qi * 128:j * 256 + (qi + 1) * 128],
                                                                              VR[:, kt, h, :], start=(kt == 0), stop=False),
                                                     reads=[K_PTp[pr], K_VR[kt]], writes=[PSk[po]], inc=False)
                                        C.op("pe", lambda e: e.matmul(PSB[po][:, hh * 64:(hh + 1) * 64], PTd[:, bi, qi * 128:(qi + 1) * 128],
                                                                      VR[:, 2 * b, h, :], start=(b == 0), stop=(qi == 0)),
                                             reads=[K_PTd[bi], K_VR[2 * b]], writes=[PSk[po]], inc=(qi == 0))
                                        if qi == 1:
                                            C.op("pe", lambda e: e.matmul(PSB[po][:, hh * 64:(hh + 1) * 64], PTd[:, bi, 256:384],
                                                                          VR[:, 2 * b + 1, h, :], start=False, stop=True),
                                                 reads=[K_PTd[bi], K_VR[2 * b + 1]], writes=[PSk[po]])
                                for qi in range(2):
                                    tt = 2 * b + qi
                                    po = 4 + qi
                                    p = qi
                                    C.op("act", lambda e: e.copy(out=t1[:, p, :], in_=PSB[po][:, 0:128]), reads=[PSk[po]], writes=[K_t1[p]])
                                    C.op("act", lambda e: e.activation(out=sq[:, p, :], in_=t1[:, p, :], func=AF.Square), reads=[K_t1[p]], writes=[K_sq[p]])
                                    ov = t1[:, p, :].rearrange("p (h d) -> p h d", h=2)
                                    C.op("dve", lambda e: e.tensor_reduce(out=st[:, p, 0, :], in_=sq[:, p, :].rearrange("p (h d) -> p h d", h=2), axis=AX.X, op=ALU.add),
                                         reads=[K_sq[p]], writes=[K_st[p]])
                                    C.op("dve", lambda e: e.tensor_reduce(out=st[:, p, 1, :], in_=ov, axis=AX.X, op=ALU.add), reads=[K_t1[p]], writes=[K_st[p]])
                                    C.op("dve", lambda e: e.tensor_scalar(out=st[:, p, 1, :], in0=st[:, p, 1, :], scalar1=-1.0 / 64, scalar2=None, op0=ALU.mult),
                                         reads=[K_st[p]], writes=[K_st[p]])
                                    C.op("dve", lambda e: e.tensor_tensor(out=st[:, p, 2, :], in0=st[:, p, 1, :], in1=st[:, p, 1, :], op=ALU.mult),
                                         reads=[K_st[p]], writes=[K_st[p]])
                                    C.op("dve", lambda e: e.scalar_tensor_tensor(out=st[:, p, 0, :], in0=st[:, p, 0, :], scalar=1.0 / 64, in1=st[:, p, 2, :],
                                                                                 op0=ALU.mult, op1=ALU.subtract), reads=[K_st[p]], writes=[K_st[p]])
                                    C.op("dve", lambda e: e.tensor_scalar(out=st[:, p, 0, :], in0=st[:, p, 0, :], scalar1=EPS, scalar2=None, op0=ALU.add),
                                         reads=[K_st[p]], writes=[K_st[p]])
                                    C.op("act", lambda e: e.activation(out=st[:, p, 0, :], in_=st[:, p, 0, :], func=AF.Sqrt), reads=[K_st[p]], writes=[K_st[p]])
                                    C.op("dve", lambda e: e.reciprocal(out=st[:, p, 0, :], in_=st[:, p, 0, :]), reads=[K_st[p]], writes=[K_st[p]])
                                    C.op("dve", lambda e: e.tensor_tensor(out=ov, in0=ov, in1=st[:, p, 1, :].unsqueeze(2).to_broadcast([128, 2, 64]), op=ALU.add),
                                         reads=[K_t1[p], K_st[p]], writes=[K_t1[p]])
                                    C.op("dve", lambda e: e.tensor_tensor(out=ov, in0=ov, in1=st[:, p, 0, :].unsqueeze(2).to_broadcast([128, 2, 64]), op=ALU.mult),
                                         reads=[K_t1[p], K_st[p]], writes=[K_t1[p]])
                                    C.op("dve", lambda e: e.tensor_tensor(out=ytile[:, p, 0:128], in0=t1[:, p, :], in1=GG[:, tt, pair * 128:(pair + 1) * 128], op=ALU.mult),
                                         reads=[K_t1[p], K_GG[tt]], writes=[K_yt[p]])
                                    C.op("pe", lambda e: e.transpose(ps_bf(7)[:, 0:128], ytile[:, p, 0:128], ident_bf[:]),
                                         reads=[K_yt[p], K_id], writes=[PSk[7]])
                                    C.op("act", lambda e: e.copy(out=YT[:, 4 + pair, tt * 128:(tt + 1) * 128], in_=ps_bf(7)[:, 0:128]),
                                         reads=[PSk[7]], writes=[YTk[4 + pair][tt]])
                            C.barrier()
                if stop_after == f"retq{l}":
                    tap(f"YT{l}", YT[:], [t for r in YTk for t in r], [128, 8, S], BF16)
                    break
                if stop_after in (f"gla{l}", f"ret{l}", f"retonly{l}", f"retall{l}"):
                    tap(f"YT{l}", YT[:], [t for r in YTk for t in r], [128, 8, S], BF16)
                    break

                with ExitStack() as ph:
                    MP = sb("MP", [128, 8, S], BF16, ph)
                    MPk = [tks(4, f"MP{f}_") for f in range(8)]
                    with ExitStack() as ph2:
                        wm = sb("wm", [128, 2, 8, 256], BF16, ph2)
                        wbr = sb("wbr", [128, 2, 2, D], BF16, ph2)
                        K_wm, K_wbr = tks(2), tks(2)
                        sig = sb("sig", [128, 2, 512], BF16, ph2)
                        prod = sb("prod", [128, 2, 512], F32, ph2)
                        K_sig, K_prod = tks(2), tks(2)
                        ci_ = 0
                        it = 0
                        for n in range(4):
                            wb_ = n % 2
                            for hf_ in range(2):
                                C.dma("pool", wbr[:, wb_, :, hf_ * 512:(hf_ + 1) * 512],
                                      dr["w_branch"][l][n].rearrange("(ci p) d -> p ci d", p=128)[:, :, hf_ * 512:(hf_ + 1) * 512], writes=[K_wbr[wb_]])
                            for fp in range(4):
                                w_ = ci_ % 2
                                ci_ += 1
                                c0 = O_MG + n * D + fp * 256
                                wload(wm[:, w_, :, :], K_wm[w_], WIN[:, c0:c0 + 256])
                                for fl in range(2):
                                    ft = fp * 2 + fl
                                    for g in range(4):
                                        b0, b1, bi = it % 2, 2 + it % 2, it % 2
                                        it += 1
                                        for kt in range(8):
                                            C.op("pe", lambda e: e.matmul(PSB[b0][:], wm[:, w_, kt, fl * 128:(fl + 1) * 128], HT[:, kt, g * 512:(g + 1) * 512],
                                                                          start=(kt == 0), stop=(kt == 7)),
                                                 reads=[K_wm[w_]] + HTr(g), writes=[PSk[b0]], inc=(kt == 7))
                                        for ci in range(2):
                                            C.op("pe", lambda e: e.matmul(PSB[b1][:], wbr[:, wb_, ci, ft * 128:(ft + 1) * 128], YT[:, 2 * n + ci, g * 512:(g + 1) * 512],
                                                                          start=(ci == 0), stop=(ci == 1)),
                                                 reads=[K_wbr[wb_]] + YTk[2 * n + ci][4 * g:4 * g + 4], writes=[PSk[b1]], inc=(ci == 1))
                                        C.op("act", lambda e: e.activation(out=sig[:, bi, :], in_=PSB[b0][:], func=AF.Sigmoid), reads=[PSk[b0]], writes=[K_sig[bi]])
                                        mp = MP[:, ft, g * 512:(g + 1) * 512]
                                        if n == 0:
                                            C.op("dve", lambda e: e.tensor_tensor(out=mp, in0=sig[:, bi, :], in1=PSB[b1][:], op=ALU.mult),
                                                 reads=[K_sig[bi], PSk[b1]], writes=[MPk[ft][g]])
                                        else:
                                            C.op("dve", lambda e: e.tensor_tensor(out=prod[:, bi, :], in0=sig[:, bi, :], in1=PSB[b1][:], op=ALU.mult),
                                                 reads=[K_sig[bi], PSk[b1]], writes=[K_prod[bi]])
                                            C.op("dve", lambda e: e.tensor_tensor(out=mp, in0=mp, in1=prod[:, bi, :], op=ALU.add),
                                                 reads=[K_prod[bi], MPk[ft][g]], writes=[MPk[ft][g]])
                        C.barrier()
                    with ExitStack() as ph2:
                        wo = sb("wo", [128, 8, D], BF16, ph2)
                        K_wo = Tk()
                        wload(wo[:], K_wo, dr["w_out"][l])
                        tmp = sb("otmp", [128, 2, 512], F32, ph2)
                        K_tmp = tks(2)
                        it = 0
                        for tt in range(NT):
                            for hf in range(2):
                                pb, bi = 4 + it % 4, it % 2
                                it += 1
                                for ft in range(8):
                                    C.op("pe", lambda e: e.matmul(PSB[pb][:], MP[:, ft, tt * 128:(tt + 1) * 128], wo[:, ft, hf * 512:(hf + 1) * 512],
                                                                  start=(ft == 0), stop=(ft == 7)),
                                         reads=[K_wo, MPk[ft][tt // 4]], writes=[PSk[pb]], inc=(ft == 7))
                                C.op("dve", lambda e: e.tensor_tensor(out=tmp[:, bi, :], in0=PSB[pb][:], in1=G12[:, 0, hf * 512:(hf + 1) * 512], op=ALU.mult),
                                     reads=[PSk[pb], K_G[0]], writes=[K_tmp[bi]])
                                C.op("pool", lambda e: e.tensor_tensor(out=X[:, tt, hf * 512:(hf + 1) * 512], in0=X[:, tt, hf * 512:(hf + 1) * 512],
                                                                       in1=tmp[:, bi, :], op=ALU.add),
                                     reads=[K_tmp[bi], XT[tt]], writes=[XT[tt]])
                        C.barrier()
            tap(f"X1_{l}", X[:], XT, [128, NT, D])
            if stop_after == f"mix{l}":
                break

            norm_to_HT(2)
            with ExitStack() as ph:
                comb = sb("comb", [128, NT, NE], F32, ph)
                K_comb = tks(NT, "comb")
                bguT = sb("bguT", [128, NE * 16], F32, ph)
                K_bgu = Tk()
                with ExitStack() as ph2:
                    wr = sb("wr", [128, 8, NE], BF16, ph2)
                    brt = sb("brt", [128, NE], F32, ph2)
                    bd = sb("bd", [NE, D], F32, ph2)
                    K_r = Tk()
                    C.dma("pool", wr[:], dr["w_router"][l].rearrange("(kt p) e -> p kt e", p=128), writes=[K_r])
                    C.dma("sp", brt[:], dr["b_router"][l].partition_broadcast(128), writes=[K_r])
                    C.dma("sp", bd[:], dr["b_down"][l], writes=[K_r])
                    rows = sb("bgrows", [128, 4, 128], F32, ph2)
                    K_rows = Tk()
                    C.dma("sp", rows[:], dr["b_gate_up"][l].rearrange("e (j p) -> (e j) p", p=128).rearrange("(r q) p -> q r p", q=128), writes=[K_rows])
                    for r_ in range(4):
                        C.op("pe", lambda e: e.transpose(PSB[0][:, r_ * 128:(r_ + 1) * 128], rows[:, r_, :], ident_f[:]),
                             reads=[K_rows, K_id], writes=[PSk[0]], inc=(r_ == 3))
                    C.op("dve", lambda e: e.tensor_copy(out=bguT[:], in_=PSB[0][:]), reads=[PSk[0]], writes=[K_bgu])
                    bv = bguT[:].rearrange("p (e j) -> p e j", j=16)
                    C.op("dve", lambda e: e.tensor_scalar(out=bv[:, :, 8:16], in0=bv[:, :, 8:16], scalar1=1.0, scalar2=None, op0=ALU.add),
                         reads=[K_bgu], writes=[K_bgu])
                    lg = sb("lg", [128, 2, NE], F32, ph2)
                    m8r = sb("m8r", [128, 2, 8], F32, ph2)
                    sel = sb("rsel", [128, 2, NE], F32, ph2)
                    sm = sb("rsm", [128, 2, 2], F32, ph2)
                    cT = sb("rcT", [NE, 2, 128], F32, ph2)
                    tmpb = sb("rtmp", [128, 2, 512], F32, ph2)
                    K_lg, K_m8r, K_sel, K_sm, K_cT, K_tmpb = tks(2), tks(2), tks(2), tks(2), tks(2), tks(2)
                    it = 0
                    for tt in range(NT):
                        p = tt % 2
                        for kt in range(8):
                            C.op("pe", lambda e: e.matmul(PSB[1][:, 0:NE], HT[:, kt, tt * 128:(tt + 1) * 128], wr[:, kt, :], start=(kt == 0), stop=(kt == 7)),
                                 reads=[K_r] + HTr(tt // 4), writes=[PSk[1]], inc=(kt == 7))
                        C.op("dve", lambda e: e.tensor_tensor(out=lg[:, p, :], in0=PSB[1][:, 0:NE], in1=brt[:], op=ALU.add), reads=[PSk[1], K_r], writes=[K_lg[p]])
                        C.op("dve", lambda e: e.max(out=m8r[:, p, :], in_=lg[:, p, :]), reads=[K_lg[p]], writes=[K_m8r[p]])
                        C.op("dve", lambda e: e.tensor_scalar(out=sel[:, p, :], in0=lg[:, p, :], scalar1=m8r[:, p, 3:4], scalar2=None, op0=ALU.is_ge),
                             reads=[K_lg[p], K_m8r[p]], writes=[K_sel[p]])
                        C.op("dve", lambda e: e.tensor_scalar(out=sm[:, p, 0:1], in0=m8r[:, p, 0:1], scalar1=-1.0, scalar2=None, op0=ALU.mult),
                             reads=[K_m8r[p]], writes=[K_sm[p]])
                        C.op("act", lambda e: e.activation(out=lg[:, p, :], in_=lg[:, p, :], func=AF.Exp, bias=sm[:, p, 0:1]), reads=[K_lg[p], K_sm[p]], writes=[K_lg[p]])
                        C.op("dve", lambda e: e.tensor_tensor(out=sel[:, p, :], in0=sel[:, p, :], in1=lg[:, p, :], op=ALU.mult), reads=[K_lg[p], K_sel[p]], writes=[K_sel[p]])
                        C.op("dve", lambda e: e.reduce_sum(out=sm[:, p, 1:2], in_=sel[:, p, :], axis=AX.X), reads=[K_sel[p]], writes=[K_sm[p]])
                        C.op("dve", lambda e: e.reciprocal(out=sm[:, p, 1:2], in_=sm[:, p, 1:2]), reads=[K_sm[p]], writes=[K_sm[p]])
                        C.op("dve", lambda e: e.tensor_scalar(out=comb[:, tt, :], in0=sel[:, p, :], scalar1=sm[:, p, 1:2], scalar2=None, op0=ALU.mult),
                             reads=[K_sel[p], K_sm[p]], writes=[K_comb[tt]])
                        C.op("pe", lambda e: e.transpose(PSB[2][0:NE, 0:128], comb[:, tt, :], ident_f[:]), reads=[K_comb[tt], K_id], writes=[PSk[2]])
                        C.op("act", lambda e: e.copy(out=cT[:, p, :], in_=PSB[2][0:NE, 0:128]), reads=[PSk[2]], writes=[K_cT[p]])
                        for hf in range(2):
                            pb, bi = 4 + it % 4, it % 2
                            it += 1
                            C.op("pe", lambda e: e.matmul(PSB[pb][:], cT[:, p, :], bd[:, hf * 512:(hf + 1) * 512], start=True, stop=True),
                                 reads=[K_cT[p], K_r], writes=[PSk[pb]])
                            C.op("dve", lambda e: e.tensor_tensor(out=tmpb[:, bi, :], in0=PSB[pb][:], in1=G12[:, 1, hf * 512:(hf + 1) * 512], op=ALU.mult),
                                 reads=[PSk[pb], K_G[1]], writes=[K_tmpb[bi]])
                            C.op("pool", lambda e: e.tensor_tensor(out=X[:, tt, hf * 512:(hf + 1) * 512], in0=X[:, tt, hf * 512:(hf + 1) * 512],
                                                                   in1=tmpb[:, bi, :], op=ALU.add),
                                 reads=[K_tmpb[bi], XT[tt]], writes=[XT[tt]])
                    C.barrier()
                tap(f"comb{l}", comb[:], K_comb, [128, NT, NE])
                if stop_after == f"router{l}":
                    break
                RING = 4
                ring = sb("ring", [128, RING, 8, 512], BF16, ph)
                K_ring = tks(RING, "ring")
                actT = sb("actT", [128, 8, S], BF16, ph)
                K_act = [tks(4, f"act{f}_") for f in range(8)]
                xg = sb("xg", [128, 2, 512], F32, ph)
                sg = sb("sg", [128, 2, 512], BF16, ph)
                Al = sb("Al", [128, 2, 512], F32, ph)
                tg_ = sb("tg", [128, 2, 512], F32, ph)
                yt_ = sb("ytmp", [128, 2, 512], F32, ph)
                K_xg, K_sg, K_Al, K_tg, K_ytmp = tks(2), tks(2), tks(2), tks(2), tks(2)
                n_exp = NE if stop_after != f"moe1e{l}" else 1
                chunks = []
                for e_ in range(n_exp):
                    wgu = dr["w_gate_up"][l][e_]
                    wdn = dr["w_down"][l][e_]
                    chunks += [wgu[:, 0:512], wgu[:, 1024:1536], wgu[:, 512:1024], wgu[:, 1536:2048], wdn[:, 0:512], wdn[:, 512:1024]]
                loaded = [0]

                def prefetch(upto):
                    while loaded[0] < min(upto, len(chunks)):
                        i = loaded[0]
                        wload(ring[:, i % RING, :, :], K_ring[i % RING], chunks[i])
                        loaded[0] += 1
                prefetch(RING - 1)
                ci_ = 0
                gi = 0
                yi = 0
                for e_ in range(n_exp):
                    for c in range(2):
                        sl_g, sl_l = ci_ % RING, (ci_ + 1) % RING
                        prefetch(ci_ + RING)
                        for g in range(4):
                            for i in range(4):
                                ft = c * 4 + i
                                pg, pl, bi = (gi % 2) * 2, (gi % 2) * 2 + 1, gi % 2
                                gi += 1
                                for kt in range(8):
                                    C.op("pe", lambda e: e.matmul(PSB[pg][:], ring[:, sl_g, kt, i * 128:(i + 1) * 128], HT[:, kt, g * 512:(g + 1) * 512],
                                                                  start=(kt == 0), stop=(kt == 7)),
                                         reads=[K_ring[sl_g]] + HTr(g), writes=[PSk[pg]], inc=(kt == 7))
                                for kt in range(8):
                                    C.op("pe", lambda e: e.matmul(PSB[pl][:], ring[:, sl_l, kt, i * 128:(i + 1) * 128], HT[:, kt, g * 512:(g + 1) * 512],
                                                                  start=(kt == 0), stop=(kt == 7)),
                                         reads=[K_ring[sl_l]] + HTr(g), writes=[PSk[pl]], inc=(kt == 7))
                                cg = e_ * 16 + ft
                                C.op("dve", lambda e: e.tensor_scalar(out=xg[:, bi, :], in0=PSB[pg][:], scalar1=bguT[:, cg:cg + 1], scalar2=7.0, op0=ALU.add, op1=ALU.min),
                                     reads=[PSk[pg], K_bgu], writes=[K_xg[bi]])
                                C.op("act", lambda e: e.activation(out=sg[:, bi, :], in_=xg[:, bi, :], func=AF.Sigmoid, scale=1.702), reads=[K_xg[bi]], writes=[K_sg[bi]])
                                C.op("dve", lambda e: e.tensor_scalar(out=Al[:, bi, :], in0=PSB[pl][:], scalar1=bguT[:, cg + 8:cg + 9], scalar2=8.0, op0=ALU.add, op1=ALU.min),
                                     reads=[PSk[pl], K_bgu], writes=[K_Al[bi]])
                                C.op("dve", lambda e: e.tensor_tensor(out=tg_[:, bi, :], in0=xg[:, bi, :], in1=sg[:, bi, :], op=ALU.mult),
                                     reads=[K_xg[bi], K_sg[bi]], writes=[K_tg[bi]])
                                C.op("dve", lambda e: e.scalar_tensor_tensor(out=actT[:, ft, g * 512:(g + 1) * 512], in0=Al[:, bi, :], scalar=-6.0, in1=tg_[:, bi, :],
                                                                             op0=ALU.max, op1=ALU.mult),
                                     reads=[K_Al[bi], K_tg[bi]], writes=[K_act[ft][g]])
                        ci_ += 2
                    for hf in range(2):
                        sl_d = ci_ % RING
                        prefetch(ci_ + RING)
                        for tt in range(NT):
                            pb, bi = 4 + yi % 4, yi % 2
                            yi += 1
                            for ft in range(8):
                                C.op("pe", lambda e: e.matmul(PSB[pb][:], actT[:, ft, tt * 128:(tt + 1) * 128], ring[:, sl_d, ft, :], start=(ft == 0), stop=(ft == 7)),
                                     reads=[K_ring[sl_d], K_act[ft][tt // 4]], writes=[PSk[pb]], inc=(ft == 7))
                            C.op("act", lambda e: e.activation(out=yt_[:, bi, :], in_=PSB[pb][:], func=AF.Identity, scale=comb[:, tt, e_:e_ + 1]),
                                 reads=[PSk[pb], K_comb[tt]], writes=[K_ytmp[bi]])
                            C.op("pool", lambda e: e.tensor_tensor(out=yt_[:, bi, :], in0=yt_[:, bi, :], in1=G12[:, 1, hf * 512:(hf + 1) * 512], op=ALU.mult),
                                 reads=[K_ytmp[bi], K_G[1]], writes=[K_ytmp[bi]])
                            C.op("pool", lambda e: e.tensor_tensor(out=X[:, tt, hf * 512:(hf + 1) * 512], in0=X[:, tt, hf * 512:(hf + 1) * 512],
                                                                   in1=yt_[:, bi, :], op=ALU.add),
                                 reads=[K_ytmp[bi], XT[tt]], writes=[XT[tt]])
                        ci_ += 1
                C.barrier()
            tap(f"X2_{l}", X[:], XT, [128, NT, D])
            if stop_after == f"moe{l}" or stop_after == f"moe1e{l}":
                break

        if stop_after is None:
            with ExitStack() as ph:
                gf = sb("gf", [128, D], F32, ph)
                K_gf = Tk()
                C.dma("sp", gf[:], dr["g_final"][0].partition_broadcast(128), writes=[K_gf])
                ot = sb("ot", [128, 2, D], F32, ph)
                K_ot = tks(2)
                ov_ = out_d.rearrange("(t p) d -> p t d", p=128)
                for tt in range(NT):
                    p = tt % 2
                    g = tt // 4
                    C.op("act", lambda e: e.activation(out=junk[:, p, :], in_=X[:, tt, :], func=AF.Square, accum_out=ss[:, tt:tt + 1]),
                         reads=[XT[tt]], writes=[K_junk[p], K_ss[g]])
                    C.op("dve", lambda e: e.tensor_scalar(out=rstd[:, tt:tt + 1], in0=ss[:, tt:tt + 1], scalar1=1.0 / D, scalar2=EPS, op0=ALU.mult, op1=ALU.add),
                         reads=[K_ss[g]], writes=[K_rstd[g]])
                    C.op("act", lambda e: e.activation(out=rstd[:, tt:tt + 1], in_=rstd[:, tt:tt + 1], func=AF.Sqrt), reads=[K_rstd[g]], writes=[K_rstd[g]])
                    C.op("dve", lambda e: e.reciprocal(out=rstd[:, tt:tt + 1], in_=rstd[:, tt:tt + 1]), reads=[K_rstd[g]], writes=[K_rstd[g]])
                    C.op("dve", lambda e: e.scalar_tensor_tensor(out=ot[:, p, :], in0=X[:, tt, :], scalar=rstd[:, tt:tt + 1], in1=gf[:], op0=ALU.mult, op1=ALU.mult),
                         reads=[XT[tt], K_rstd[g], K_gf], writes=[K_ot[p]])
                    C.dma("sp", ov_[:, tt, :], ot[:, p, :], reads=[K_ot[p]])
        C.barrier()
    return nc, list(tap_d.keys())


def kernel(**inputs):
    n = 8
    nc, _ = build()
    consts = make_consts()
    shared = {}
    for k in IN_SPECS:
        if k in ("x", "c"):
            continue
        a = np.ascontiguousarray(np.asarray(inputs[k], dtype=np.float32))
        if k == "g_final":
            a = a.reshape(1, D)
        shared[k] = a
    for k, v in consts.items():
        shared["k_" + k] = v
    x = np.asarray(inputs["x"], dtype=np.float32)
    c = np.asarray(inputs["c"], dtype=np.float32)
    in_maps = []
    for b in range(n):
        m = dict(shared)
        m["x"] = np.ascontiguousarray(x[b])
        m["c"] = np.ascontiguousarray(c[b:b + 1])
        in_maps.append(m)
    res = run_bass_kernel_spmd(nc, in_maps, core_ids=list(range(n)))
    return np.stack([np.asarray(r["out"], dtype=np.float32) for r in res.results], axis=0)
```

```python
import numpy as np
import ml_dtypes
from contextlib import ExitStack
import concourse.bass as bass
import concourse.mybir as mybir
from concourse.bass_utils import run_bass_kernel_spmd

F32 = mybir.dt.float32
BF16 = mybir.dt.bfloat16
AF = mybir.ActivationFunctionType
ALU = mybir.AluOpType
AX = mybir.AxisListType

D = 1024
S = 2048
NT = 16
DEPTH = 2
NE = 32
EPS = 1e-5
D_IN = 7184
O_MQ, O_MK, O_MV = 0, 256, 512
O_GQ, O_GK, O_GV, O_GA, O_GR = 768, 896, 1024, 1280, 1296
O_RQ, O_RK, O_RV, O_RG = 1552, 1808, 2064, 2320
O_SQ, O_SK, O_SV = 2576, 2832, 2960
O_MG = 3088
NEG = -30000.0
SKIP = {"ret"}
RETCUT = 99
RETTILES = 16
RETV = 0


class Tk:
    __slots__ = ("w", "r", "name")

    def __init__(self, name=""):
        self.w = None
        self.r = {}
        self.name = name


def tks(n, name=""):
    return [Tk(f"{name}{i}") for i in range(n)]


class Ctx:
    ENG = ("pe", "act", "dve", "pool", "sp")

    def __init__(self, nc, es, n_dsem=24):
        self.nc = nc
        self.E = {"pe": nc.tensor, "act": nc.scalar, "dve": nc.vector, "pool": nc.gpsimd, "sp": nc.sync}
        self.sem = {e: es.enter_context(nc.semaphore("s_" + e)) for e in self.ENG}
        self.cnt = {e: 0 for e in self.ENG}
        self.seen = {e: {} for e in self.ENG}
        self.dsem = [es.enter_context(nc.semaphore(f"s_d{i}")) for i in range(n_dsem)]
        self.dcnt = [0] * n_dsem
        half = n_dsem // 2
        self.dpool = {"sp": list(range(half)), "pool": list(range(half, n_dsem))}
        self.dnext = {"sp": 0, "pool": 0}
        self.nins = 0

    def _need(self, eng, reads, writes):
        need = {}

        def add(key, val):
            if need.get(key, 0) < val:
                need[key] = val

        for t in reads:
            if t.w is not None:
                for k, v in t.w.items():
                    add(k, v)
        for t in writes:
            if t.w is not None:
                for k, v in t.w.items():
                    add(k, v)
            for k, v in t.r.items():
                add(k, v)
        for key, val in need.items():
            if key == ("e", "pe") and eng == "pe":
                continue
            if self.seen[eng].get(key, 0) >= val:
                continue
            sem = self.sem[key[1]] if key[0] == "e" else self.dsem[key[1]]
            self.E[eng].wait_ge(sem, val)
            self.seen[eng][key] = val
            self.nins += 1

    def _mark(self, key, val, reads, writes):
        for t in reads:
            if t.r.get(key, 0) < val:
                t.r[key] = val
        for t in writes:
            if key[0] == "d" and t.w is not None:
                t.w[key] = val
            else:
                t.w = {key: val}
            t.r = {}

    def op(self, eng, fn, reads=(), writes=(), inc=True):
        self._need(eng, reads, writes)
        ins = fn(self.E[eng])
        self.nins += 1
        if inc:
            ins.then_inc(self.sem[eng], 1)
            self.cnt[eng] += 1
            val = self.cnt[eng]
        else:
            assert eng == "pe"
            val = self.cnt[eng] + 1
        self._mark(("e", eng), val, reads, writes)
        return ins

    def dma(self, q, out, in_, reads=(), writes=(), **kw):
        self._need(q, reads, writes)
        i = self.dpool[q][self.dnext[q]]
        self.dnext[q] = (self.dnext[q] + 1) % len(self.dpool[q])
        key = ("d", i)
        if self.dcnt[i] > 0 and self.seen[q].get(key, 0) < self.dcnt[i] * 16:
            self.E[q].wait_ge(self.dsem[i], self.dcnt[i] * 16)
            self.seen[q][key] = self.dcnt[i] * 16
        ins = self.E[q].dma_start(out=out, in_=in_, **kw)
        ins.then_inc(self.dsem[i], 16)
        self.nins += 1
        self.dcnt[i] += 1
        self._mark(key, self.dcnt[i] * 16, reads, writes)
        return ins

    def barrier(self):
        for i in range(len(self.dsem)):
            if self.dcnt[i] > 0 and self.seen["sp"].get(("d", i), 0) < self.dcnt[i] * 16:
                self.E["sp"].wait_ge(self.dsem[i], self.dcnt[i] * 16)
                self.seen["sp"][("d", i)] = self.dcnt[i] * 16
        ins = self.E["sp"].nop() if False else None
        for e in self.ENG:
            for f in self.ENG:
                if e == f:
                    continue
                key = ("e", f)
                if self.cnt[f] > 0 and self.seen[e].get(key, 0) < self.cnt[f]:
                    self.E[e].wait_ge(self.sem[f], self.cnt[f])
                    self.seen[e][key] = self.cnt[f]
            for i in range(len(self.dsem)):
                if self.dcnt[i] > 0 and self.seen[e].get(("d", i), 0) < self.dcnt[i] * 16:
                    self.E[e].wait_ge(self.dsem[i], self.dcnt[i] * 16)
                    self.seen[e][("d", i)] = self.dcnt[i] * 16


def make_consts():
    bf = ml_dtypes.bfloat16
    c = {}
    c["ident_bf"] = np.eye(128, dtype=np.float32).astype(bf)
    c["ident_f"] = np.eye(128, dtype=np.float32)
    k = np.arange(128)[:, None]
    q = np.arange(128)[None, :]
    c["tri_bf"] = (k <= q).astype(np.float32).astype(bf)
    slopes = 2.0 ** (-(np.arange(8, dtype=np.float64) + 1.0))
    swa = np.zeros((128, 4, 2, 128), np.float64)
    for h in range(4):
        sl = slopes[h]
        dist_prev = 128 + q - k
        swa[:, h, 0, :] = np.where(k > q, np.exp(-sl * dist_prev), 0.0)
        dist_own = q - k
        swa[:, h, 1, :] = np.where(k <= q, np.exp(-sl * dist_own), 0.0)
    c["swa_mask"] = swa.astype(np.float32).astype(bf)
    t = np.arange(S)
    a = (t // 128).astype(np.float64)
    r = (t % 128).astype(np.float64)
    qa = np.zeros((4, 12, S), np.float64)
    ka = np.zeros((4, 12, S), np.float64)
    for h in range(4):
        sl = slopes[4 + h] * 8.0
        for j in range(8):
            ka[h, j] = (t // 256 == j)
        qa[h, 8] = -sl * 128.0 * a
        ka[h, 8] = 1.0
        qa[h, 9] = -sl * r
        ka[h, 9] = 1.0
        qa[h, 10] = 1.0
        ka[h, 10] = sl * 128.0 * a
        qa[h, 11] = 1.0
        ka[h, 11] = sl * r
    c["moba_qa"] = qa.astype(np.float32).astype(bf)
    c["moba_ka"] = ka.astype(np.float32).astype(bf)
    m = np.arange(128)[:, None]
    l = np.arange(128)[None, :]
    c["cum_incl"] = ((m <= l) * (-1.0 / 16.0)).astype(np.float32)
    c["cum_after"] = ((m > l) * (-1.0 / 16.0)).astype(np.float32)
    gam = 1.0 - 2.0 ** (-5.0 - np.arange(4, dtype=np.float64))
    pos = np.arange(128, dtype=np.float64)
    rq = np.zeros((128, 2, 128)); rk = np.zeros((128, 2, 128))
    rke = np.zeros((128, 256)); rdec = np.zeros((128, 2))
    for h in range(4):
        j, o = h // 2, (h % 2) * 64
        rq[o:o + 64, j, :] = gam[h] ** (pos + 1.0)
        rk[o:o + 64, j, :] = gam[h] ** (-(pos + 1.0)) * 64 ** -0.5
        rke[:, h * 64:(h + 1) * 64] = (gam[h] ** (127.0 - pos))[:, None] * 64 ** -0.5
        rdec[o:o + 64, j] = gam[h] ** 128.0
    tt_ = np.arange(16)[:, None]; nn_ = np.arange(8)[None, :]
    c["moba_past"] = np.where(nn_ < tt_ // 2, 0.0, -1e30).astype(np.float32).reshape(1, 128)
    c["moba_notown"] = np.where(nn_ == tt_ // 2, 0.0, 1.0).astype(np.float32).reshape(1, 128)
    bd = np.zeros((128, 256), np.float32)
    for h in range(4):
        bd[32 * h:32 * h + 32, 64 * h:64 * h + 64] = 1.0
    c["bd_gla"] = bd
    bd = np.zeros((128, 128), np.float32)
    for h in range(2):
        bd[64 * h:64 * h + 64, 64 * h:64 * h + 64] = 1.0
    c["bd_ret"] = bd
    tpos = np.arange(S, dtype=np.float64) - 1024.0
    c["ret_qd"] = np.stack([gam[h] ** tpos for h in range(4)]).astype(np.float32)
    c["ret_kd"] = np.stack([gam[h] ** (-tpos) for h in range(4)]).astype(np.float32)
    c["ret_q"] = rq.astype(np.float32)
    c["ret_k"] = rk.astype(np.float32)
    c["ret_kend"] = rke.astype(np.float32)
    c["ret_dec"] = rdec.astype(np.float32)
    return c


CONST_SPECS = {
    "ident_bf": ([128, 128], BF16), "ident_f": ([128, 128], F32), "tri_bf": ([128, 128], BF16),
    "swa_mask": ([128, 4, 2, 128], BF16), "moba_qa": ([4, 12, S], BF16), "moba_ka": ([4, 12, S], BF16),
    "cum_incl": ([128, 128], F32), "cum_after": ([128, 128], F32),
    "ret_q": ([128, 2, 128], F32), "ret_k": ([128, 2, 128], F32), "ret_kend": ([128, 256], F32),
    "ret_dec": ([128, 2], F32), "moba_past": ([1, 128], F32), "ret_qd": ([4, S], F32), "ret_kd": ([4, S], F32), "bd_gla": ([128, 256], F32), "bd_ret": ([128, 128], F32), "moba_notown": ([1, 128], F32),
}

IN_SPECS = {
    "x": [S, D], "c": [1, D], "w_ada": [DEPTH, D, 6 * D], "b_ada": [DEPTH, 6 * D], "g_norm_mix": [DEPTH, D],
    "w_in": [DEPTH, D, D_IN], "w_gla_gate": [DEPTH, 16, 128], "b_gla_gate": [DEPTH, 128],
    "g_gla_norm": [DEPTH, 256], "g_ret_norm": [DEPTH, 256], "attn_sinks": [DEPTH, 4],
    "w_branch": [DEPTH, 4, 256, D], "w_out": [DEPTH, D, D], "g_norm_ffn": [DEPTH, D],
    "w_router": [DEPTH, D, NE], "b_router": [DEPTH, NE], "w_gate_up": [DEPTH, NE, D, 2 * D],
    "b_gate_up": [DEPTH, NE, 2 * D], "w_down": [DEPTH, NE, D, D], "b_down": [DEPTH, NE, D],
    "g_final": [1, D],
}


def build(n_layers=DEPTH, stop_after=None, taps=()):
    nc = bass.Bass("TRN2", target_bir_lowering=False)
    small = stop_after is not None and not stop_after.startswith(("moe", "router"))
    dr = {k: nc.dram_tensor(k, shp, F32, kind="ExternalInput").ap() for k, shp in IN_SPECS.items()
          if not (small and k in ("w_gate_up", "w_down"))}
    cst = {k: nc.dram_tensor("k_" + k, shp, dt, kind="ExternalInput").ap() for k, (shp, dt) in CONST_SPECS.items()}
    out_d = nc.dram_tensor("out", [S, D], F32, kind="ExternalOutput").ap()
    tap_d = {}
    es = ExitStack()
    with es:
        C = Ctx(nc, es)

        uniq = [0]

        def sb(name, shape, dt, stack=es):
            uniq[0] += 1
            return stack.enter_context(nc.sbuf_tensor(f"{name}_{uniq[0]}", shape, dt))

        X = sb("X", [128, NT, D], F32)
        XT = tks(NT, "X")
        HT = sb("HT", [128, 8, S], BF16)
        HTk = [[Tk(f"HT{f}_{g}") for g in range(4)] for f in range(8)]
        ident_bf = sb("ident_bf", [128, 128], BF16)
        ident_f = sb("ident_f", [128, 128], F32)
        K_id = Tk("ident")
        PSB = [es.enter_context(nc.psum_tensor(f"ps{i}", [128, 512], F32)) for i in range(8)]
        PSk = tks(8, "ps")
        colsT = sb("colsT", [128, 64], F32)
        K_cols = Tk("colsT")
        modT = sb("modT", [128, 48], F32)
        K_mod = Tk("modT")
        AB = sb("AB", [128, 4, 8], F32)
        K_AB = Tk("AB")
        G12 = sb("G12", [128, 2, D], F32)
        K_G = tks(2, "G")
        cactT = sb("cactT", [128, 8], BF16)
        cactB = sb("cactB", [128, 8, 128], BF16)
        K_cact = Tk("cact")
        ss = sb("ss", [128, NT], F32)
        rstd = sb("rstd", [128, NT], F32)
        K_ss = tks(4, "ss")
        K_rstd = tks(4, "rstd")
        junk = sb("junk", [128, 2, D], BF16)
        K_junk = tks(2, "junk")

        def tap(name, ap, reads, shape, dt=F32):
            if name not in taps:
                return
            d = nc.dram_tensor("tap_" + name, list(shape), dt, kind="ExternalOutput").ap()
            tap_d[name] = d
            C.dma("sp", d, ap, reads=reads)

        def wload(dst, dst_tk, src2d, q="pool"):
            n = src2d.shape[1]
            sv = src2d.rearrange("(kt p) c -> p kt c", p=128)
            for c0 in range(0, n, 512):
                c1 = min(n, c0 + 512)
                C.dma(q, dst[:, :, c0:c1], sv[:, :, c0:c1], writes=[dst_tk])

        def ps_bf(i):
            return PSB[i][:].bitcast(BF16)

        C.dma("sp", ident_bf[:], cst["ident_bf"], writes=[K_id])
        C.dma("sp", ident_f[:], cst["ident_f"], writes=[K_id])
        xv = dr["x"].rearrange("(t p) d -> p t d", p=128)
        for g in range(4):
            C.dma("sp", X[:, 4 * g:4 * g + 4, :], xv[:, 4 * g:4 * g + 4, :], writes=XT[4 * g:4 * g + 4])

        with ExitStack() as ph:
            crow = sb("crow", [8, 128], F32, ph)
            K_crow = Tk()
            ccol = sb("ccol", [128, 8], F32, ph)
            K_ccol = Tk()
            C.dma("sp", crow[:], dr["c"].rearrange("o (kt p) -> (o kt) p", p=128), writes=[K_crow])
            C.op("pe", lambda e: e.transpose(PSB[0][:, 0:8], crow[:], ident_f[0:8, 0:8]),
                 reads=[K_crow, K_id], writes=[PSk[0]])
            C.op("act", lambda e: e.activation(out=ccol[:], in_=PSB[0][:, 0:8], func=AF.Silu),
                 reads=[PSk[0]], writes=[K_ccol])
            C.op("dve", lambda e: e.tensor_copy(out=cactT[:], in_=ccol[:]), reads=[K_ccol], writes=[K_cact])
            C.op("dve", lambda e: e.tensor_copy(out=cactB[:], in_=ccol[:].unsqueeze(2).to_broadcast([128, 8, 128])),
                 reads=[K_ccol], writes=[K_cact])
            C.barrier()

        def adaln(l):
            with ExitStack() as ph:
                rows = sb("rows", [64, 128], F32, ph)
                K_rows = Tk()
                C.dma("sp", rows[0:48, :], dr["b_ada"][l].rearrange("(r p) -> r p", p=128), writes=[K_rows])
                C.dma("sp", rows[48:56, :], dr["g_norm_mix"][l].rearrange("(r p) -> r p", p=128), writes=[K_rows])
                C.dma("sp", rows[56:64, :], dr["g_norm_ffn"][l].rearrange("(r p) -> r p", p=128), writes=[K_rows])
                C.op("pe", lambda e: e.transpose(PSB[0][:, 0:64], rows[:], ident_f[0:64, 0:64]),
                     reads=[K_rows, K_id], writes=[PSk[0]])
                C.op("dve", lambda e: e.tensor_copy(out=colsT[:], in_=PSB[0][:, 0:64]), reads=[PSk[0]], writes=[K_cols])
                wa = [sb(f"wa{i}", [128, 8, 512], BF16, ph) for i in range(2)]
                K_wa = tks(2, "wa")
                bbc = sb("bbc", [128, D], F32, ph)
                K_bbc = Tk()
                ci = 0
                for j in range(6):
                    for half in range(2):
                        w = ci % 2
                        ci += 1
                        c0 = j * D + half * 512
                        wload(wa[w][:], K_wa[w], dr["w_ada"][l][:, c0:c0 + 512])
                        if j in (2, 5):
                            gi = 0 if j == 2 else 1
                            pb = 2 + half
                            for kt in range(8):
                                C.op("pe", lambda e: e.matmul(PSB[pb][:], cactB[:, kt, :], wa[w][:, kt, :],
                                                              start=(kt == 0), stop=(kt == 7)),
                                     reads=[K_cact, K_wa[w]], writes=[PSk[pb]], inc=(kt == 7))
                            if half == 0:
                                C.dma("sp", bbc[:], dr["b_ada"][l][j * D:(j + 1) * D].partition_broadcast(128),
                                      writes=[K_bbc])
                            C.op("dve", lambda e: e.tensor_tensor(out=G12[:, gi, half * 512:(half + 1) * 512],
                                                                  in0=PSB[pb][:], in1=bbc[:, half * 512:(half + 1) * 512],
                                                                  op=ALU.add),
                                 reads=[PSk[pb], K_bbc], writes=[K_G[gi]])
                        else:
                            for fl in range(4):
                                col = j * 8 + half * 4 + fl
                                for kt in range(8):
                                    C.op("pe", lambda e: e.matmul(PSB[1][:, col:col + 1], wa[w][:, kt, fl * 128:(fl + 1) * 128],
                                                                  cactT[:, kt:kt + 1], start=(kt == 0), stop=(kt == 7)),
                                         reads=[K_cact, K_wa[w]], writes=[PSk[1]], inc=(kt == 7))
                for j in (0, 1, 3, 4):
                    C.op("dve", lambda e: e.tensor_tensor(out=modT[:, j * 8:(j + 1) * 8], in0=PSB[1][:, j * 8:(j + 1) * 8],
                                                          in1=colsT[:, j * 8:(j + 1) * 8], op=ALU.add),
                         reads=[PSk[1], K_cols], writes=[K_mod])
                C.op("dve", lambda e: e.scalar_tensor_tensor(out=AB[:, 0, :], in0=modT[:, 8:16], scalar=1.0,
                                                             in1=colsT[:, 48:56], op0=ALU.add, op1=ALU.mult),
                     reads=[K_mod, K_cols], writes=[K_AB])
                C.op("dve", lambda e: e.tensor_copy(out=AB[:, 1, :], in_=modT[:, 0:8]), reads=[K_mod], writes=[K_AB])
                C.op("dve", lambda e: e.scalar_tensor_tensor(out=AB[:, 2, :], in0=modT[:, 32:40], scalar=1.0,
                                                             in1=colsT[:, 56:64], op0=ALU.add, op1=ALU.mult),
                     reads=[K_mod, K_cols], writes=[K_AB])
                C.op("dve", lambda e: e.tensor_copy(out=AB[:, 3, :], in_=modT[:, 24:32]), reads=[K_mod], writes=[K_AB])
                C.barrier()

        def norm_to_HT(ai):
            with ExitStack() as ph:
                xn = sb("xn", [128, 4, D], BF16, ph)
                K_xn = tks(4, "xn")
                ev = 0
                for g in range(4):
                    for i in range(4):
                        tt = 4 * g + i
                        C.op("act", lambda e: e.activation(out=junk[:, i % 2, :], in_=X[:, tt, :], func=AF.Square,
                                                           accum_out=ss[:, tt:tt + 1]),
                             reads=[XT[tt]], writes=[K_junk[i % 2], K_ss[g]])
                    C.op("dve", lambda e: e.tensor_scalar(out=rstd[:, 4 * g:4 * g + 4], in0=ss[:, 4 * g:4 * g + 4],
                                                          scalar1=1.0 / D, scalar2=EPS, op0=ALU.mult, op1=ALU.add),
                         reads=[K_ss[g]], writes=[K_rstd[g]])
                    C.op("act", lambda e: e.activation(out=rstd[:, 4 * g:4 * g + 4], in_=rstd[:, 4 * g:4 * g + 4], func=AF.Sqrt),
                         reads=[K_rstd[g]], writes=[K_rstd[g]])
                    C.op("dve", lambda e: e.reciprocal(out=rstd[:, 4 * g:4 * g + 4], in_=rstd[:, 4 * g:4 * g + 4]),
                         reads=[K_rstd[g]], writes=[K_rstd[g]])
                    for i in range(4):
                        tt = 4 * g + i
                        C.op("dve", lambda e: e.tensor_scalar(out=xn[:, i, :], in0=X[:, tt, :], scalar1=rstd[:, tt:tt + 1],
                                                              scalar2=None, op0=ALU.mult),
                             reads=[XT[tt], K_rstd[g]], writes=[K_xn[i]])
                    for ft in range(8):
                        pb = ft % 4
                        for i in range(4):
                            C.op("pe", lambda e: e.transpose(ps_bf(pb)[:, i * 128:(i + 1) * 128],
                                                             xn[:, i, ft * 128:(ft + 1) * 128], ident_bf[:]),
                                 reads=[K_xn[i], K_id], writes=[PSk[pb]], inc=(i == 3))
                        dst = HT[:, ft, g * 512:(g + 1) * 512]
                        if ev % 2 == 0:
                            C.op("act", lambda e: e.activation(out=dst, in_=ps_bf(pb)[:, 0:512], func=AF.Identity,
                                                               bias=AB[:, ai + 1, ft:ft + 1], scale=AB[:, ai, ft:ft + 1]),
                                 reads=[PSk[pb], K_AB], writes=[HTk[ft][g]])
                        else:
                            C.op("dve", lambda e: e.tensor_scalar(out=dst, in0=ps_bf(pb)[:, 0:512],
                                                                  scalar1=AB[:, ai, ft:ft + 1], scalar2=AB[:, ai + 1, ft:ft + 1],
                                                                  op0=ALU.mult, op1=ALU.add),
                                 reads=[PSk[pb], K_AB], writes=[HTk[ft][g]])
                        ev += 1
                C.barrier()

        for l in range(n_layers):
            adaln(l)
            tap(f"modT{l}", modT[:], [K_mod], [128, 48])
            tap(f"G{l}", G12[:], K_G, [128, 2, D])
            norm_to_HT(0)
            tap(f"HT{l}", HT[:], [t for r in HTk for t in r], [128, 8, S], BF16)
            if stop_after == f"norm{l}":
                break

            with ExitStack() as mx:
                YT = sb("YT", [128, 8, S], BF16, mx)
                YTk = [tks(NT, f"YT{c}_") for c in range(8)]
                tri = sb("tri", [128, 128], BF16, mx)
                K_tri = Tk()
                C.dma("sp", tri[:], cst["tri_bf"], writes=[K_tri])
                ytile = sb("ytile", [128, 2, 256], BF16, mx)
                K_yt = tks(2, "yt")
                WIN = dr["w_in"][l]

                def emit_y(n, tt, par):
                    pb = 7
                    for ci in range(2):
                        C.op("pe", lambda e: e.transpose(ps_bf(pb)[:, ci * 128:(ci + 1) * 128], ytile[:, par, ci * 128:(ci + 1) * 128], ident_bf[:]),
                             reads=[K_yt[par], K_id], writes=[PSk[pb]], inc=(ci == 1))
                    C.op("act", lambda e: e.copy(out=YT[:, 2 * n:2 * n + 2, tt * 128:(tt + 1) * 128],
                                                 in_=ps_bf(pb)[:, 0:256].rearrange("p (c x) -> p c x", c=2)),
                         reads=[PSk[pb]], writes=[YTk[2 * n][tt], YTk[2 * n + 1][tt]])

                def HTr(g):
                    return [HTk[f][g] for f in range(8)]

                def proj_fm(dst_fn, w, K_w, c0, M, evi=[0]):
                    for g in range(4):
                        pb = evi[0] % 2
                        for kt in range(8):
                            C.op("pe", lambda e: e.matmul(PSB[pb][0:M, :], w[:, kt, c0:c0 + M], HT[:, kt, g * 512:(g + 1) * 512],
                                                          start=(kt == 0), stop=(kt == 7)),
                                 reads=[K_w] + HTr(g), writes=[PSk[pb]], inc=(kt == 7))
                        dst, K_dst = dst_fn(g)
                        if evi[0] % 2 == 0:
                            C.op("act", lambda e: e.copy(out=dst, in_=PSB[pb][0:M, :]), reads=[PSk[pb]], writes=[K_dst])
                        else:
                            C.op("dve", lambda e: e.tensor_copy(out=dst, in_=PSB[pb][0:M, :]), reads=[PSk[pb]], writes=[K_dst])
                        evi[0] += 1

                def proj_tm(dst_fn, w, K_w, c0, N, evi=[0]):
                    for tt in range(NT):
                        pb = 2 + evi[0] % 2
                        evi[0] += 1
                        for kt in range(8):
                            C.op("pe", lambda e: e.matmul(PSB[pb][:, 0:N], HT[:, kt, tt * 128:(tt + 1) * 128], w[:, kt, c0:c0 + N],
                                                          start=(kt == 0), stop=(kt == 7)),
                                 reads=[K_w] + HTr(tt // 4), writes=[PSk[pb]], inc=(kt == 7))
                        dst_fn(tt, pb)

                if "swa" not in SKIP:
                    with ExitStack() as ph:
                        QS = [sb(f"QS{h}", [64, S], BF16, ph) for h in range(4)]
                        K_QS = [tks(4, f"QS{h}_") for h in range(4)]
                        KS = [sb(f"KS{g}", [64, S], BF16, ph) for g in range(2)]
                        K_KS = [tks(4, f"KS{g}_") for g in range(2)]
                        VS = sb("VS", [128, NT, 2, 65], BF16, ph)
                        K_VS = tks(NT, "VS")
                        wq = sb("swq", [128, 8, 256], BF16, ph)
                        wkv = sb("swkv", [128, 8, 256], BF16, ph)
                        K_wq, K_wkv = Tk(), Tk()
                        msk = sb("smask", [128, 4, 2, 128], BF16, ph)
                        K_msk = Tk()
                        esink = sb("esink", [128, 4], F32, ph)
                        K_es = Tk()
                        wload(wq[:], K_wq, WIN[:, O_SQ:O_SQ + 256])
                        wload(wkv[:], K_wkv, WIN[:, O_SK:O_SK + 256])
                        C.dma("sp", msk[:], cst["swa_mask"], writes=[K_msk])
                        C.dma("sp", esink[:], dr["attn_sinks"][l].partition_broadcast(128), writes=[K_es])
                        C.op("act", lambda e: e.activation(out=esink[:], in_=esink[:], func=AF.Exp), reads=[K_es], writes=[K_es])
                        C.op("dve", lambda e: e.memset(VS[:, :, :, 64:65], 1.0), writes=K_VS)
                        for h in range(4):
                            proj_fm(lambda g: (QS[h][:, g * 512:(g + 1) * 512], K_QS[h][g]), wq, K_wq, h * 64, 64)
                        for g2 in range(2):
                            proj_fm(lambda g: (KS[g2][:, g * 512:(g + 1) * 512], K_KS[g2][g]), wkv, K_wkv, g2 * 64, 64)

                        def v_ev(tt, pb):
                            C.op("act", lambda e: e.copy(out=VS[:, tt, :, 0:64], in_=PSB[pb][:, 0:128].rearrange("p (g d) -> p g d", g=2)),
                                 reads=[PSk[pb]], writes=[K_VS[tt]])
                        proj_tm(v_ev, wkv, K_wkv, 128, 128)
                        EX = sb("sEX", [128, 2, 512], BF16, ph)
                        PT = sb("sPT", [128, 2, 512], BF16, ph)
                        K_EX, K_PT = tks(2), tks(2)
                        den = sb("sden", [128, 2, 4], F32, ph)
                        K_den = tks(2)
                        it = 0
                        for tt in range(NT):
                            par = tt % 2
                            po = 4 + par
                            for g2 in range(2):
                                pb = it % 2
                                bi = it % 2
                                it += 1
                                pvs = (1,) if tt == 0 else (0, 1)
                                for hh in range(2):
                                    for pv in pvs:
                                        kt = tt - 1 + pv
                                        o = (hh * 2 + pv) * 128
                                        C.op("pe", lambda e: e.matmul(PSB[pb][:, o:o + 128], KS[g2][:, kt * 128:(kt + 1) * 128],
                                                                      QS[2 * g2 + hh][:, tt * 128:(tt + 1) * 128], start=True, stop=True),
                                             reads=[K_KS[g2][kt // 4], K_QS[2 * g2 + hh][tt // 4]], writes=[PSk[pb]],
                                             inc=(hh == 1 and pv == 1))
                                if tt == 0:
                                    exv = EX[:, bi, :].rearrange("p (a b c) -> p a b c", a=2, b=2)[:, :, 1, :]
                                    psv = PSB[pb][:].rearrange("p (a b c) -> p a b c", a=2, b=2)[:, :, 1, :]
                                    ptv = PT[:, bi, :].rearrange("p (a b c) -> p a b c", a=2, b=2)[:, :, 1, :]
                                    C.op("act", lambda e: e.activation(out=exv, in_=psv, func=AF.Exp, scale=0.125),
                                         reads=[PSk[pb]], writes=[K_EX[bi]])
                                    C.op("dve", lambda e: e.tensor_tensor(out=ptv, in0=exv, in1=msk[:, 2 * g2:2 * g2 + 2, 1, :], op=ALU.mult),
                                         reads=[K_EX[bi], K_msk], writes=[K_PT[bi]])
                                else:
                                    C.op("act", lambda e: e.activation(out=EX[:, bi, :], in_=PSB[pb][:], func=AF.Exp, scale=0.125),
                                         reads=[PSk[pb]], writes=[K_EX[bi]])
                                    C.op("dve", lambda e: e.tensor_tensor(out=PT[:, bi, :], in0=EX[:, bi, :],
                                                                          in1=msk[:, 2 * g2:2 * g2 + 2, :, :].rearrange("p a b c -> p (a b c)"),
                                                                          op=ALU.mult),
                                         reads=[K_EX[bi], K_msk], writes=[K_PT[bi]])
                                for hh in range(2):
                                    h = 2 * g2 + hh
                                    for pv in pvs:
                                        kt = tt - 1 + pv
                                        o = (hh * 2 + pv) * 128
                                        C.op("pe", lambda e: e.matmul(PSB[po][:, h * 65:(h + 1) * 65], PT[:, bi, o:o + 128], VS[:, kt, g2, :],
                                                                      start=(pv == pvs[0]), stop=(pv == 1)),
                                             reads=[K_PT[bi], K_VS[kt]], writes=[PSk[po]], inc=(pv == 1))
                            pov = PSB[po][:, 0:260].rearrange("p (h d) -> p h d", h=4)
                            C.op("dve", lambda e: e.tensor_tensor(out=den[:, par, :], in0=pov[:, :, 64], in1=esink[:], op=ALU.add),
                                 reads=[PSk[po], K_es], writes=[K_den[par]])
                            C.op("dve", lambda e: e.reciprocal(out=den[:, par, :], in_=den[:, par, :]), reads=[K_den[par]], writes=[K_den[par]])
                            C.op("dve", lambda e: e.tensor_tensor(out=ytile[:, par, :].rearrange("p (h d) -> p h d", h=4), in0=pov[:, :, 0:64],
                                                                  in1=den[:, par, :].unsqueeze(2).to_broadcast([128, 4, 64]), op=ALU.mult),
                                 reads=[PSk[po], K_den[par]], writes=[K_yt[par]])
                            emit_y(3, tt, par)
                        C.barrier()
                if stop_after == f"swa{l}":
                    tap(f"YT{l}", YT[:], [t for r in YTk for t in r], [128, 8, S], BF16)
                    break

                if "moba" not in SKIP:
                    with ExitStack() as ph:
                        QA = [sb(f"QA{h}", [76, S], BF16, ph) for h in range(4)]
                        K_QA = [tks(4, f"QA{h}_") for h in range(4)]
                        K_QAs = [tks(4, f"QAs{h}_") for h in range(4)]
                        KA = [sb(f"KA{h}", [76, S], BF16, ph) for h in range(4)]
                        K_KA = [tks(4, f"KA{h}_") for h in range(4)]
                        VM = sb("VM", [128, NT, 4, 65], BF16, ph)
                        K_VM = tks(NT, "VM")
                        past = sb("mpast", [128, 128], F32, ph)
                        notown = sb("mnotown", [128, 128], F32, ph)
                        phA = ExitStack()
                        wq = sb("mwq", [128, 8, 256], BF16, phA)
                        wk = sb("mwk", [128, 8, 256], BF16, phA)
                        wv = sb("mwv", [128, 8, 256], BF16, phA)
                        K_wq, K_wk, K_wv = Tk(), Tk(), Tk()
                        wload(wq[:], K_wq, WIN[:, O_MQ:O_MQ + 256])
                        wload(wk[:], K_wk, WIN[:, O_MK:O_MK + 256])
                        wload(wv[:], K_wv, WIN[:, O_MV:O_MV + 256])
                        K_aug = Tk()
                        for h in range(4):
                            C.dma("sp", QA[h][64:76, :], cst["moba_qa"][h], writes=[K_aug])
                            C.dma("sp", KA[h][64:76, :], cst["moba_ka"][h], writes=[K_aug])
                        K_pm = Tk()
                        C.dma("sp", past[:], cst["moba_past"][0].partition_broadcast(128), writes=[K_pm])
                        C.dma("sp", notown[:], cst["moba_notown"][0].partition_broadcast(128), writes=[K_pm])
                        C.op("dve", lambda e: e.memset(VM[:, :, :, 64:65], 1.0), writes=K_VM)
                        for h in range(4):
                            proj_fm(lambda g: (QA[h][0:64, g * 512:(g + 1) * 512], K_QA[h][g]), wq, K_wq, h * 64, 64)
                            proj_fm(lambda g: (KA[h][0:64, g * 512:(g + 1) * 512], K_KA[h][g]), wk, K_wk, h * 64, 64)

                        def vm_ev(tt, pb):
                            C.op("act", lambda e: e.copy(out=VM[:, tt, :, 0:64], in_=PSB[pb][:, 0:256].rearrange("p (g d) -> p g d", g=4)),
                                 reads=[PSk[pb]], writes=[K_VM[tt]])
                        proj_tm(vm_ev, wv, K_wv, 0, 256)
                        C.barrier()
                        phA.close()
                        phB = ExitStack()
                        kms = sb("kms", [64, 4, 8], F32, phB)
                        kmb = sb("kmb", [64, 4, 8], BF16, phB)
                        K_km = Tk()
                        for h in range(4):
                            C.op("dve", lambda e: e.tensor_reduce(out=kms[:, h, :], in_=KA[h][0:64, :].rearrange("p (n s) -> p n s", n=8),
                                                                  axis=AX.X, op=ALU.add),
                                 reads=K_KA[h], writes=[K_km])
                        C.op("dve", lambda e: e.tensor_copy(out=kmb[:], in_=kms[:]), reads=[K_km], writes=[K_km])
                        for h in range(4):
                            for tt in range(NT):
                                o = (h * NT + tt) * 8
                                C.op("pe", lambda e: e.matmul(PSB[0][:, o:o + 8], QA[h][0:64, tt * 128:(tt + 1) * 128], kmb[:, h, :],
                                                              start=True, stop=True),
                                     reads=[K_QA[h][tt // 4], K_km], writes=[PSk[0]], inc=(h == 3 and tt == NT - 1))
                        gm = sb("mgm", [128, 4, 128], F32, phB)
                        m8 = sb("mm8", [128, 64, 8], F32, phB)
                        selb = sb("mselb", [128, 4, 128], F32, phB)
                        SP = sb("mSP", [128, 64, 72], BF16, phB)
                        K_gm, K_m8, K_selb, K_SP = Tk(), Tk(), Tk(), Tk()
                        C.op("dve", lambda e: e.tensor_tensor(out=gm[:], in0=PSB[0][:].rearrange("p (h x) -> p h x", h=4),
                                                              in1=past[:].unsqueeze(1).to_broadcast([128, 4, 128]), op=ALU.add),
                             reads=[PSk[0], K_pm], writes=[K_gm])
                        gmv = gm[:].rearrange("p h (t n) -> p (h t) n", n=8)
                        for gi in range(64):
                            C.op("dve", lambda e: e.max(out=m8[:, gi, :], in_=gmv[:, gi, :]), reads=[K_gm], writes=[K_m8])
                        C.op("dve", lambda e: e.tensor_tensor(out=selb[:].rearrange("p h (t n) -> p (h t) n", n=8), in0=gmv,
                                                              in1=m8[:, :, 2:3].to_broadcast([128, 64, 8]), op=ALU.is_ge),
                             reads=[K_gm, K_m8], writes=[K_selb])
                        C.op("dve", lambda e: e.tensor_scalar(out=selb[:], in0=selb[:], scalar1=-1.0, scalar2=-NEG, op0=ALU.add, op1=ALU.mult),
                             reads=[K_selb], writes=[K_selb])
                        C.op("dve", lambda e: e.tensor_tensor(out=selb[:], in0=selb[:], in1=notown[:].unsqueeze(1).to_broadcast([128, 4, 128]),
                                                              op=ALU.mult),
                             reads=[K_selb, K_pm], writes=[K_selb])
                        C.op("dve", lambda e: e.memset(SP[:], 0.0), writes=[K_SP])
                        C.op("dve", lambda e: e.tensor_copy(out=SP[:, :, 64:72], in_=selb[:].rearrange("p h (t n) -> p (h t) n", n=8)),
                             reads=[K_selb], writes=[K_SP])
                        for h in range(4):
                            for g in range(4):
                                pb = 1 + (h * 4 + g) % 2
                                for i in range(4):
                                    tt = 4 * g + i
                                    C.op("pe", lambda e: e.matmul(PSB[pb][0:72, i * 128:(i + 1) * 128], SP[:, h * NT + tt, :], ident_bf[:],
                                                                  start=True, stop=True),
                                         reads=[K_SP, K_id], writes=[PSk[pb]], inc=(i == 3))
                                C.op("act", lambda e: e.copy(out=QA[h][64:72, g * 512:(g + 1) * 512], in_=PSB[pb][64:72, :]),
                                     reads=[PSk[pb], K_aug], writes=[K_QAs[h][g]])
                        C.barrier()
                        phB.close()
                        PTp = sb("mPTp", [128, 2, 8, 512], BF16, ph)
                        PTd = sb("mPTd", [128, 2, 384], BF16, ph)
                        K_PTp = [tks(8), tks(8)]
                        K_PTd = tks(2)
                        rd = sb("mrd", [128, 2, 4], F32, ph)
                        K_rd = tks(2)
                        it = 0
                        sc = 0
                        for b in range(8):
                            for h in range(4):
                                bi = it % 2
                                it += 1
                                qrd = [K_QA[h][b // 2], K_QAs[h][b // 2]]
                                for pr in range(b):
                                    pb = sc % 2
                                    sc += 1
                                    for j in range(2):
                                        kt = 2 * pr + j
                                        C.op("pe", lambda e: e.matmul(PSB[pb][:, j * 256:(j + 1) * 256], KA[h][:, kt * 128:(kt + 1) * 128],
                                                                      QA[h][:, b * 256:(b + 1) * 256], start=True, stop=True),
                                             reads=[K_KA[h][kt // 4], K_aug] + qrd, writes=[PSk[pb]], inc=(j == 1))
                                    C.op("act", lambda e: e.activation(out=PTp[:, bi, pr, :], in_=PSB[pb][:], func=AF.Exp, scale=0.125),
                                         reads=[PSk[pb]], writes=[K_PTp[bi][pr]])
                                pb = sc % 2
                                sc += 1
                                kt = 2 * b
                                C.op("pe", lambda e: e.matmul(PSB[pb][:, 0:256], KA[h][:, kt * 128:(kt + 1) * 128],
                                                              QA[h][:, b * 256:(b + 1) * 256], start=True, stop=True),
                                     reads=[K_KA[h][kt // 4], K_aug] + qrd, writes=[PSk[pb]], inc=False)
                                kt = 2 * b + 1
                                C.op("pe", lambda e: e.matmul(PSB[pb][:, 256:384], KA[h][:, kt * 128:(kt + 1) * 128],
                                                              QA[h][:, b * 256 + 128:(b + 1) * 256], start=True, stop=True),
                                     reads=[K_KA[h][kt // 4], K_aug] + qrd, writes=[PSk[pb]])
                                C.op("act", lambda e: e.activation(out=PTd[:, bi, :], in_=PSB[pb][:, 0:384], func=AF.Exp, scale=0.125),
                                     reads=[PSk[pb]], writes=[K_PTd[bi]])
                                C.op("dve", lambda e: e.tensor_tensor(out=PTd[:, bi, 0:128], in0=PTd[:, bi, 0:128], in1=tri[:], op=ALU.mult),
                                     reads=[K_PTd[bi], K_tri], writes=[K_PTd[bi]])
                                C.op("dve", lambda e: e.tensor_tensor(out=PTd[:, bi, 256:384], in0=PTd[:, bi, 256:384], in1=tri[:], op=ALU.mult),
                                     reads=[K_PTd[bi], K_tri], writes=[K_PTd[bi]])
                                for qi in range(2):
                                    po = 4 + qi
                                    for pr in range(b):
                                        for j in range(2):
                                            kt = 2 * pr + j
                                            C.op("pe", lambda e: e.matmul(PSB[po][:, h * 65:(h + 1) * 65],
                                                                          PTp[:, bi, pr, j * 256 + qi * 128:j * 256 + (qi + 1) * 128],
                                                                          VM[:, kt, h, :], start=(kt == 0), stop=False),
                                                 reads=[K_PTp[bi][pr], K_VM[kt]], writes=[PSk[po]], inc=False)
                                    C.op("pe", lambda e: e.matmul(PSB[po][:, h * 65:(h + 1) * 65], PTd[:, bi, qi * 128:(qi + 1) * 128],
                                                                  VM[:, 2 * b, h, :], start=(b == 0), stop=(qi == 0)),
                                         reads=[K_PTd[bi], K_VM[2 * b]], writes=[PSk[po]], inc=(qi == 0))
                                    if qi == 1:
                                        C.op("pe", lambda e: e.matmul(PSB[po][:, h * 65:(h + 1) * 65], PTd[:, bi, 256:384],
                                                                      VM[:, 2 * b + 1, h, :], start=False, stop=True),
                                             reads=[K_PTd[bi], K_VM[2 * b + 1]], writes=[PSk[po]])
                            for qi in range(2):
                                tt = 2 * b + qi
                                po = 4 + qi
                                par = qi
                                pov = PSB[po][:, 0:260].rearrange("p (h d) -> p h d", h=4)
                                C.op("dve", lambda e: e.reciprocal(out=rd[:, par, :], in_=pov[:, :, 64]), reads=[PSk[po]], writes=[K_rd[par]])
                                C.op("dve", lambda e: e.tensor_tensor(out=ytile[:, par, :].rearrange("p (h d) -> p h d", h=4), in0=pov[:, :, 0:64],
                                                                      in1=rd[:, par, :].unsqueeze(2).to_broadcast([128, 4, 64]), op=ALU.mult),
                                     reads=[PSk[po], K_rd[par]], writes=[K_yt[par]])
                                emit_y(0, tt, par)
                        C.barrier()
                if stop_after == f"moba{l}":
                    tap(f"YT{l}", YT[:], [t for r in YTk for t in r], [128, 8, S], BF16)
                    break

                for br in (("ret",) if stop_after in (f"retonly{l}", f"retall{l}") else tuple(b_ for b_ in ("gla", "ret") if b_ not in SKIP)):
                    with ExitStack() as ph:
                        gla = br == "gla"
                        ncol = 784 if gla else 1024
                        wg = sb("lw", [128, 8, ncol], BF16, ph)
                        K_wg = Tk()
                        wload(wg[:], K_wg, WIN[:, (O_GQ if gla else O_RQ):(O_GQ if gla else O_RQ) + ncol])
                        gbc = sb("lgbc", [128, 256], F32, ph)
                        K_gbc = Tk()
                        C.dma("sp", gbc[:], dr["g_gla_norm" if gla else "g_ret_norm"][l].partition_broadcast(128), writes=[K_gbc])
                        K_cn = Tk()
                        if gla:
                            wgg = sb("lwgg", [32, 128], BF16, ph)
                            C.dma("pool", wgg[0:16, :], dr["w_gla_gate"][l], writes=[K_cn])
                            C.dma("pool", wgg[16:17, :], dr["b_gla_gate"][l:l + 1, :], writes=[K_cn])
                            cinc = sb("lcinc", [128, 128], F32, ph)
                            caft = sb("lcaft", [128, 128], F32, ph)
                            C.dma("sp", cinc[:], cst["cum_incl"], writes=[K_cn])
                            C.dma("sp", caft[:], cst["cum_after"], writes=[K_cn])
                            gaT = sb("lgaT", [32, 2, 128], BF16, ph)
                            K_gaT = tks(2)
                            C.op("dve", lambda e: e.memset(gaT[:], 1.0), writes=K_gaT)
                            LA = sb("lLA", [128, 2, 128], F32, ph)
                            K_LA = tks(2)
                            EB = sb("lEB", [128, 2, 3, 128], F32, ph)
                            K_EB = tks(2)
                            dec = sb("ldec", [128, 2], F32, ph)
                            nft = 1
                            kd = 32
                        else:
                            rqc = sb("lrqc", [128, 2, 128], F32, ph)
                            rkc = sb("lrkc", [128, 2, 128], F32, ph)
                            rkend = sb("lrkend", [128, 256], F32, ph)
                            rdec = sb("lrdec", [128, 2], F32, ph)
                            C.dma("sp", rqc[:], cst["ret_q"], writes=[K_cn])
                            C.dma("sp", rkc[:], cst["ret_k"], writes=[K_cn])
                            C.dma("sp", rkend[:], cst["ret_kend"], writes=[K_cn])
                            K_rd_ = Tk()
                            for h_ in range(4):
                                gv_ = float((1.0 - 2.0 ** (-5.0 - h_)) ** 128.0)
                                o_ = (h_ % 2) * 64
                                C.op("dve", lambda e: e.memset(rdec[o_:o_ + 64, h_ // 2:h_ // 2 + 1], gv_), writes=[K_rd_])
                            nft = 2
                            kd = 64
                        qd = sb("lqd", [128, 2, nft, 128], BF16, ph)
                        kin = sb("lkin", [128, 2, 4, 128], BF16, ph)
                        K_kin = tks(2)
                        C.op("dve", lambda e: e.memset(kin[:], 0.0), writes=K_kin)
                        SW = 256 if gla else 128
                        bdm = sb("lbdm", [128, SW], F32, ph)
                        C.dma("sp", bdm[:], cst["bd_gla" if gla else "bd_ret"], writes=[K_cn])
                        kvt = sb("lkvt", [128, 2, SW], F32, ph)
                        K_kvt = tks(2)
                        kend = sb("lkend", [128, 2, nft * 128], BF16, ph)
                        vv = sb("lvv", [128, 2, 256], BF16, ph)
                        ggr = sb("lggr", [128, 2, 256], F32, ph)
                        atm = sb("latm", [128, 2, 4, 128], BF16, ph)
                        K_qd, K_kend, K_vv, K_ggr, K_atm = tks(2), tks(2), tks(2), tks(2), tks(2)
                        Sf = sb("lSf", [128, 2, nft, SW], F32, ph)
                        Sb = sb("lSb", [128, 2, nft, SW], BF16, ph)
                        K_Sf, K_Sb = tks(2), tks(2)
                        C.op("dve", lambda e: e.memset(Sf[:], 0.0), writes=K_Sf)
                        sq = sb("lsq", [128, 2, 256], F32, ph)
                        st = sb("lst", [128, 2, 3, 4], F32, ph)
                        K_sq, K_st = tks(2), tks(2)
                        t1 = sb("lt1", [128, 2, 256], F32, ph)
                        K_t1 = tks(2)
                        for tt in range((NT if gla else RETTILES) if stop_after != f"retonly{l}" else 1):
                            if RETV in (32, 33) and tt > 0 and not gla:
                                C.barrier()
                            _rc = RETCUT
                            if RETV in (20, 22, 32, 30):
                                _rc = 3 if tt == 0 else 2
                            if RETV == 21:
                                _rc = 3 if tt == 0 else 1
                            p = tt % 2 if RETV != 4 else 0
                            q = 1 - p
                            hr = HTr(tt // 4)
                            tok = slice(tt * 128, (tt + 1) * 128)

                            def mmK(out, lhs, rhs, wr, inc8=True):
                                for kt in range(8):
                                    C.op("pe", lambda e: e.matmul(out, lhs(kt), rhs(kt), start=(kt == 0), stop=(kt == 7)),
                                         reads=[K_wg] + hr, writes=[wr], inc=(kt == 7))
                            for j in range(nft):
                                mmK(PSB[0][:, j * 128:(j + 1) * 128], lambda kt: wg[:, kt, j * 128:(j + 1) * 128], lambda kt: HT[:, kt, tok], PSk[0])
                                ko = 128 if gla else 256
                                mmK(PSB[0][:, (nft + j) * 128:(nft + j + 1) * 128], lambda kt: wg[:, kt, ko + j * 128:ko + (j + 1) * 128],
                                    lambda kt: HT[:, kt, tok], PSk[0])
                            if gla:
                                mmK(PSB[1][0:16, 0:128], lambda kt: wg[:, kt, 512:528], lambda kt: HT[:, kt, tok], PSk[1])
                                C.op("act", lambda e: e.copy(out=gaT[0:16, p, :], in_=PSB[1][0:16, 0:128]), reads=[PSk[1]], writes=[K_gaT[p]])
                                C.op("pe", lambda e: e.matmul(PSB[1][:, 128:256], gaT[0:17, p, :], wgg[0:17, :], start=True, stop=True),
                                     reads=[K_gaT[p], K_cn], writes=[PSk[1]])
                                C.op("act", lambda e: e.activation(out=LA[:, p, :], in_=PSB[1][:, 128:256], func=AF.Exp, scale=-1.0),
                                     reads=[PSk[1]], writes=[K_LA[p]])
                                C.op("act", lambda e: e.activation(out=LA[:, p, :], in_=LA[:, p, :], func=AF.Ln, bias=1.0),
                                     reads=[K_LA[p]], writes=[K_LA[p]])
                                C.op("pe", lambda e: e.matmul(PSB[1][:, 256:384], LA[:, p, :], cinc[:], start=True, stop=True),
                                     reads=[K_LA[p], K_cn], writes=[PSk[1]])
                                C.op("pe", lambda e: e.matmul(PSB[1][:, 384:512], caft[:], LA[:, p, :], start=True, stop=True),
                                     reads=[K_LA[p], K_cn], writes=[PSk[1]])
                                C.op("act", lambda e: e.activation(out=EB[:, p, 0, :], in_=PSB[1][:, 256:384], func=AF.Exp), reads=[PSk[1]], writes=[K_EB[p]])
                                C.op("act", lambda e: e.activation(out=EB[:, p, 1, :], in_=PSB[1][:, 256:384], func=AF.Exp, scale=-1.0), reads=[PSk[1]], writes=[K_EB[p]])
                                C.op("act", lambda e: e.activation(out=EB[:, p, 2, :], in_=PSB[1][:, 384:512], func=AF.Exp), reads=[PSk[1]], writes=[K_EB[p]])
                                C.op("dve", lambda e: e.scalar_tensor_tensor(out=qd[:, p, 0, :], in0=PSB[0][:, 0:128], scalar=32.0 ** -0.5, in1=EB[:, p, 0, :],
                                                                             op0=ALU.mult, op1=ALU.mult), reads=[PSk[0], K_EB[p]], writes=[K_qd[p]])
                                for h in range(4):
                                    o = 32 * h
                                    C.op("dve", lambda e: e.tensor_tensor(out=kin[o:o + 32, p, h, :], in0=PSB[0][o:o + 32, 128:256], in1=EB[o:o + 32, p, 1, :], op=ALU.mult),
                                         reads=[PSk[0], K_EB[p]], writes=[K_kin[p]])
                            else:
                                if RETV in (10, 12):
                                    mmK(PSB[1][0:16, 256:384], lambda kt: wg[:, kt, 0:16], lambda kt: HT[:, kt, tok], PSk[1])
                                if RETV in (11, 12):
                                    C.op("pe", lambda e: e.matmul(PSB[1][:, 384:512], rqc[:, 0, :], rkc[:, 0, :], start=True, stop=True),
                                         reads=[K_cn], writes=[PSk[1]])
                                C.op("dve", lambda e: e.tensor_tensor(out=qd[:, p, :, :], in0=PSB[0][:, 0:256].rearrange("p (j x) -> p j x", j=2),
                                                                      in1=rqc[:], op=ALU.mult), reads=[PSk[0], K_cn], writes=[K_qd[p]])
                                for h in range(4):
                                    j, o = h // 2, (h % 2) * 64
                                    C.op("dve", lambda e: e.tensor_tensor(out=kin[o:o + 64, p, h, :], in0=PSB[0][o:o + 64, 256 + j * 128:256 + (j + 1) * 128],
                                                                          in1=rkc[o:o + 64, j, :], op=ALU.mult), reads=[PSk[0], K_cn], writes=[K_kin[p]])
                            if _rc <= 1:
                                continue
                            nk = nft * 128
                            ko = 128 if gla else 256
                            if RETV == 30:
                                mmK(PSB[2][:, 0:nk], lambda kt: HT[:, kt, tok], lambda kt: wg[:, kt, ko:ko + nk], PSk[2])
                                mmK(PSB[2][:, nk:nk + 256], lambda kt: HT[:, kt, tok], lambda kt: wg[:, kt, ko + nk:ko + nk + 256], PSk[2])
                            else:
                                mmK(PSB[2][:, 0:nk + 256], lambda kt: HT[:, kt, tok], lambda kt: wg[:, kt, ko:ko + nk + 256], PSk[2])
                            go = 528 if gla else 768
                            mmK(PSB[3][:, 0:256], lambda kt: HT[:, kt, tok], lambda kt: wg[:, kt, go:go + 256], PSk[3])
                            if gla:
                                C.op("dve", lambda e: e.tensor_tensor(out=kend[:, p, :], in0=PSB[2][:, 0:128], in1=EB[:, p, 2, :], op=ALU.mult),
                                     reads=[PSk[2], K_EB[p]], writes=[K_kend[p]])
                            else:
                                C.op("dve", lambda e: e.tensor_tensor(out=kend[:, p, :], in0=PSB[2][:, 0:256], in1=rkend[:], op=ALU.mult),
                                     reads=[PSk[2], K_cn], writes=[K_kend[p]])
                            C.op("act", lambda e: e.copy(out=vv[:, p, :], in_=PSB[2][:, nk:nk + 256]), reads=[PSk[2]], writes=[K_vv[p]])
                            C.op("act", lambda e: e.activation(out=ggr[:, p, :], in_=PSB[3][:, 0:256], func=AF.Silu), reads=[PSk[3]], writes=[K_ggr[p]])
                            C.op("dve", lambda e: e.tensor_tensor(out=ggr[:, p, :], in0=ggr[:, p, :], in1=gbc[:], op=ALU.mult),
                                 reads=[K_ggr[p], K_gbc], writes=[K_ggr[p]])
                            if _rc <= 2:
                                continue
                            for h in range(4 if not (RETV == 9 and tt == 1) else 0):
                                j = 0 if gla else h // 2
                                C.op("pe", lambda e: e.matmul(PSB[4][:, h * 128:(h + 1) * 128], kin[:, p, h, :], qd[:, p, j, :],
                                                              start=True, stop=True),
                                     reads=[K_kin[p], K_qd[p]], writes=[PSk[4]], inc=(h == 3))
                            if not (RETV in (9, 13) and tt == 1):
                                C.op("dve", lambda e: e.tensor_tensor(out=atm[:, p, :, :], in0=PSB[4][:].rearrange("p (h x) -> p h x", h=4),
                                                                      in1=tri[:].unsqueeze(1).to_broadcast([128, 4, 128]), op=ALU.mult),
                                     reads=[PSk[4], K_tri], writes=[K_atm[p]])
                            obank = [5, 6]
                            if tt > 0 and RETV not in (1, 5, 6, 7, 8):
                                for j in range(nft if RETV != 2 else 1):
                                    C.op("pe", lambda e: e.matmul(PSB[obank[j]][:, 0:SW], qd[:, p, j, :], Sb[:, q, j, :], start=True, stop=False),
                                         reads=[K_qd[p], K_Sb[q]], writes=[PSk[obank[j]]], inc=(RETV == 3))
                            for h in range(4):
                                if (tt == 1 and RETV in (5, 13)) or (tt == 0 and RETV == 22) or (tt == 1 and False) or (tt == 1 and (False or (RETV == 6 and h >= 2) or (RETV == 8 and h < 2))):
                                    continue
                                j, oc = (0, h * 64) if gla else (h // 2, (h % 2) * 64)
                                C.op("pe", lambda e: e.matmul(PSB[obank[j]][:, oc:oc + 64], atm[:, p, h, :], vv[:, p, h * 64:(h + 1) * 64],
                                                              start=(tt == 0 or RETV in (1, 5, 6, 7, 8, 9, 13) or (RETV == 2 and h >= 2)), stop=(tt == 0 or h == 3 or (not gla and h == 1))),
                                     reads=[K_atm[p], K_vv[p]], writes=[PSk[obank[j]]], inc=True)
                            if _rc <= 3:
                                continue
                            if tt < NT - 1:
                                kvb = 1 if not gla else 3
                                for j in range(nft):
                                    C.op("pe", lambda e: e.matmul(PSB[kvb][:, 256:256 + SW] if gla else PSB[kvb][:, j * 128:(j + 1) * 128],
                                                                  kend[:, p, j * 128:(j + 1) * 128], vv[:, p, j * SW:(j + 1) * SW] if not gla else vv[:, p, :],
                                                                  start=True, stop=True),
                                         reads=[K_kend[p], K_vv[p]], writes=[PSk[kvb]])
                                    src = PSB[kvb][:, 256:256 + SW] if gla else PSB[kvb][:, j * 128:(j + 1) * 128]
                                    C.op("dve", lambda e: e.tensor_tensor(out=kvt[:, j if not gla else 0, :], in0=src, in1=bdm[:], op=ALU.mult),
                                         reads=[PSk[kvb], K_cn], writes=[K_kvt[j]])
                                    if gla:
                                        C.op("act", lambda e: e.copy(out=dec[:, p:p + 1], in_=EB[:, p, 0, 127:128]), reads=[K_EB[p]], writes=[K_EB[p]])
                                        dsc = dec[:, p:p + 1]
                                    else:
                                        dsc = rdec[:, j:j + 1]
                                    C.op("dve", lambda e: e.scalar_tensor_tensor(out=Sf[:, p, j, :], in0=Sf[:, q, j, :], scalar=dsc,
                                                                                 in1=kvt[:, j if not gla else 0, :], op0=ALU.mult, op1=ALU.add),
                                         reads=[K_Sf[q], K_kvt[j], K_EB[p] if gla else K_cn], writes=[K_Sf[p]])
                                C.op("act", lambda e: e.copy(out=Sb[:, p, :, :], in_=Sf[:, p, :, :]), reads=[K_Sf[p]], writes=[K_Sb[p]])
                            if _rc <= 4:
                                continue
                            osb = t1
                            for j in range(nft):
                                C.op("act", lambda e: e.copy(out=t1[:, p, j * SW:(j + 1) * SW], in_=PSB[obank[j]][:, 0:SW]), reads=[PSk[obank[j]]], writes=[K_t1[p]])
                            ov = t1[:, p, :].rearrange("p (h d) -> p h d", h=4)
                            C.op("act", lambda e: e.activation(out=sq[:, p, :], in_=t1[:, p, :], func=AF.Square), reads=[K_t1[p]], writes=[K_sq[p]])
                            C.op("dve", lambda e: e.tensor_reduce(out=st[:, p, 0, :], in_=sq[:, p, :].rearrange("p (h d) -> p h d", h=4), axis=AX.X, op=ALU.add),
                                 reads=[K_sq[p]], writes=[K_st[p]])
                            if _rc <= 4.2:
                                continue
                            if gla:
                                C.op("dve", lambda e: e.tensor_scalar(out=st[:, p, 0, :], in0=st[:, p, 0, :], scalar1=1.0 / 64, scalar2=EPS, op0=ALU.mult, op1=ALU.add),
                                     reads=[K_st[p]], writes=[K_st[p]])
                            else:
                                C.op("dve", lambda e: e.tensor_reduce(out=st[:, p, 1, :], in_=ov, axis=AX.X, op=ALU.add), reads=[K_t1[p]], writes=[K_st[p]])
                                C.op("dve", lambda e: e.tensor_scalar(out=st[:, p, 1, :], in0=st[:, p, 1, :], scalar1=-1.0 / 64, scalar2=None, op0=ALU.mult),
                                     reads=[K_st[p]], writes=[K_st[p]])
                                C.op("dve", lambda e: e.tensor_tensor(out=st[:, p, 2, :], in0=st[:, p, 1, :], in1=st[:, p, 1, :], op=ALU.mult),
                                     reads=[K_st[p]], writes=[K_st[p]])
                                C.op("dve", lambda e: e.scalar_tensor_tensor(out=st[:, p, 0, :], in0=st[:, p, 0, :], scalar=1.0 / 64, in1=st[:, p, 2, :],
                                                                             op0=ALU.mult, op1=ALU.subtract), reads=[K_st[p]], writes=[K_st[p]])
                                C.op("dve", lambda e: e.tensor_scalar(out=st[:, p, 0, :], in0=st[:, p, 0, :], scalar1=EPS, scalar2=None, op0=ALU.add),
                                     reads=[K_st[p]], writes=[K_st[p]])
                            if _rc <= 4.4:
                                continue
                            C.op("act", lambda e: e.activation(out=st[:, p, 0, :], in_=st[:, p, 0, :], func=AF.Sqrt), reads=[K_st[p]], writes=[K_st[p]])
                            C.op("dve", lambda e: e.reciprocal(out=st[:, p, 0, :], in_=st[:, p, 0, :]), reads=[K_st[p]], writes=[K_st[p]])
                            if _rc <= 4.6:
                                continue
                            t1v = t1[:, p, :].rearrange("p (h d) -> p h d", h=4)
                            if gla:
                                C.op("dve", lambda e: e.tensor_tensor(out=t1v, in0=ov, in1=st[:, p, 0, :].unsqueeze(2).to_broadcast([128, 4, 64]), op=ALU.mult),
                                     reads=[K_t1[p], K_st[p]], writes=[K_t1[p]])
                            else:
                                C.op("dve", lambda e: e.tensor_tensor(out=t1v, in0=ov, in1=st[:, p, 1, :].unsqueeze(2).to_broadcast([128, 4, 64]), op=ALU.add),
                                     reads=[K_t1[p], K_st[p]], writes=[K_t1[p]])
                                C.op("dve", lambda e: e.tensor_tensor(out=t1v, in0=t1v, in1=st[:, p, 0, :].unsqueeze(2).to_broadcast([128, 4, 64]), op=ALU.mult),
                                     reads=[K_t1[p], K_st[p]], writes=[K_t1[p]])
                            if _rc <= 4.8:
                                continue
                            C.op("dve", lambda e: e.tensor_tensor(out=ytile[:, p, :], in0=t1[:, p, :], in1=ggr[:, p, :], op=ALU.mult),
                                 reads=[K_t1[p], K_ggr[p]], writes=[K_yt[p]])
                            if _rc <= 5:
                                continue
                            emit_y(1 if gla else 2, tt, p)
                        C.barrier()
                    if stop_after == f"{br}{l}":
                        break
                    if stop_after == f"retall{l}" and "memsetYT" not in SKIP:
                        pass
                if "retq" not in SKIP:
                    with ExitStack() as ph:
                        VR = sb("VR", [128, NT, 4, 64], BF16, ph)
                        K_VR = tks(NT, "VR")
                        GG = sb("GG", [128, NT, 256], BF16, ph)
                        K_GG = tks(NT, "GG")
                        gbc = sb("rgbc", [128, 256], F32, ph)
                        K_gbc = Tk()
                        C.dma("sp", gbc[:], dr["g_ret_norm"][l].partition_broadcast(128), writes=[K_gbc])
                        gtmp = sb("rgtmp", [128, 2, 256], F32, ph)
                        K_gtmp = tks(2)
                        PTp = sb("rPTp", [128, 8, 512], BF16, ph)
                        PTd = sb("rPTd", [128, 2, 384], BF16, ph)
                        K_PTp = tks(8)
                        K_PTd = tks(2)
                        t1 = sb("rt1", [128, 2, 128], F32, ph)
                        sq = sb("rsq", [128, 2, 128], F32, ph)
                        st = sb("rst", [128, 2, 3, 2], F32, ph)
                        K_t1, K_sq, K_st = tks(2), tks(2), tks(2)
                        QR = [sb(f"QR{i}", [64, S], BF16, ph) for i in range(2)]
                        KR = [sb(f"KR{i}", [64, S], BF16, ph) for i in range(2)]
                        K_QR = [tks(4), tks(4)]
                        K_KR = [tks(4), tks(4)]
                        with ExitStack() as phA:
                            wvg = sb("rwvg", [128, 8, 512], BF16, phA)
                            K_wvg = Tk()
                            wload(wvg[:], K_wvg, WIN[:, O_RV:O_RV + 512])

                            def vr_ev(tt, pb):
                                C.op("act", lambda e: e.copy(out=VR[:, tt, :, :], in_=PSB[pb][:, 0:256].rearrange("p (g d) -> p g d", g=4)),
                                     reads=[PSk[pb]], writes=[K_VR[tt]])
                            proj_tm(vr_ev, wvg, K_wvg, 0, 256)

                            def gg_ev(tt, pb):
                                bi = tt % 2
                                C.op("act", lambda e: e.activation(out=gtmp[:, bi, :], in_=PSB[pb][:, 0:256], func=AF.Silu), reads=[PSk[pb]], writes=[K_gtmp[bi]])
                                C.op("dve", lambda e: e.tensor_tensor(out=GG[:, tt, :], in0=gtmp[:, bi, :], in1=gbc[:], op=ALU.mult),
                                     reads=[K_gtmp[bi], K_gbc], writes=[K_GG[tt]])
                            proj_tm(gg_ev, wvg, K_wvg, 256, 256)
                            C.barrier()
                        for pair in range(2):
                            with ExitStack() as phB:
                                wqk = sb("rwqk", [128, 8, 256], BF16, phB)
                                K_wqk = Tk()
                                wload(wqk[:, :, 0:128], K_wqk, WIN[:, O_RQ + pair * 128:O_RQ + (pair + 1) * 128])
                                wload(wqk[:, :, 128:256], K_wqk, WIN[:, O_RK + pair * 128:O_RK + (pair + 1) * 128])
                                dq = sb("rdq", [64, 1, S], BF16, phB)
                                dk = sb("rdk", [64, 1, S], BF16, phB)
                                K_dqk = Tk()
                                ev = 0
                                for hh in range(2):
                                    C.dma("pool", dq[:, 0, :], cst["ret_qd"][2 * pair + hh].partition_broadcast(64), writes=[K_dqk])
                                    C.dma("pool", dk[:, 0, :], cst["ret_kd"][2 * pair + hh].partition_broadcast(64), writes=[K_dqk])
                                    for which in range(2):
                                        for g in range(4):
                                            pb = ev % 2
                                            ev += 1
                                            c0 = which * 128 + hh * 64
                                            for kt in range(8):
                                                C.op("pe", lambda e: e.matmul(PSB[pb][0:64, :], wqk[:, kt, c0:c0 + 64], HT[:, kt, g * 512:(g + 1) * 512],
                                                                              start=(kt == 0), stop=(kt == 7)),
                                                     reads=[K_wqk] + HTr(g), writes=[PSk[pb]], inc=(kt == 7))
                                            dst = (QR if which == 0 else KR)[hh][:, g * 512:(g + 1) * 512]
                                            dtk = (K_QR if which == 0 else K_KR)[hh][g]
                                            dec_ = (dq if which == 0 else dk)[:, 0, g * 512:(g + 1) * 512]
                                            C.op("dve", lambda e: e.tensor_tensor(out=dst, in0=PSB[pb][0:64, :], in1=dec_, op=ALU.mult),
                                                 reads=[PSk[pb], K_dqk], writes=[dtk])
                                C.barrier()
                            sc = 0
                            for b in range(8):
                                for hh in range(2):
                                    h = 2 * pair + hh
                                    for pr in range(b):
                                        pb = sc % 2
                                        sc += 1
                                        for j in range(2):
                                            kt = 2 * pr + j
                                            C.op("pe", lambda e: e.matmul(PSB[pb][:, j * 256:(j + 1) * 256], KR[hh][:, kt * 128:(kt + 1) * 128],
                                                                          QR[hh][:, b * 256:(b + 1) * 256], start=True, stop=True),
                                                 reads=[K_KR[hh][kt // 4], K_QR[hh][b // 2]], writes=[PSk[pb]], inc=(j == 1))
                                        C.op("act", lambda e: e.activation(out=PTp[:, pr, :], in_=PSB[pb][:], func=AF.Identity, scale=0.125),
                                             reads=[PSk[pb]], writes=[K_PTp[pr]])
                                    pb = sc % 2
                                    sc += 1
                                    bi = hh
                                    kt = 2 * b
                                    C.op("pe", lambda e: e.matmul(PSB[pb][:, 0:256], KR[hh][:, kt * 128:(kt + 1) * 128],
                                                                  QR[hh][:, b * 256:(b + 1) * 256], start=True, stop=True),
                                         reads=[K_KR[hh][kt // 4], K_QR[hh][b // 2]], writes=[PSk[pb]], inc=False)
                                    kt = 2 * b + 1
                                    C.op("pe", lambda e: e.matmul(PSB[pb][:, 256:384], KR[hh][:, kt * 128:(kt + 1) * 128],
                                                                  QR[hh][:, b * 256 + 128:(b + 1) * 256], start=True, stop=True),
                                         reads=[K_KR[hh][kt // 4], K_QR[hh][b // 2]], writes=[PSk[pb]])
                                    C.op("act", lambda e: e.activation(out=PTd[:, bi, :], in_=PSB[pb][:, 0:384], func=AF.Identity, scale=0.125),
                                         reads=[PSk[pb]], writes=[K_PTd[bi]])
                                    C.op("dve", lambda e: e.tensor_tensor(out=PTd[:, bi, 0:128], in0=PTd[:, bi, 0:128], in1=tri[:], op=ALU.mult),
                                         reads=[K_PTd[bi], K_tri], writes=[K_PTd[bi]])
                                    C.op("dve", lambda e: e.tensor_tensor(out=PTd[:, bi, 256:384], in0=PTd[:, bi, 256:384], in1=tri[:], op=ALU.mult),
                                         reads=[K_PTd[bi], K_tri], writes=[K_PTd[bi]])
                                    for qi in range(2):
                                        po = 4 + qi
                                        for pr in range(b):
                                            for j in range(2):
                                                kt = 2 * pr + j
                                                C.op("pe", lambda e: e.matmul(PSB[po][:, hh * 64:(hh + 1) * 64],
                                                                              PTp[:, pr, j * 256 + qi * 128:j * 256 + (qi + 1) * 128],
                                                                              VR[:, kt, h, :], start=(kt == 0), stop=False),
                                                     reads=[K_PTp[pr], K_VR[kt]], writes=[PSk[po]], inc=False)
                                        C.op("pe", lambda e: e.matmul(PSB[po][:, hh * 64:(hh + 1) * 64], PTd[:, bi, qi * 128:(qi + 1) * 128],
                                                                      VR[:, 2 * b, h, :], start=(b == 0), stop=(qi == 0)),
                                             reads=[K_PTd[bi], K_VR[2 * b]], writes=[PSk[po]], inc=(qi == 0))
                                        if qi == 1:
                                            C.op("pe", lambda e: e.matmul(PSB[po][:, hh * 64:(hh + 1) * 64], PTd[:, bi, 256:384],
                                                                          VR[:, 2 * b + 1, h, :], start=False, stop=True),
                                                 reads=[K_PTd[bi], K_VR[2 * b + 1]], writes=[PSk[po]])
                                for qi in range(2):
                                    tt = 2 * b + qi
                                    po = 4 + qi
                                    p = qi
                                    C.op("act", lambda e: e.copy(out=t1[:, p, :], in_=PSB[po][:, 0:128]), reads=[PSk[po]], writes=[K_t1[p]])
                                    C.op("act", lambda e: e.activation(out=sq[:, p, :], in_=t1[:, p, :], func=AF.Square), reads=[K_t1[p]], writes=[K_sq[p]])
                                    ov = t1[:, p, :].rearrange("p (h d) -> p h d", h=2)
                                    C.op("dve", lambda e: e.tensor_reduce(out=st[:, p, 0, :], in_=sq[:, p, :].rearrange("p (h d) -> p h d", h=2), axis=AX.X, op=ALU.add),
                                         reads=[K_sq[p]], writes=[K_st[p]])
                                    C.op("dve", lambda e: e.tensor_reduce(out=st[:, p, 1, :], in_=ov, axis=AX.X, op=ALU.add), reads=[K_t1[p]], writes=[K_st[p]])
                                    C.op("dve", lambda e: e.tensor_scalar(out=st[:, p, 1, :], in0=st[:, p, 1, :], scalar1=-1.0 / 64, scalar2=None, op0=ALU.mult),
                                         reads=[K_st[p]], writes=[K_st[p]])
                                    C.op("dve", lambda e: e.tensor_tensor(out=st[:, p, 2, :], in0=st[:, p, 1, :], in1=st[:, p, 1, :], op=ALU.mult),
                                         reads=[K_st[p]], writes=[K_st[p]])
                                    C.op("dve", lambda e: e.scalar_tensor_tensor(out=st[:, p, 0, :], in0=st[:, p, 0, :], scalar=1.0 / 64, in1=st[:, p, 2, :],
                                                                                 op0=ALU.mult, op1=ALU.subtract), reads=[K_st[p]], writes=[K_st[p]])
                                    C.op("dve", lambda e: e.tensor_scalar(out=st[:, p, 0, :], in0=st[:, p, 0, :], scalar1=EPS, scalar2=None, op0=ALU.add),
                                         reads=[K_st[p]], writes=[K_st[p]])
                                    C.op("act", lambda e: e.activation(out=st[:, p, 0, :], in_=st[:, p, 0, :], func=AF.Sqrt), reads=[K_st[p]], writes=[K_st[p]])
                                    C.op("dve", lambda e: e.reciprocal(out=st[:, p, 0, :], in_=st[:, p, 0, :]), reads=[K_st[p]], writes=[K_st[p]])
                                    C.op("dve", lambda e: e.tensor_tensor(out=ov, in0=ov, in1=st[:, p, 1, :].unsqueeze(2).to_broadcast([128, 2, 64]), op=ALU.add),
                                         reads=[K_t1[p], K_st[p]], writes=[K_t1[p]])
                                    C.op("dve", lambda e: e.tensor_tensor(out=ov, in0=ov, in1=st[:, p, 0, :].unsqueeze(2).to_broadcast([128, 2, 64]), op=ALU.mult),
                                         reads=[K_t1[p], K_st[p]], writes=[K_t1[p]])
                                    C.op("dve", lambda e: e.tensor_tensor(out=ytile[:, p, 0:128], in0=t1[:, p, :], in1=GG[:, tt, pair * 128:(pair + 1) * 128], op=ALU.mult),
                                         reads=[K_t1[p], K_GG[tt]], writes=[K_yt[p]])
                                    C.op("pe", lambda e: e.transpose(ps_bf(7)[:, 0:128], ytile[:, p, 0:128], ident_bf[:]),
                                         reads=[K_yt[p], K_id], writes=[PSk[7]])
                                    C.op("act", lambda e: e.copy(out=YT[:, 4 + pair, tt * 128:(tt + 1) * 128], in_=ps_bf(7)[:, 0:128]),
                                         reads=[PSk[7]], writes=[YTk[4 + pair][tt]])
                            C.barrier()
                if stop_after == f"retq{l}":
                    tap(f"YT{l}", YT[:], [t for r in YTk for t in r], [128, 8, S], BF16)
                    break
                if stop_after in (f"gla{l}", f"ret{l}", f"retonly{l}", f"retall{l}"):
                    tap(f"YT{l}", YT[:], [t for r in YTk for t in r], [128, 8, S], BF16)
                    break

                with ExitStack() as ph:
                    MP = sb("MP", [128, 8, S], BF16, ph)
                    MPk = [tks(4, f"MP{f}_") for f in range(8)]
                    with ExitStack() as ph2:
                        wm = sb("wm", [128, 2, 8, 256], BF16, ph2)
                        wbr = sb("wbr", [128, 2, 2, D], BF16, ph2)
                        K_wm, K_wbr = tks(2), tks(2)
                        sig = sb("sig", [128, 2, 512], BF16, ph2)
                        prod = sb("prod", [128, 2, 512], F32, ph2)
                        K_sig, K_prod = tks(2), tks(2)
                        ci_ = 0
                        it = 0
                        for n in range(4):
                            wb_ = n % 2
                            for hf_ in range(2):
                                C.dma("pool", wbr[:, wb_, :, hf_ * 512:(hf_ + 1) * 512],
                                      dr["w_branch"][l][n].rearrange("(ci p) d -> p ci d", p=128)[:, :, hf_ * 512:(hf_ + 1) * 512], writes=[K_wbr[wb_]])
                            for fp in range(4):
                                w_ = ci_ % 2
                                ci_ += 1
                                c0 = O_MG + n * D + fp * 256
                                wload(wm[:, w_, :, :], K_wm[w_], WIN[:, c0:c0 + 256])
                                for fl in range(2):
                                    ft = fp * 2 + fl
                                    for g in range(4):
                                        b0, b1, bi = it % 2, 2 + it % 2, it % 2
                                        it += 1
                                        for kt in range(8):
                                            C.op("pe", lambda e: e.matmul(PSB[b0][:], wm[:, w_, kt, fl * 128:(fl + 1) * 128], HT[:, kt, g * 512:(g + 1) * 512],
                                                                          start=(kt == 0), stop=(kt == 7)),
                                                 reads=[K_wm[w_]] + HTr(g), writes=[PSk[b0]], inc=(kt == 7))
                                        for ci in range(2):
                                            C.op("pe", lambda e: e.matmul(PSB[b1][:], wbr[:, wb_, ci, ft * 128:(ft + 1) * 128], YT[:, 2 * n + ci, g * 512:(g + 1) * 512],
                                                                          start=(ci == 0), stop=(ci == 1)),
                                                 reads=[K_wbr[wb_]] + YTk[2 * n + ci][4 * g:4 * g + 4], writes=[PSk[b1]], inc=(ci == 1))
                                        C.op("act", lambda e: e.activation(out=sig[:, bi, :], in_=PSB[b0][:], func=AF.Sigmoid), reads=[PSk[b0]], writes=[K_sig[bi]])
                                        mp = MP[:, ft, g * 512:(g + 1) * 512]
                                        if n == 0:
                                            C.op("dve", lambda e: e.tensor_tensor(out=mp, in0=sig[:, bi, :], in1=PSB[b1][:], op=ALU.mult),
                                                 reads=[K_sig[bi], PSk[b1]], writes=[MPk[ft][g]])
                                        else:
                                            C.op("dve", lambda e: e.tensor_tensor(out=prod[:, bi, :], in0=sig[:, bi, :], in1=PSB[b1][:], op=ALU.mult),
                                                 reads=[K_sig[bi], PSk[b1]], writes=[K_prod[bi]])
                                            C.op("dve", lambda e: e.tensor_tensor(out=mp, in0=mp, in1=prod[:, bi, :], op=ALU.add),
                                                 reads=[K_prod[bi], MPk[ft][g]], writes=[MPk[ft][g]])
                        C.barrier()
                    with ExitStack() as ph2:
                        wo = sb("wo", [128, 8, D], BF16, ph2)
                        K_wo = Tk()
                        wload(wo[:], K_wo, dr["w_out"][l])
                        tmp = sb("otmp", [128, 2, 512], F32, ph2)
                        K_tmp = tks(2)
                        it = 0
                        for tt in range(NT):
                            for hf in range(2):
                                pb, bi = 4 + it % 4, it % 2
                                it += 1
                                for ft in range(8):
                                    C.op("pe", lambda e: e.matmul(PSB[pb][:], MP[:, ft, tt * 128:(tt + 1) * 128], wo[:, ft, hf * 512:(hf + 1) * 512],
                                                                  start=(ft == 0), stop=(ft == 7)),
                                         reads=[K_wo, MPk[ft][tt // 4]], writes=[PSk[pb]], inc=(ft == 7))
                                C.op("dve", lambda e: e.tensor_tensor(out=tmp[:, bi, :], in0=PSB[pb][:], in1=G12[:, 0, hf * 512:(hf + 1) * 512], op=ALU.mult),
                                     reads=[PSk[pb], K_G[0]], writes=[K_tmp[bi]])
                                C.op("pool", lambda e: e.tensor_tensor(out=X[:, tt, hf * 512:(hf + 1) * 512], in0=X[:, tt, hf * 512:(hf + 1) * 512],
                                                                       in1=tmp[:, bi, :], op=ALU.add),
                                     reads=[K_tmp[bi], XT[tt]], writes=[XT[tt]])
                        C.barrier()
            tap(f"X1_{l}", X[:], XT, [128, NT, D])
            if stop_after == f"mix{l}":
                break

            norm_to_HT(2)
            with ExitStack() as ph:
                comb = sb("comb", [128, NT, NE], F32, ph)
                K_comb = tks(NT, "comb")
                bguT = sb("bguT", [128, NE * 16], F32, ph)
                K_bgu = Tk()
                with ExitStack() as ph2:
                    wr = sb("wr", [128, 8, NE], BF16, ph2)
                    brt = sb("brt", [128, NE], F32, ph2)
                    bd = sb("bd", [NE, D], F32, ph2)
                    K_r = Tk()
                    C.dma("pool", wr[:], dr["w_router"][l].rearrange("(kt p) e -> p kt e", p=128), writes=[K_r])
                    C.dma("sp", brt[:], dr["b_router"][l].partition_broadcast(128), writes=[K_r])
                    C.dma("sp", bd[:], dr["b_down"][l], writes=[K_r])
                    rows = sb("bgrows", [128, 4, 128], F32, ph2)
                    K_rows = Tk()
                    C.dma("sp", rows[:], dr["b_gate_up"][l].rearrange("e (j p) -> (e j) p", p=128).rearrange("(r q) p -> q r p", q=128), writes=[K_rows])
                    for r_ in range(4):
                        C.op("pe", lambda e: e.transpose(PSB[0][:, r_ * 128:(r_ + 1) * 128], rows[:, r_, :], ident_f[:]),
                             reads=[K_rows, K_id], writes=[PSk[0]], inc=(r_ == 3))
                    C.op("dve", lambda e: e.tensor_copy(out=bguT[:], in_=PSB[0][:]), reads=[PSk[0]], writes=[K_bgu])
                    bv = bguT[:].rearrange("p (e j) -> p e j", j=16)
                    C.op("dve", lambda e: e.tensor_scalar(out=bv[:, :, 8:16], in0=bv[:, :, 8:16], scalar1=1.0, scalar2=None, op0=ALU.add),
                         reads=[K_bgu], writes=[K_bgu])
                    lg = sb("lg", [128, 2, NE], F32, ph2)
                    m8r = sb("m8r", [128, 2, 8], F32, ph2)
                    sel = sb("rsel", [128, 2, NE], F32, ph2)
                    sm = sb("rsm", [128, 2, 2], F32, ph2)
                    cT = sb("rcT", [NE, 2, 128], F32, ph2)
                    tmpb = sb("rtmp", [128, 2, 512], F32, ph2)
                    K_lg, K_m8r, K_sel, K_sm, K_cT, K_tmpb = tks(2), tks(2), tks(2), tks(2), tks(2), tks(2)
                    it = 0
                    for tt in range(NT):
                        p = tt % 2
                        for kt in range(8):
                            C.op("pe", lambda e: e.matmul(PSB[1][:, 0:NE], HT[:, kt, tt * 128:(tt + 1) * 128], wr[:, kt, :], start=(kt == 0), stop=(kt == 7)),
                                 reads=[K_r] + HTr(tt // 4), writes=[PSk[1]], inc=(kt == 7))
                        C.op("dve", lambda e: e.tensor_tensor(out=lg[:, p, :], in0=PSB[1][:, 0:NE], in1=brt[:], op=ALU.add), reads=[PSk[1], K_r], writes=[K_lg[p]])
                        C.op("dve", lambda e: e.max(out=m8r[:, p, :], in_=lg[:, p, :]), reads=[K_lg[p]], writes=[K_m8r[p]])
                        C.op("dve", lambda e: e.tensor_scalar(out=sel[:, p, :], in0=lg[:, p, :], scalar1=m8r[:, p, 3:4], scalar2=None, op0=ALU.is_ge),
                             reads=[K_lg[p], K_m8r[p]], writes=[K_sel[p]])
                        C.op("dve", lambda e: e.tensor_scalar(out=sm[:, p, 0:1], in0=m8r[:, p, 0:1], scalar1=-1.0, scalar2=None, op0=ALU.mult),
                             reads=[K_m8r[p]], writes=[K_sm[p]])
                        C.op("act", lambda e: e.activation(out=lg[:, p, :], in_=lg[:, p, :], func=AF.Exp, bias=sm[:, p, 0:1]), reads=[K_lg[p], K_sm[p]], writes=[K_lg[p]])
                        C.op("dve", lambda e: e.tensor_tensor(out=sel[:, p, :], in0=sel[:, p, :], in1=lg[:, p, :], op=ALU.mult), reads=[K_lg[p], K_sel[p]], writes=[K_sel[p]])
                        C.op("dve", lambda e: e.reduce_sum(out=sm[:, p, 1:2], in_=sel[:, p, :], axis=AX.X), reads=[K_sel[p]], writes=[K_sm[p]])
                        C.op("dve", lambda e: e.reciprocal(out=sm[:, p, 1:2], in_=sm[:, p, 1:2]), reads=[K_sm[p]], writes=[K_sm[p]])
                        C.op("dve", lambda e: e.tensor_scalar(out=comb[:, tt, :], in0=sel[:, p, :], scalar1=sm[:, p, 1:2], scalar2=None, op0=ALU.mult),
                             reads=[K_sel[p], K_sm[p]], writes=[K_comb[tt]])
                        C.op("pe", lambda e: e.transpose(PSB[2][0:NE, 0:128], comb[:, tt, :], ident_f[:]), reads=[K_comb[tt], K_id], writes=[PSk[2]])
                        C.op("act", lambda e: e.copy(out=cT[:, p, :], in_=PSB[2][0:NE, 0:128]), reads=[PSk[2]], writes=[K_cT[p]])
                        for hf in range(2):
                            pb, bi = 4 + it % 4, it % 2
                            it += 1
                            C.op("pe", lambda e: e.matmul(PSB[pb][:], cT[:, p, :], bd[:, hf * 512:(hf + 1) * 512], start=True, stop=True),
                                 reads=[K_cT[p], K_r], writes=[PSk[pb]])
                            C.op("dve", lambda e: e.tensor_tensor(out=tmpb[:, bi, :], in0=PSB[pb][:], in1=G12[:, 1, hf * 512:(hf + 1) * 512], op=ALU.mult),
                                 reads=[PSk[pb], K_G[1]], writes=[K_tmpb[bi]])
                            C.op("pool", lambda e: e.tensor_tensor(out=X[:, tt, hf * 512:(hf + 1) * 512], in0=X[:, tt, hf * 512:(hf + 1) * 512],
                                                                   in1=tmpb[:, bi, :], op=ALU.add),
                                 reads=[K_tmpb[bi], XT[tt]], writes=[XT[tt]])
                    C.barrier()
                tap(f"comb{l}", comb[:], K_comb, [128, NT, NE])
                if stop_after == f"router{l}":
                    break
                RING = 4
                ring = sb("ring", [128, RING, 8, 512], BF16, ph)
                K_ring = tks(RING, "ring")
                actT = sb("actT", [128, 8, S], BF16, ph)
                K_act = [tks(4, f"act{f}_") for f in range(8)]
                xg = sb("xg", [128, 2, 512], F32, ph)
                sg = sb("sg", [128, 2, 512], BF16, ph)
                Al = sb("Al", [128, 2, 512], F32, ph)
                tg_ = sb("tg", [128, 2, 512], F32, ph)
                yt_ = sb("ytmp", [128, 2, 512], F32, ph)
                K_xg, K_sg, K_Al, K_tg, K_ytmp = tks(2), tks(2), tks(2), tks(2), tks(2)
                n_exp = NE if stop_after != f"moe1e{l}" else 1
                chunks = []
                for e_ in range(n_exp):
                    wgu = dr["w_gate_up"][l][e_]
                    wdn = dr["w_down"][l][e_]
                    chunks += [wgu[:, 0:512], wgu[:, 1024:1536], wgu[:, 512:1024], wgu[:, 1536:2048], wdn[:, 0:512], wdn[:, 512:1024]]
                loaded = [0]

                def prefetch(upto):
                    while loaded[0] < min(upto, len(chunks)):
                        i = loaded[0]
                        wload(ring[:, i % RING, :, :], K_ring[i % RING], chunks[i])
                        loaded[0] += 1
                prefetch(RING - 1)
                ci_ = 0
                gi = 0
                yi = 0
                for e_ in range(n_exp):
                    for c in range(2):
                        sl_g, sl_l = ci_ % RING, (ci_ + 1) % RING
                        prefetch(ci_ + RING)
                        for g in range(4):
                            for i in range(4):
                                ft = c * 4 + i
                                pg, pl, bi = (gi % 2) * 2, (gi % 2) * 2 + 1, gi % 2
                                gi += 1
                                for kt in range(8):
                                    C.op("pe", lambda e: e.matmul(PSB[pg][:], ring[:, sl_g, kt, i * 128:(i + 1) * 128], HT[:, kt, g * 512:(g + 1) * 512],
                                                                  start=(kt == 0), stop=(kt == 7)),
                                         reads=[K_ring[sl_g]] + HTr(g), writes=[PSk[pg]], inc=(kt == 7))
                                for kt in range(8):
                                    C.op("pe", lambda e: e.matmul(PSB[pl][:], ring[:, sl_l, kt, i * 128:(i + 1) * 128], HT[:, kt, g * 512:(g + 1) * 512],
                                                                  start=(kt == 0), stop=(kt == 7)),
                                         reads=[K_ring[sl_l]] + HTr(g), writes=[PSk[pl]], inc=(kt == 7))
                                cg = e_ * 16 + ft
                                C.op("dve", lambda e: e.tensor_scalar(out=xg[:, bi, :], in0=PSB[pg][:], scalar1=bguT[:, cg:cg + 1], scalar2=7.0, op0=ALU.add, op1=ALU.min),
                                     reads=[PSk[pg], K_bgu], writes=[K_xg[bi]])
                                C.op("act", lambda e: e.activation(out=sg[:, bi, :], in_=xg[:, bi, :], func=AF.Sigmoid, scale=1.702), reads=[K_xg[bi]], writes=[K_sg[bi]])
                                C.op("dve", lambda e: e.tensor_scalar(out=Al[:, bi, :], in0=PSB[pl][:], scalar1=bguT[:, cg + 8:cg + 9], scalar2=8.0, op0=ALU.add, op1=ALU.min),
                                     reads=[PSk[pl], K_bgu], writes=[K_Al[bi]])
                                C.op("dve", lambda e: e.tensor_tensor(out=tg_[:, bi, :], in0=xg[:, bi, :], in1=sg[:, bi, :], op=ALU.mult),
                                     reads=[K_xg[bi], K_sg[bi]], writes=[K_tg[bi]])
                                C.op("dve", lambda e: e.scalar_tensor_tensor(out=actT[:, ft, g * 512:(g + 1) * 512], in0=Al[:, bi, :], scalar=-6.0, in1=tg_[:, bi, :],
                                                                             op0=ALU.max, op1=ALU.mult),
                                     reads=[K_Al[bi], K_tg[bi]], writes=[K_act[ft][g]])
                        ci_ += 2
                    for hf in range(2):
                        sl_d = ci_ % RING
                        prefetch(ci_ + RING)
                        for tt in range(NT):
                            pb, bi = 4 + yi % 4, yi % 2
                            yi += 1
                            for ft in range(8):
                                C.op("pe", lambda e: e.matmul(PSB[pb][:], actT[:, ft, tt * 128:(tt + 1) * 128], ring[:, sl_d, ft, :], start=(ft == 0), stop=(ft == 7)),
                                     reads=[K_ring[sl_d], K_act[ft][tt // 4]], writes=[PSk[pb]], inc=(ft == 7))
                            C.op("act", lambda e: e.activation(out=yt_[:, bi, :], in_=PSB[pb][:], func=AF.Identity, scale=comb[:, tt, e_:e_ + 1]),
                                 reads=[PSk[pb], K_comb[tt]], writes=[K_ytmp[bi]])
                            C.op("pool", lambda e: e.tensor_tensor(out=yt_[:, bi, :], in0=yt_[:, bi, :], in1=G12[:, 1, hf * 512:(hf + 1) * 512], op=ALU.mult),
                                 reads=[K_ytmp[bi], K_G[1]], writes=[K_ytmp[bi]])
                            C.op("pool", lambda e: e.tensor_tensor(out=X[:, tt, hf * 512:(hf + 1) * 512], in0=X[:, tt, hf * 512:(hf + 1) * 512],
                                                                   in1=yt_[:, bi, :], op=ALU.add),
                                 reads=[K_ytmp[bi], XT[tt]], writes=[XT[tt]])
                        ci_ += 1
                C.barrier()
            tap(f"X2_{l}", X[:], XT, [128, NT, D])
            if stop_after == f"moe{l}" or stop_after == f"moe1e{l}":
                break

        if stop_after is None:
            with ExitStack() as ph:
                gf = sb("gf", [128, D], F32, ph)
                K_gf = Tk()
                C.dma("sp", gf[:], dr["g_final"][0].partition_broadcast(128), writes=[K_gf])
                ot = sb("ot", [128, 2, D], F32, ph)
                K_ot = tks(2)
                ov_ = out_d.rearrange("(t p) d -> p t d", p=128)
                for tt in range(NT):
                    p = tt % 2
                    g = tt // 4
                    C.op("act", lambda e: e.activation(out=junk[:, p, :], in_=X[:, tt, :], func=AF.Square, accum_out=ss[:, tt:tt + 1]),
                         reads=[XT[tt]], writes=[K_junk[p], K_ss[g]])
                    C.op("dve", lambda e: e.tensor_scalar(out=rstd[:, tt:tt + 1], in0=ss[:, tt:tt + 1], scalar1=1.0 / D, scalar2=EPS, op0=ALU.mult, op1=ALU.add),
                         reads=[K_ss[g]], writes=[K_rstd[g]])
                    C.op("act", lambda e: e.activation(out=rstd[:, tt:tt + 1], in_=rstd[:, tt:tt + 1], func=AF.Sqrt), reads=[K_rstd[g]], writes=[K_rstd[g]])
                    C.op("dve", lambda e: e.reciprocal(out=rstd[:, tt:tt + 1], in_=rstd[:, tt:tt + 1]), reads=[K_rstd[g]], writes=[K_rstd[g]])
                    C.op("dve", lambda e: e.scalar_tensor_tensor(out=ot[:, p, :], in0=X[:, tt, :], scalar=rstd[:, tt:tt + 1], in1=gf[:], op0=ALU.mult, op1=ALU.mult),
                         reads=[XT[tt], K_rstd[g], K_gf], writes=[K_ot[p]])
                    C.dma("sp", ov_[:, tt, :], ot[:, p, :], reads=[K_ot[p]])
        C.barrier()
    return nc, list(tap_d.keys())


def kernel(**inputs):
    n = 8
    nc, _ = build()
    consts = make_consts()
    shared = {}
    for k in IN_SPECS:
        if k in ("x", "c"):
            continue
        a = np.ascontiguousarray(np.asarray(inputs[k], dtype=np.float32))
        if k == "g_final":
            a = a.reshape(1, D)
        shared[k] = a
    for k, v in consts.items():
        shared["k_" + k] = v
    x = np.asarray(inputs["x"], dtype=np.float32)
    c = np.asarray(inputs["c"], dtype=np.float32)
    in_maps = []
    for b in range(n):
        m = dict(shared)
        m["x"] = np.ascontiguousarray(x[b])
        m["c"] = np.ascontiguousarray(c[b:b + 1])
        in_maps.append(m)
    res = run_bass_kernel_spmd(nc, in_maps, core_ids=list(range(n)))
    return np.stack([np.asarray(r["out"], dtype=np.float32) for r in res.results], axis=0)
```

```python
import numpy as np
import ml_dtypes
from contextlib import ExitStack
import concourse.bass as bass
import concourse.mybir as mybir
from concourse.bass_utils import run_bass_kernel_spmd

F32 = mybir.dt.float32
BF16 = mybir.dt.bfloat16
AF = mybir.ActivationFunctionType
ALU = mybir.AluOpType
AX = mybir.AxisListType

D = 1024
S = 2048
NT = 16
DEPTH = 2
NE = 32
EPS = 1e-5
D_IN = 7184
O_MQ, O_MK, O_MV = 0, 256, 512
O_GQ, O_GK, O_GV, O_GA, O_GR = 768, 896, 1024, 1280, 1296
O_RQ, O_RK, O_RV, O_RG = 1552, 1808, 2064, 2320
O_SQ, O_SK, O_SV = 2576, 2832, 2960
O_MG = 3088
NEG = -30000.0
SKIP = {"ret"}
RETCUT = 99
RETTILES = 16
RETV = 0


class Tk:
    __slots__ = ("w", "r", "name")

    def __init__(self, name=""):
        self.w = None
        self.r = {}
        self.name = name


def tks(n, name=""):
    return [Tk(f"{name}{i}") for i in range(n)]


class Ctx:
    ENG = ("pe", "act", "dve", "pool", "sp")

    def __init__(self, nc, es, n_dsem=24):
        self.nc = nc
        self.E = {"pe": nc.tensor, "act": nc.scalar, "dve": nc.vector, "pool": nc.gpsimd, "sp": nc.sync}
        self.sem = {e: es.enter_context(nc.semaphore("s_" + e)) for e in self.ENG}
        self.cnt = {e: 0 for e in self.ENG}
        self.seen = {e: {} for e in self.ENG}
        self.dsem = [es.enter_context(nc.semaphore(f"s_d{i}")) for i in range(n_dsem)]
        self.dcnt = [0] * n_dsem
        half = n_dsem // 2
        self.dpool = {"sp": list(range(half)), "pool": list(range(half, n_dsem))}
        self.dnext = {"sp": 0, "pool": 0}
        self.nins = 0

    def _need(self, eng, reads, writes):
        need = {}

        def add(key, val):
            if need.get(key, 0) < val:
                need[key] = val

        for t in reads:
            if t.w is not None:
                for k, v in t.w.items():
                    add(k, v)
        for t in writes:
            if t.w is not None:
                for k, v in t.w.items():
                    add(k, v)
            for k, v in t.r.items():
                add(k, v)
        for key, val in need.items():
            if key == ("e", "pe") and eng == "pe":
                continue
            if self.seen[eng].get(key, 0) >= val:
                continue
            sem = self.sem[key[1]] if key[0] == "e" else self.dsem[key[1]]
            self.E[eng].wait_ge(sem, val)
            self.seen[eng][key] = val
            self.nins += 1

    def _mark(self, key, val, reads, writes):
        for t in reads:
            if t.r.get(key, 0) < val:
                t.r[key] = val
        for t in writes:
            if key[0] == "d" and t.w is not None:
                t.w[key] = val
            else:
                t.w = {key: val}
            t.r = {}

    def op(self, eng, fn, reads=(), writes=(), inc=True):
        self._need(eng, reads, writes)
        ins = fn(self.E[eng])
        self.nins += 1
        if inc:
            ins.then_inc(self.sem[eng], 1)
            self.cnt[eng] += 1
            val = self.cnt[eng]
        else:
            assert eng == "pe"
            val = self.cnt[eng] + 1
        self._mark(("e", eng), val, reads, writes)
        return ins

    def dma(self, q, out, in_, reads=(), writes=(), **kw):
        self._need(q, reads, writes)
        i = self.dpool[q][self.dnext[q]]
        self.dnext[q] = (self.dnext[q] + 1) % len(self.dpool[q])
        key = ("d", i)
        if self.dcnt[i] > 0 and self.seen[q].get(key, 0) < self.dcnt[i] * 16:
            self.E[q].wait_ge(self.dsem[i], self.dcnt[i] * 16)
            self.seen[q][key] = self.dcnt[i] * 16
        ins = self.E[q].dma_start(out=out, in_=in_, **kw)
        ins.then_inc(self.dsem[i], 16)
        self.nins += 1
        self.dcnt[i] += 1
        self._mark(key, self.dcnt[i] * 16, reads, writes)
        return ins

    def barrier(self):
        for i in range(len(self.dsem)):
            if self.dcnt[i] > 0 and self.seen["sp"].get(("d", i), 0) < self.dcnt[i] * 16:
                self.E["sp"].wait_ge(self.dsem[i], self.dcnt[i] * 16)
                self.seen["sp"][("d", i)] = self.dcnt[i] * 16
        ins = self.E["sp"].nop() if False else None
        for e in self.ENG:
            for f in self.ENG:
                key = ("e", f)
                if self.cnt[f] > 0 and self.seen[e].get(key, 0) < self.cnt[f]:
                    self.E[e].wait_ge(self.sem[f], self.cnt[f])
                    self.seen[e][key] = self.cnt[f]
            for i in range(len(self.dsem)):
                if self.dcnt[i] > 0 and self.seen[e].get(("d", i), 0) < self.dcnt[i] * 16:
                    self.E[e].wait_ge(self.dsem[i], self.dcnt[i] * 16)
                    self.seen[e][("d", i)] = self.dcnt[i] * 16


def make_consts():
    bf = ml_dtypes.bfloat16
    c = {}
    c["ident_bf"] = np.eye(128, dtype=np.float32).astype(bf)
    c["ident_f"] = np.eye(128, dtype=np.float32)
    k = np.arange(128)[:, None]
    q = np.arange(128)[None, :]
    c["tri_bf"] = (k <= q).astype(np.float32).astype(bf)
    slopes = 2.0 ** (-(np.arange(8, dtype=np.float64) + 1.0))
    swa = np.zeros((128, 4, 2, 128), np.float64)
    for h in range(4):
        sl = slopes[h]
        dist_prev = 128 + q - k
        swa[:, h, 0, :] = np.where(k > q, np.exp(-sl * dist_prev), 0.0)
        dist_own = q - k
        swa[:, h, 1, :] = np.where(k <= q, np.exp(-sl * dist_own), 0.0)
    c["swa_mask"] = swa.astype(np.float32).astype(bf)
    t = np.arange(S)
    a = (t // 128).astype(np.float64)
    r = (t % 128).astype(np.float64)
    qa = np.zeros((4, 12, S), np.float64)
    ka = np.zeros((4, 12, S), np.float64)
    for h in range(4):
        sl = slopes[4 + h] * 8.0
        for j in range(8):
            ka[h, j] = (t // 256 == j)
        qa[h, 8] = -sl * 128.0 * a
        ka[h, 8] = 1.0
        qa[h, 9] = -sl * r
        ka[h, 9] = 1.0
        qa[h, 10] = 1.0
        ka[h, 10] = sl * 128.0 * a
        qa[h, 11] = 1.0
        ka[h, 11] = sl * r
    c["moba_qa"] = qa.astype(np.float32).astype(bf)
    c["moba_ka"] = ka.astype(np.float32).astype(bf)
    m = np.arange(128)[:, None]
    l = np.arange(128)[None, :]
    c["cum_incl"] = ((m <= l) * (-1.0 / 16.0)).astype(np.float32)
    c["cum_after"] = ((m > l) * (-1.0 / 16.0)).astype(np.float32)
    gam = 1.0 - 2.0 ** (-5.0 - np.arange(4, dtype=np.float64))
    pos = np.arange(128, dtype=np.float64)
    rq = np.zeros((128, 2, 128)); rk = np.zeros((128, 2, 128))
    rke = np.zeros((128, 256)); rdec = np.zeros((128, 2))
    for h in range(4):
        j, o = h // 2, (h % 2) * 64
        rq[o:o + 64, j, :] = gam[h] ** (pos + 1.0)
        rk[o:o + 64, j, :] = gam[h] ** (-(pos + 1.0)) * 64 ** -0.5
        rke[:, h * 64:(h + 1) * 64] = (gam[h] ** (127.0 - pos))[:, None] * 64 ** -0.5
        rdec[o:o + 64, j] = gam[h] ** 128.0
    tt_ = np.arange(16)[:, None]; nn_ = np.arange(8)[None, :]
    c["moba_past"] = np.where(nn_ < tt_ // 2, 0.0, -1e30).astype(np.float32).reshape(1, 128)
    c["moba_notown"] = np.where(nn_ == tt_ // 2, 0.0, 1.0).astype(np.float32).reshape(1, 128)
    bd = np.zeros((128, 256), np.float32)
    for h in range(4):
        bd[32 * h:32 * h + 32, 64 * h:64 * h + 64] = 1.0
    c["bd_gla"] = bd
    bd = np.zeros((128, 128), np.float32)
    for h in range(2):
        bd[64 * h:64 * h + 64, 64 * h:64 * h + 64] = 1.0
    c["bd_ret"] = bd
    tpos = np.arange(S, dtype=np.float64) - 1024.0
    c["ret_qd"] = np.stack([gam[h] ** tpos for h in range(4)]).astype(np.float32)
    c["ret_kd"] = np.stack([gam[h] ** (-tpos) for h in range(4)]).astype(np.float32)
    c["ret_q"] = rq.astype(np.float32)
    c["ret_k"] = rk.astype(np.float32)
    c["ret_kend"] = rke.astype(np.float32)
    c["ret_dec"] = rdec.astype(np.float32)
    return c


CONST_SPECS = {
    "ident_bf": ([128, 128], BF16), "ident_f": ([128, 128], F32), "tri_bf": ([128, 128], BF16),
    "swa_mask": ([128, 4, 2, 128], BF16), "moba_qa": ([4, 12, S], BF16), "moba_ka": ([4, 12, S], BF16),
    "cum_incl": ([128, 128], F32), "cum_after": ([128, 128], F32),
    "ret_q": ([128, 2, 128], F32), "ret_k": ([128, 2, 128], F32), "ret_kend": ([128, 256], F32),
    "ret_dec": ([128, 2], F32), "moba_past": ([1, 128], F32), "ret_qd": ([4, S], F32), "ret_kd": ([4, S], F32), "bd_gla": ([128, 256], F32), "bd_ret": ([128, 128], F32), "moba_notown": ([1, 128], F32),
}

IN_SPECS = {
    "x": [S, D], "c": [1, D], "w_ada": [DEPTH, D, 6 * D], "b_ada": [DEPTH, 6 * D], "g_norm_mix": [DEPTH, D],
    "w_in": [DEPTH, D, D_IN], "w_gla_gate": [DEPTH, 16, 128], "b_gla_gate": [DEPTH, 128],
    "g_gla_norm": [DEPTH, 256], "g_ret_norm": [DEPTH, 256], "attn_sinks": [DEPTH, 4],
    "w_branch": [DEPTH, 4, 256, D], "w_out": [DEPTH, D, D], "g_norm_ffn": [DEPTH, D],
    "w_router": [DEPTH, D, NE], "b_router": [DEPTH, NE], "w_gate_up": [DEPTH, NE, D, 2 * D],
    "b_gate_up": [DEPTH, NE, 2 * D], "w_down": [DEPTH, NE, D, D], "b_down": [DEPTH, NE, D],
    "g_final": [1, D],
}


def build(n_layers=DEPTH, stop_after=None, taps=()):
    nc = bass.Bass("TRN2", target_bir_lowering=False)
    small = stop_after is not None and not stop_after.startswith(("moe", "router"))
    dr = {k: nc.dram_tensor(k, shp, F32, kind="ExternalInput").ap() for k, shp in IN_SPECS.items()
          if not (small and k in ("w_gate_up", "w_down"))}
    cst = {k: nc.dram_tensor("k_" + k, shp, dt, kind="ExternalInput").ap() for k, (shp, dt) in CONST_SPECS.items()}
    out_d = nc.dram_tensor("out", [S, D], F32, kind="ExternalOutput").ap()
    tap_d = {}
    es = ExitStack()
    with es:
        C = Ctx(nc, es)

        uniq = [0]

        def sb(name, shape, dt, stack=es):
            uniq[0] += 1
            return stack.enter_context(nc.sbuf_tensor(f"{name}_{uniq[0]}", shape, dt))

        X = sb("X", [128, NT, D], F32)
        XT = tks(NT, "X")
        HT = sb("HT", [128, 8, S], BF16)
        HTk = [[Tk(f"HT{f}_{g}") for g in range(4)] for f in range(8)]
        ident_bf = sb("ident_bf", [128, 128], BF16)
        ident_f = sb("ident_f", [128, 128], F32)
        K_id = Tk("ident")
        PSB = [es.enter_context(nc.psum_tensor(f"ps{i}", [128, 512], F32)) for i in range(8)]
        PSk = tks(8, "ps")
        colsT = sb("colsT", [128, 64], F32)
        K_cols = Tk("colsT")
        modT = sb("modT", [128, 48], F32)
        K_mod = Tk("modT")
        AB = sb("AB", [128, 4, 8], F32)
        K_AB = Tk("AB")
        G12 = sb("G12", [128, 2, D], F32)
        K_G = tks(2, "G")
        cactT = sb("cactT", [128, 8], BF16)
        cactB = sb("cactB", [128, 8, 128], BF16)
        K_cact = Tk("cact")
        ss = sb("ss", [128, NT], F32)
        rstd = sb("rstd", [128, NT], F32)
        K_ss = tks(4, "ss")
        K_rstd = tks(4, "rstd")
        junk = sb("junk", [128, 2, D], BF16)
        K_junk = tks(2, "junk")

        def tap(name, ap, reads, shape, dt=F32):
            if name not in taps:
                return
            d = nc.dram_tensor("tap_" + name, list(shape), dt, kind="ExternalOutput").ap()
            tap_d[name] = d
            C.dma("sp", d, ap, reads=reads)

        def wload(dst, dst_tk, src2d, q="pool"):
            n = src2d.shape[1]
            sv = src2d.rearrange("(kt p) c -> p kt c", p=128)
            for c0 in range(0, n, 512):
                c1 = min(n, c0 + 512)
                C.dma(q, dst[:, :, c0:c1], sv[:, :, c0:c1], writes=[dst_tk])

        def ps_bf(i):
            return PSB[i][:].bitcast(BF16)

        C.dma("sp", ident_bf[:], cst["ident_bf"], writes=[K_id])
        C.dma("sp", ident_f[:], cst["ident_f"], writes=[K_id])
        xv = dr["x"].rearrange("(t p) d -> p t d", p=128)
        for g in range(4):
            C.dma("sp", X[:, 4 * g:4 * g + 4, :], xv[:, 4 * g:4 * g + 4, :], writes=XT[4 * g:4 * g + 4])

        with ExitStack() as ph:
            crow = sb("crow", [8, 128], F32, ph)
            K_crow = Tk()
            ccol = sb("ccol", [128, 8], F32, ph)
            K_ccol = Tk()
            C.dma("sp", crow[:], dr["c"].rearrange("o (kt p) -> (o kt) p", p=128), writes=[K_crow])
            C.op("pe", lambda e: e.transpose(PSB[0][:, 0:8], crow[:], ident_f[0:8, 0:8]),
                 reads=[K_crow, K_id], writes=[PSk[0]])
            C.op("act", lambda e: e.activation(out=ccol[:], in_=PSB[0][:, 0:8], func=AF.Silu),
                 reads=[PSk[0]], writes=[K_ccol])
            C.op("dve", lambda e: e.tensor_copy(out=cactT[:], in_=ccol[:]), reads=[K_ccol], writes=[K_cact])
            C.op("dve", lambda e: e.tensor_copy(out=cactB[:], in_=ccol[:].unsqueeze(2).to_broadcast([128, 8, 128])),
                 reads=[K_ccol], writes=[K_cact])
            C.barrier()

        def adaln(l):
            with ExitStack() as ph:
                rows = sb("rows", [64, 128], F32, ph)
                K_rows = Tk()
                C.dma("sp", rows[0:48, :], dr["b_ada"][l].rearrange("(r p) -> r p", p=128), writes=[K_rows])
                C.dma("sp", rows[48:56, :], dr["g_norm_mix"][l].rearrange("(r p) -> r p", p=128), writes=[K_rows])
                C.dma("sp", rows[56:64, :], dr["g_norm_ffn"][l].rearrange("(r p) -> r p", p=128), writes=[K_rows])
                C.op("pe", lambda e: e.transpose(PSB[0][:, 0:64], rows[:], ident_f[0:64, 0:64]),
                     reads=[K_rows, K_id], writes=[PSk[0]])
                C.op("dve", lambda e: e.tensor_copy(out=colsT[:], in_=PSB[0][:, 0:64]), reads=[PSk[0]], writes=[K_cols])
                wa = [sb(f"wa{i}", [128, 8, 512], BF16, ph) for i in range(2)]
                K_wa = tks(2, "wa")
                bbc = sb("bbc", [128, D], F32, ph)
                K_bbc = Tk()
                ci = 0
                for j in range(6):
                    for half in range(2):
                        w = ci % 2
                        ci += 1
                        c0 = j * D + half * 512
                        wload(wa[w][:], K_wa[w], dr["w_ada"][l][:, c0:c0 + 512])
                        if j in (2, 5):
                            gi = 0 if j == 2 else 1
                            pb = 2 + half
                            for kt in range(8):
                                C.op("pe", lambda e: e.matmul(PSB[pb][:], cactB[:, kt, :], wa[w][:, kt, :],
                                                              start=(kt == 0), stop=(kt == 7)),
                                     reads=[K_cact, K_wa[w]], writes=[PSk[pb]], inc=(kt == 7))
                            if half == 0:
                                C.dma("sp", bbc[:], dr["b_ada"][l][j * D:(j + 1) * D].partition_broadcast(128),
                                      writes=[K_bbc])
                            C.op("dve", lambda e: e.tensor_tensor(out=G12[:, gi, half * 512:(half + 1) * 512],
                                                                  in0=PSB[pb][:], in1=bbc[:, half * 512:(half + 1) * 512],
                                                                  op=ALU.add),
                                 reads=[PSk[pb], K_bbc], writes=[K_G[gi]])
                        else:
                            for fl in range(4):
                                col = j * 8 + half * 4 + fl
                                for kt in range(8):
                                    C.op("pe", lambda e: e.matmul(PSB[1][:, col:col + 1], wa[w][:, kt, fl * 128:(fl + 1) * 128],
                                                                  cactT[:, kt:kt + 1], start=(kt == 0), stop=(kt == 7)),
                                         reads=[K_cact, K_wa[w]], writes=[PSk[1]], inc=(kt == 7))
                for j in (0, 1, 3, 4):
                    C.op("dve", lambda e: e.tensor_tensor(out=modT[:, j * 8:(j + 1) * 8], in0=PSB[1][:, j * 8:(j + 1) * 8],
                                                          in1=colsT[:, j * 8:(j + 1) * 8], op=ALU.add),
                         reads=[PSk[1], K_cols], writes=[K_mod])
                C.op("dve", lambda e: e.scalar_tensor_tensor(out=AB[:, 0, :], in0=modT[:, 8:16], scalar=1.0,
                                                             in1=colsT[:, 48:56], op0=ALU.add, op1=ALU.mult),
                     reads=[K_mod, K_cols], writes=[K_AB])
                C.op("dve", lambda e: e.tensor_copy(out=AB[:, 1, :], in_=modT[:, 0:8]), reads=[K_mod], writes=[K_AB])
                C.op("dve", lambda e: e.scalar_tensor_tensor(out=AB[:, 2, :], in0=modT[:, 32:40], scalar=1.0,
                                                             in1=colsT[:, 56:64], op0=ALU.add, op1=ALU.mult),
                     reads=[K_mod, K_cols], writes=[K_AB])
                C.op("dve", lambda e: e.tensor_copy(out=AB[:, 3, :], in_=modT[:, 24:32]), reads=[K_mod], writes=[K_AB])
                C.barrier()

        def norm_to_HT(ai):
            with ExitStack() as ph:
                xn = sb("xn", [128, 4, D], BF16, ph)
                K_xn = tks(4, "xn")
                ev = 0
                for g in range(4):
                    for i in range(4):
                        tt = 4 * g + i
                        C.op("act", lambda e: e.activation(out=junk[:, i % 2, :], in_=X[:, tt, :], func=AF.Square,
                                                           accum_out=ss[:, tt:tt + 1]),
                             reads=[XT[tt]], writes=[K_junk[i % 2], K_ss[g]])
                    C.op("dve", lambda e: e.tensor_scalar(out=rstd[:, 4 * g:4 * g + 4], in0=ss[:, 4 * g:4 * g + 4],
                                                          scalar1=1.0 / D, scalar2=EPS, op0=ALU.mult, op1=ALU.add),
                         reads=[K_ss[g]], writes=[K_rstd[g]])
                    C.op("act", lambda e: e.activation(out=rstd[:, 4 * g:4 * g + 4], in_=rstd[:, 4 * g:4 * g + 4], func=AF.Sqrt),
                         reads=[K_rstd[g]], writes=[K_rstd[g]])
                    C.op("dve", lambda e: e.reciprocal(out=rstd[:, 4 * g:4 * g + 4], in_=rstd[:, 4 * g:4 * g + 4]),
                         reads=[K_rstd[g]], writes=[K_rstd[g]])
                    for i in range(4):
                        tt = 4 * g + i
                        C.op("dve", lambda e: e.tensor_scalar(out=xn[:, i, :], in0=X[:, tt, :], scalar1=rstd[:, tt:tt + 1],
                                                              scalar2=None, op0=ALU.mult),
                             reads=[XT[tt], K_rstd[g]], writes=[K_xn[i]])
                    for ft in range(8):
                        pb = ft % 4
                        for i in range(4):
                            C.op("pe", lambda e: e.transpose(ps_bf(pb)[:, i * 128:(i + 1) * 128],
                                                             xn[:, i, ft * 128:(ft + 1) * 128], ident_bf[:]),
                                 reads=[K_xn[i], K_id], writes=[PSk[pb]], inc=(i == 3))
                        dst = HT[:, ft, g * 512:(g + 1) * 512]
                        if ev % 2 == 0:
                            C.op("act", lambda e: e.activation(out=dst, in_=ps_bf(pb)[:, 0:512], func=AF.Identity,
                                                               bias=AB[:, ai + 1, ft:ft + 1], scale=AB[:, ai, ft:ft + 1]),
                                 reads=[PSk[pb], K_AB], writes=[HTk[ft][g]])
                        else:
                            C.op("dve", lambda e: e.tensor_scalar(out=dst, in0=ps_bf(pb)[:, 0:512],
                                                                  scalar1=AB[:, ai, ft:ft + 1], scalar2=AB[:, ai + 1, ft:ft + 1],
                                                                  op0=ALU.mult, op1=ALU.add),
                                 reads=[PSk[pb], K_AB], writes=[HTk[ft][g]])
                        ev += 1
                C.barrier()

        for l in range(n_layers):
            adaln(l)
            tap(f"modT{l}", modT[:], [K_mod], [128, 48])
            tap(f"G{l}", G12[:], K_G, [128, 2, D])
            norm_to_HT(0)
            tap(f"HT{l}", HT[:], [t for r in HTk for t in r], [128, 8, S], BF16)
            if stop_after == f"norm{l}":
                break

            with ExitStack() as mx:
                YT = sb("YT", [128, 8, S], BF16, mx)
                YTk = [tks(NT, f"YT{c}_") for c in range(8)]
                tri = sb("tri", [128, 128], BF16, mx)
                K_tri = Tk()
                C.dma("sp", tri[:], cst["tri_bf"], writes=[K_tri])
                ytile = sb("ytile", [128, 2, 256], BF16, mx)
                K_yt = tks(2, "yt")
                WIN = dr["w_in"][l]

                def emit_y(n, tt, par):
                    pb = 7
                    for ci in range(2):
                        C.op("pe", lambda e: e.transpose(ps_bf(pb)[:, ci * 128:(ci + 1) * 128], ytile[:, par, ci * 128:(ci + 1) * 128], ident_bf[:]),
                             reads=[K_yt[par], K_id], writes=[PSk[pb]], inc=(ci == 1))
                    C.op("act", lambda e: e.copy(out=YT[:, 2 * n:2 * n + 2, tt * 128:(tt + 1) * 128],
                                                 in_=ps_bf(pb)[:, 0:256].rearrange("p (c x) -> p c x", c=2)),
                         reads=[PSk[pb]], writes=[YTk[2 * n][tt], YTk[2 * n + 1][tt]])

                def HTr(g):
                    return [HTk[f][g] for f in range(8)]

                def proj_fm(dst_fn, w, K_w, c0, M, evi=[0]):
                    for g in range(4):
                        pb = evi[0] % 2
                        for kt in range(8):
                            C.op("pe", lambda e: e.matmul(PSB[pb][0:M, :], w[:, kt, c0:c0 + M], HT[:, kt, g * 512:(g + 1) * 512],
                                                          start=(kt == 0), stop=(kt == 7)),
                                 reads=[K_w] + HTr(g), writes=[PSk[pb]], inc=(kt == 7))
                        dst, K_dst = dst_fn(g)
                        if evi[0] % 2 == 0:
                            C.op("act", lambda e: e.copy(out=dst, in_=PSB[pb][0:M, :]), reads=[PSk[pb]], writes=[K_dst])
                        else:
                            C.op("dve", lambda e: e.tensor_copy(out=dst, in_=PSB[pb][0:M, :]), reads=[PSk[pb]], writes=[K_dst])
                        evi[0] += 1

                def proj_tm(dst_fn, w, K_w, c0, N, evi=[0]):
                    for tt in range(NT):
                        pb = 2 + evi[0] % 2
                        evi[0] += 1
                        for kt in range(8):
                            C.op("pe", lambda e: e.matmul(PSB[pb][:, 0:N], HT[:, kt, tt * 128:(tt + 1) * 128], w[:, kt, c0:c0 + N],
                                                          start=(kt == 0), stop=(kt == 7)),
                                 reads=[K_w] + HTr(tt // 4), writes=[PSk[pb]], inc=(kt == 7))
                        dst_fn(tt, pb)

                if "swa" not in SKIP:
                    with ExitStack() as ph:
                        QS = [sb(f"QS{h}", [64, S], BF16, ph) for h in range(4)]
                        K_QS = [tks(4, f"QS{h}_") for h in range(4)]
                        KS = [sb(f"KS{g}", [64, S], BF16, ph) for g in range(2)]
                        K_KS = [tks(4, f"KS{g}_") for g in range(2)]
                        VS = sb("VS", [128, NT, 2, 65], BF16, ph)
                        K_VS = tks(NT, "VS")
                        wq = sb("swq", [128, 8, 256], BF16, ph)
                        wkv = sb("swkv", [128, 8, 256], BF16, ph)
                        K_wq, K_wkv = Tk(), Tk()
                        msk = sb("smask", [128, 4, 2, 128], BF16, ph)
                        K_msk = Tk()
                        esink = sb("esink", [128, 4], F32, ph)
                        K_es = Tk()
                        wload(wq[:], K_wq, WIN[:, O_SQ:O_SQ + 256])
                        wload(wkv[:], K_wkv, WIN[:, O_SK:O_SK + 256])
                        C.dma("sp", msk[:], cst["swa_mask"], writes=[K_msk])
                        C.dma("sp", esink[:], dr["attn_sinks"][l].partition_broadcast(128), writes=[K_es])
                        C.op("act", lambda e: e.activation(out=esink[:], in_=esink[:], func=AF.Exp), reads=[K_es], writes=[K_es])
                        C.op("dve", lambda e: e.memset(VS[:, :, :, 64:65], 1.0), writes=K_VS)
                        for h in range(4):
                            proj_fm(lambda g: (QS[h][:, g * 512:(g + 1) * 512], K_QS[h][g]), wq, K_wq, h * 64, 64)
                        for g2 in range(2):
                            proj_fm(lambda g: (KS[g2][:, g * 512:(g + 1) * 512], K_KS[g2][g]), wkv, K_wkv, g2 * 64, 64)

                        def v_ev(tt, pb):
                            C.op("act", lambda e: e.copy(out=VS[:, tt, :, 0:64], in_=PSB[pb][:, 0:128].rearrange("p (g d) -> p g d", g=2)),
                                 reads=[PSk[pb]], writes=[K_VS[tt]])
                        proj_tm(v_ev, wkv, K_wkv, 128, 128)
                        EX = sb("sEX", [128, 2, 512], BF16, ph)
                        PT = sb("sPT", [128, 2, 512], BF16, ph)
                        K_EX, K_PT = tks(2), tks(2)
                        den = sb("sden", [128, 2, 4], F32, ph)
                        K_den = tks(2)
                        it = 0
                        for tt in range(NT):
                            par = tt % 2
                            po = 4 + par
                            for g2 in range(2):
                                pb = it % 2
                                bi = it % 2
                                it += 1
                                pvs = (1,) if tt == 0 else (0, 1)
                                for hh in range(2):
                                    for pv in pvs:
                                        kt = tt - 1 + pv
                                        o = (hh * 2 + pv) * 128
                                        C.op("pe", lambda e: e.matmul(PSB[pb][:, o:o + 128], KS[g2][:, kt * 128:(kt + 1) * 128],
                                                                      QS[2 * g2 + hh][:, tt * 128:(tt + 1) * 128], start=True, stop=True),
                                             reads=[K_KS[g2][kt // 4], K_QS[2 * g2 + hh][tt // 4]], writes=[PSk[pb]],
                                             inc=(hh == 1 and pv == 1))
                                if tt == 0:
                                    exv = EX[:, bi, :].rearrange("p (a b c) -> p a b c", a=2, b=2)[:, :, 1, :]
                                    psv = PSB[pb][:].rearrange("p (a b c) -> p a b c", a=2, b=2)[:, :, 1, :]
                                    ptv = PT[:, bi, :].rearrange("p (a b c) -> p a b c", a=2, b=2)[:, :, 1, :]
                                    C.op("act", lambda e: e.activation(out=exv, in_=psv, func=AF.Exp, scale=0.125),
                                         reads=[PSk[pb]], writes=[K_EX[bi]])
                                    C.op("dve", lambda e: e.tensor_tensor(out=ptv, in0=exv, in1=msk[:, 2 * g2:2 * g2 + 2, 1, :], op=ALU.mult),
                                         reads=[K_EX[bi], K_msk], writes=[K_PT[bi]])
                                else:
                                    C.op("act", lambda e: e.activation(out=EX[:, bi, :], in_=PSB[pb][:], func=AF.Exp, scale=0.125),
                                         reads=[PSk[pb]], writes=[K_EX[bi]])
                                    C.op("dve", lambda e: e.tensor_tensor(out=PT[:, bi, :], in0=EX[:, bi, :],
                                                                          in1=msk[:, 2 * g2:2 * g2 + 2, :, :].rearrange("p a b c -> p (a b c)"),
                                                                          op=ALU.mult),
                                         reads=[K_EX[bi], K_msk], writes=[K_PT[bi]])
                                for hh in range(2):
                                    h = 2 * g2 + hh
                                    for pv in pvs:
                                        kt = tt - 1 + pv
                                        o = (hh * 2 + pv) * 128
                                        C.op("pe", lambda e: e.matmul(PSB[po][:, h * 65:(h + 1) * 65], PT[:, bi, o:o + 128], VS[:, kt, g2, :],
                                                                      start=(pv == pvs[0]), stop=(pv == 1)),
                                             reads=[K_PT[bi], K_VS[kt]], writes=[PSk[po]], inc=(pv == 1))
                            pov = PSB[po][:, 0:260].rearrange("p (h d) -> p h d", h=4)
                            C.op("dve", lambda e: e.tensor_tensor(out=den[:, par, :], in0=pov[:, :, 64], in1=esink[:], op=ALU.add),
                                 reads=[PSk[po], K_es], writes=[K_den[par]])
                            C.op("dve", lambda e: e.reciprocal(out=den[:, par, :], in_=den[:, par, :]), reads=[K_den[par]], writes=[K_den[par]])
                            C.op("dve", lambda e: e.tensor_tensor(out=ytile[:, par, :].rearrange("p (h d) -> p h d", h=4), in0=pov[:, :, 0:64],
                                                                  in1=den[:, par, :].unsqueeze(2).to_broadcast([128, 4, 64]), op=ALU.mult),
                                 reads=[PSk[po], K_den[par]], writes=[K_yt[par]])
                            emit_y(3, tt, par)
                        C.barrier()
                if stop_after == f"swa{l}":
                    tap(f"YT{l}", YT[:], [t for r in YTk for t in r], [128, 8, S], BF16)
                    break

                if "moba" not in SKIP:
                    with ExitStack() as ph:
                        QA = [sb(f"QA{h}", [76, S], BF16, ph) for h in range(4)]
                        K_QA = [tks(4, f"QA{h}_") for h in range(4)]
                        K_QAs = [tks(4, f"QAs{h}_") for h in range(4)]
                        KA = [sb(f"KA{h}", [76, S], BF16, ph) for h in range(4)]
                        K_KA = [tks(4, f"KA{h}_") for h in range(4)]
                        VM = sb("VM", [128, NT, 4, 65], BF16, ph)
                        K_VM = tks(NT, "VM")
                        past = sb("mpast", [128, 128], F32, ph)
                        notown = sb("mnotown", [128, 128], F32, ph)
                        phA = ExitStack()
                        wq = sb("mwq", [128, 8, 256], BF16, phA)
                        wk = sb("mwk", [128, 8, 256], BF16, phA)
                        wv = sb("mwv", [128, 8, 256], BF16, phA)
                        K_wq, K_wk, K_wv = Tk(), Tk(), Tk()
                        wload(wq[:], K_wq, WIN[:, O_MQ:O_MQ + 256])
                        wload(wk[:], K_wk, WIN[:, O_MK:O_MK + 256])
                        wload(wv[:], K_wv, WIN[:, O_MV:O_MV + 256])
                        K_aug = Tk()
                        for h in range(4):
                            C.dma("sp", QA[h][64:76, :], cst["moba_qa"][h], writes=[K_aug])
                            C.dma("sp", KA[h][64:76, :], cst["moba_ka"][h], writes=[K_aug])
                        K_pm = Tk()
                        C.dma("sp", past[:], cst["moba_past"][0].partition_broadcast(128), writes=[K_pm])
                        C.dma("sp", notown[:], cst["moba_notown"][0].partition_broadcast(128), writes=[K_pm])
                        C.op("dve", lambda e: e.memset(VM[:, :, :, 64:65], 1.0), writes=K_VM)
                        for h in range(4):
                            proj_fm(lambda g: (QA[h][0:64, g * 512:(g + 1) * 512], K_QA[h][g]), wq, K_wq, h * 64, 64)
                            proj_fm(lambda g: (KA[h][0:64, g * 512:(g + 1) * 512], K_KA[h][g]), wk, K_wk, h * 64, 64)

                        def vm_ev(tt, pb):
                            C.op("act", lambda e: e.copy(out=VM[:, tt, :, 0:64], in_=PSB[pb][:, 0:256].rearrange("p (g d) -> p g d", g=4)),
                                 reads=[PSk[pb]], writes=[K_VM[tt]])
                        proj_tm(vm_ev, wv, K_wv, 0, 256)
                        C.barrier()
                        phA.close()
                        phB = ExitStack()
                        kms = sb("kms", [64, 4, 8], F32, phB)
                        kmb = sb("kmb", [64, 4, 8], BF16, phB)
                        K_km = Tk()
                        for h in range(4):
                            C.op("dve", lambda e: e.tensor_reduce(out=kms[:, h, :], in_=KA[h][0:64, :].rearrange("p (n s) -> p n s", n=8),
                                                                  axis=AX.X, op=ALU.add),
                                 reads=K_KA[h], writes=[K_km])
                        C.op("dve", lambda e: e.tensor_copy(out=kmb[:], in_=kms[:]), reads=[K_km], writes=[K_km])
                        for h in range(4):
                            for tt in range(NT):
                                o = (h * NT + tt) * 8
                                C.op("pe", lambda e: e.matmul(PSB[0][:, o:o + 8], QA[h][0:64, tt * 128:(tt + 1) * 128], kmb[:, h, :],
                                                              start=True, stop=True),
                                     reads=[K_QA[h][tt // 4], K_km], writes=[PSk[0]], inc=(h == 3 and tt == NT - 1))
                        gm = sb("mgm", [128, 4, 128], F32, phB)
                        m8 = sb("mm8", [128, 64, 8], F32, phB)
                        selb = sb("mselb", [128, 4, 128], F32, phB)
                        SP = sb("mSP", [128, 64, 72], BF16, phB)
                        K_gm, K_m8, K_selb, K_SP = Tk(), Tk(), Tk(), Tk()
                        C.op("dve", lambda e: e.tensor_tensor(out=gm[:], in0=PSB[0][:].rearrange("p (h x) -> p h x", h=4),
                                                              in1=past[:].unsqueeze(1).to_broadcast([128, 4, 128]), op=ALU.add),
                             reads=[PSk[0], K_pm], writes=[K_gm])
                        gmv = gm[:].rearrange("p h (t n) -> p (h t) n", n=8)
                        for gi in range(64):
                            C.op("dve", lambda e: e.max(out=m8[:, gi, :], in_=gmv[:, gi, :]), reads=[K_gm], writes=[K_m8])
                        C.op("dve", lambda e: e.tensor_tensor(out=selb[:].rearrange("p h (t n) -> p (h t) n", n=8), in0=gmv,
                                                              in1=m8[:, :, 2:3].to_broadcast([128, 64, 8]), op=ALU.is_ge),
                             reads=[K_gm, K_m8], writes=[K_selb])
                        C.op("dve", lambda e: e.tensor_scalar(out=selb[:], in0=selb[:], scalar1=-1.0, scalar2=-NEG, op0=ALU.add, op1=ALU.mult),
                             reads=[K_selb], writes=[K_selb])
                        C.op("dve", lambda e: e.tensor_tensor(out=selb[:], in0=selb[:], in1=notown[:].unsqueeze(1).to_broadcast([128, 4, 128]),
                                                              op=ALU.mult),
                             reads=[K_selb, K_pm], writes=[K_selb])
                        C.op("dve", lambda e: e.memset(SP[:], 0.0), writes=[K_SP])
                        C.op("dve", lambda e: e.tensor_copy(out=SP[:, :, 64:72], in_=selb[:].rearrange("p h (t n) -> p (h t) n", n=8)),
                             reads=[K_selb], writes=[K_SP])
                        for h in range(4):
                            for g in range(4):
                                pb = 1 + (h * 4 + g) % 2
                                for i in range(4):
                                    tt = 4 * g + i
                                    C.op("pe", lambda e: e.matmul(PSB[pb][0:72, i * 128:(i + 1) * 128], SP[:, h * NT + tt, :], ident_bf[:],
                                                                  start=True, stop=True),
                                         reads=[K_SP, K_id], writes=[PSk[pb]], inc=(i == 3))
                                C.op("act", lambda e: e.copy(out=QA[h][64:72, g * 512:(g + 1) * 512], in_=PSB[pb][64:72, :]),
                                     reads=[PSk[pb], K_aug], writes=[K_QAs[h][g]])
                        C.barrier()
                        phB.close()
                        PTp = sb("mPTp", [128, 2, 8, 512], BF16, ph)
                        PTd = sb("mPTd", [128, 2, 384], BF16, ph)
                        K_PTp = [tks(8), tks(8)]
                        K_PTd = tks(2)
                        rd = sb("mrd", [128, 2, 4], F32, ph)
                        K_rd = tks(2)
                        it = 0
                        sc = 0
                        for b in range(8):
                            for h in range(4):
                                bi = it % 2
                                it += 1
                                qrd = [K_QA[h][b // 2], K_QAs[h][b // 2]]
                                for pr in range(b):
                                    pb = sc % 2
                                    sc += 1
                                    for j in range(2):
                                        kt = 2 * pr + j
                                        C.op("pe", lambda e: e.matmul(PSB[pb][:, j * 256:(j + 1) * 256], KA[h][:, kt * 128:(kt + 1) * 128],
                                                                      QA[h][:, b * 256:(b + 1) * 256], start=True, stop=True),
                                             reads=[K_KA[h][kt // 4], K_aug] + qrd, writes=[PSk[pb]], inc=(j == 1))
                                    C.op("act", lambda e: e.activation(out=PTp[:, bi, pr, :], in_=PSB[pb][:], func=AF.Exp, scale=0.125),
                                         reads=[PSk[pb]], writes=[K_PTp[bi][pr]])
                                pb = sc % 2
                                sc += 1
                                kt = 2 * b
                                C.op("pe", lambda e: e.matmul(PSB[pb][:, 0:256], KA[h][:, kt * 128:(kt + 1) * 128],
                                                              QA[h][:, b * 256:(b + 1) * 256], start=True, stop=True),
                                     reads=[K_KA[h][kt // 4], K_aug] + qrd, writes=[PSk[pb]], inc=False)
                                kt = 2 * b + 1
                                C.op("pe", lambda e: e.matmul(PSB[pb][:, 256:384], KA[h][:, kt * 128:(kt + 1) * 128],
                                                              QA[h][:, b * 256 + 128:(b + 1) * 256], start=True, stop=True),
                                     reads=[K_KA[h][kt // 4], K_aug] + qrd, writes=[PSk[pb]])
                                C.op("act", lambda e: e.activation(out=PTd[:, bi, :], in_=PSB[pb][:, 0:384], func=AF.Exp, scale=0.125),
                                     reads=[PSk[pb]], writes=[K_PTd[bi]])
                                C.op("dve", lambda e: e.tensor_tensor(out=PTd[:, bi, 0:128], in0=PTd[:, bi, 0:128], in1=tri[:], op=ALU.mult),
                                     reads=[K_PTd[bi], K_tri], writes=[K_PTd[bi]])
                                C.op("dve", lambda e: e.tensor_tensor(out=PTd[:, bi, 256:384], in0=PTd[:, bi, 256:384], in1=tri[:], op=ALU.mult),
                                     reads=[K_PTd[bi], K_tri], writes=[K_PTd[bi]])
                                for qi in range(2):
                                    po = 4 + qi
                                    for pr in range(b):
                                        for j in range(2):
                                            kt = 2 * pr + j
                                            C.op("pe", lambda e: e.matmul(PSB[po][:, h * 65:(h + 1) * 65],
                                                                          PTp[:, bi, pr, j * 256 + qi * 128:j * 256 + (qi + 1) * 128],
                                                                          VM[:, kt, h, :], start=(kt == 0), stop=False),
                                                 reads=[K_PTp[bi][pr], K_VM[kt]], writes=[PSk[po]], inc=False)
                                    C.op("pe", lambda e: e.matmul(PSB[po][:, h * 65:(h + 1) * 65], PTd[:, bi, qi * 128:(qi + 1) * 128],
                                                                  VM[:, 2 * b, h, :], start=(b == 0), stop=(qi == 0)),
                                         reads=[K_PTd[bi], K_VM[2 * b]], writes=[PSk[po]], inc=(qi == 0))
                                    if qi == 1:
                                        C.op("pe", lambda e: e.matmul(PSB[po][:, h * 65:(h + 1) * 65], PTd[:, bi, 256:384],
                                                                      VM[:, 2 * b + 1, h, :], start=False, stop=True),
                                             reads=[K_PTd[bi], K_VM[2 * b + 1]], writes=[PSk[po]])
                            for qi in range(2):
                                tt = 2 * b + qi
                                po = 4 + qi
                                par = qi
                                pov = PSB[po][:, 0:260].rearrange("p (h d) -> p h d", h=4)
                                C.op("dve", lambda e: e.reciprocal(out=rd[:, par, :], in_=pov[:, :, 64]), reads=[PSk[po]], writes=[K_rd[par]])
                                C.op("dve", lambda e: e.tensor_tensor(out=ytile[:, par, :].rearrange("p (h d) -> p h d", h=4), in0=pov[:, :, 0:64],
                                                                      in1=rd[:, par, :].unsqueeze(2).to_broadcast([128, 4, 64]), op=ALU.mult),
                                     reads=[PSk[po], K_rd[par]], writes=[K_yt[par]])
                                emit_y(0, tt, par)
                        C.barrier()
                if stop_after == f"moba{l}":
                    tap(f"YT{l}", YT[:], [t for r in YTk for t in r], [128, 8, S], BF16)
                    break

                for br in (("ret",) if stop_after in (f"retonly{l}", f"retall{l}") else tuple(b_ for b_ in ("gla", "ret") if b_ not in SKIP)):
                    with ExitStack() as ph:
                        gla = br == "gla"
                        ncol = 784 if gla else 1024
                        wg = sb("lw", [128, 8, ncol], BF16, ph)
                        K_wg = Tk()
                        wload(wg[:], K_wg, WIN[:, (O_GQ if gla else O_RQ):(O_GQ if gla else O_RQ) + ncol])
                        gbc = sb("lgbc", [128, 256], F32, ph)
                        K_gbc = Tk()
                        C.dma("sp", gbc[:], dr["g_gla_norm" if gla else "g_ret_norm"][l].partition_broadcast(128), writes=[K_gbc])
                        K_cn = Tk()
                        if gla:
                            wgg = sb("lwgg", [32, 128], BF16, ph)
                            C.dma("pool", wgg[0:16, :], dr["w_gla_gate"][l], writes=[K_cn])
                            C.dma("pool", wgg[16:17, :], dr["b_gla_gate"][l:l + 1, :], writes=[K_cn])
                            cinc = sb("lcinc", [128, 128], F32, ph)
                            caft = sb("lcaft", [128, 128], F32, ph)
                            C.dma("sp", cinc[:], cst["cum_incl"], writes=[K_cn])
                            C.dma("sp", caft[:], cst["cum_after"], writes=[K_cn])
                            gaT = sb("lgaT", [32, 2, 128], BF16, ph)
                            K_gaT = tks(2)
                            C.op("dve", lambda e: e.memset(gaT[:], 1.0), writes=K_gaT)
                            LA = sb("lLA", [128, 2, 128], F32, ph)
                            K_LA = tks(2)
                            EB = sb("lEB", [128, 2, 3, 128], F32, ph)
                            K_EB = tks(2)
                            dec = sb("ldec", [128, 2], F32, ph)
                            nft = 1
                            kd = 32
                        else:
                            rqc = sb("lrqc", [128, 2, 128], F32, ph)
                            rkc = sb("lrkc", [128, 2, 128], F32, ph)
                            rkend = sb("lrkend", [128, 256], F32, ph)
                            rdec = sb("lrdec", [128, 2], F32, ph)
                            C.dma("sp", rqc[:], cst["ret_q"], writes=[K_cn])
                            C.dma("sp", rkc[:], cst["ret_k"], writes=[K_cn])
                            C.dma("sp", rkend[:], cst["ret_kend"], writes=[K_cn])
                            K_rd_ = Tk()
                            for h_ in range(4):
                                gv_ = float((1.0 - 2.0 ** (-5.0 - h_)) ** 128.0)
                                o_ = (h_ % 2) * 64
                                C.op("dve", lambda e: e.memset(rdec[o_:o_ + 64, h_ // 2:h_ // 2 + 1], gv_), writes=[K_rd_])
                            nft = 2
                            kd = 64
                        qd = sb("lqd", [128, 2, nft, 128], BF16, ph)
                        kin = sb("lkin", [128, 2, 4, 128], BF16, ph)
                        K_kin = tks(2)
                        C.op("dve", lambda e: e.memset(kin[:], 0.0), writes=K_kin)
                        SW = 256 if gla else 128
                        bdm = sb("lbdm", [128, SW], F32, ph)
                        C.dma("sp", bdm[:], cst["bd_gla" if gla else "bd_ret"], writes=[K_cn])
                        kvt = sb("lkvt", [128, 2, SW], F32, ph)
                        K_kvt = tks(2)
                        kend = sb("lkend", [128, 2, nft * 128], BF16, ph)
                        vv = sb("lvv", [128, 2, 256], BF16, ph)
                        ggr = sb("lggr", [128, 2, 256], F32, ph)
                        atm = sb("latm", [128, 2, 4, 128], BF16, ph)
                        K_qd, K_kend, K_vv, K_ggr, K_atm = tks(2), tks(2), tks(2), tks(2), tks(2)
                        Sf = sb("lSf", [128, 2, nft, SW], F32, ph)
                        Sb = sb("lSb", [128, 2, nft, SW], BF16, ph)
                        K_Sf, K_Sb = tks(2), tks(2)
                        C.op("dve", lambda e: e.memset(Sf[:], 0.0), writes=K_Sf)
                        sq = sb("lsq", [128, 2, 256], F32, ph)
                        st = sb("lst", [128, 2, 3, 4], F32, ph)
                        K_sq, K_st = tks(2), tks(2)
                        t1 = sb("lt1", [128, 2, 256], F32, ph)
                        K_t1 = tks(2)
                        for tt in range((NT if gla else RETTILES) if stop_after != f"retonly{l}" else 1):
                            if RETV in (32, 33) and tt > 0 and not gla:
                                C.barrier()
                            _rc = RETCUT
                            if RETV in (20, 22, 32, 30):
                                _rc = 3 if tt == 0 else 2
                            if RETV == 21:
                                _rc = 3 if tt == 0 else 1
                            p = tt % 2 if RETV != 4 else 0
                            q = 1 - p
                            hr = HTr(tt // 4)
                            tok = slice(tt * 128, (tt + 1) * 128)

                            def mmK(out, lhs, rhs, wr, inc8=True):
                                for kt in range(8):
                                    C.op("pe", lambda e: e.matmul(out, lhs(kt), rhs(kt), start=(kt == 0), stop=(kt == 7)),
                                         reads=[K_wg] + hr, writes=[wr], inc=(kt == 7))
                            for j in range(nft):
                                mmK(PSB[0][:, j * 128:(j + 1) * 128], lambda kt: wg[:, kt, j * 128:(j + 1) * 128], lambda kt: HT[:, kt, tok], PSk[0])
                                ko = 128 if gla else 256
                                mmK(PSB[0][:, (nft + j) * 128:(nft + j + 1) * 128], lambda kt: wg[:, kt, ko + j * 128:ko + (j + 1) * 128],
                                    lambda kt: HT[:, kt, tok], PSk[0])
                            if gla:
                                mmK(PSB[1][0:16, 0:128], lambda kt: wg[:, kt, 512:528], lambda kt: HT[:, kt, tok], PSk[1])
                                C.op("act", lambda e: e.copy(out=gaT[0:16, p, :], in_=PSB[1][0:16, 0:128]), reads=[PSk[1]], writes=[K_gaT[p]])
                                C.op("pe", lambda e: e.matmul(PSB[1][:, 128:256], gaT[0:17, p, :], wgg[0:17, :], start=True, stop=True),
                                     reads=[K_gaT[p], K_cn], writes=[PSk[1]])
                                C.op("act", lambda e: e.activation(out=LA[:, p, :], in_=PSB[1][:, 128:256], func=AF.Exp, scale=-1.0),
                                     reads=[PSk[1]], writes=[K_LA[p]])
                                C.op("act", lambda e: e.activation(out=LA[:, p, :], in_=LA[:, p, :], func=AF.Ln, bias=1.0),
                                     reads=[K_LA[p]], writes=[K_LA[p]])
                                C.op("pe", lambda e: e.matmul(PSB[1][:, 256:384], LA[:, p, :], cinc[:], start=True, stop=True),
                                     reads=[K_LA[p], K_cn], writes=[PSk[1]])
                                C.op("pe", lambda e: e.matmul(PSB[1][:, 384:512], caft[:], LA[:, p, :], start=True, stop=True),
                                     reads=[K_LA[p], K_cn], writes=[PSk[1]])
                                C.op("act", lambda e: e.activation(out=EB[:, p, 0, :], in_=PSB[1][:, 256:384], func=AF.Exp), reads=[PSk[1]], writes=[K_EB[p]])
                                C.op("act", lambda e: e.activation(out=EB[:, p, 1, :], in_=PSB[1][:, 256:384], func=AF.Exp, scale=-1.0), reads=[PSk[1]], writes=[K_EB[p]])
                                C.op("act", lambda e: e.activation(out=EB[:, p, 2, :], in_=PSB[1][:, 384:512], func=AF.Exp), reads=[PSk[1]], writes=[K_EB[p]])
                                C.op("dve", lambda e: e.scalar_tensor_tensor(out=qd[:, p, 0, :], in0=PSB[0][:, 0:128], scalar=32.0 ** -0.5, in1=EB[:, p, 0, :],
                                                                             op0=ALU.mult, op1=ALU.mult), reads=[PSk[0], K_EB[p]], writes=[K_qd[p]])
                                for h in range(4):
                                    o = 32 * h
                                    C.op("dve", lambda e: e.tensor_tensor(out=kin[o:o + 32, p, h, :], in0=PSB[0][o:o + 32, 128:256], in1=EB[o:o + 32, p, 1, :], op=ALU.mult),
                                         reads=[PSk[0], K_EB[p]], writes=[K_kin[p]])
                            else:
                                if RETV in (10, 12):
                                    mmK(PSB[1][0:16, 256:384], lambda kt: wg[:, kt, 0:16], lambda kt: HT[:, kt, tok], PSk[1])
                                if RETV in (11, 12):
                                    C.op("pe", lambda e: e.matmul(PSB[1][:, 384:512], rqc[:, 0, :], rkc[:, 0, :], start=True, stop=True),
                                         reads=[K_cn], writes=[PSk[1]])
                                C.op("dve", lambda e: e.tensor_tensor(out=qd[:, p, :, :], in0=PSB[0][:, 0:256].rearrange("p (j x) -> p j x", j=2),
                                                                      in1=rqc[:], op=ALU.mult), reads=[PSk[0], K_cn], writes=[K_qd[p]])
                                for h in range(4):
                                    j, o = h // 2, (h % 2) * 64
                                    C.op("dve", lambda e: e.tensor_tensor(out=kin[o:o + 64, p, h, :], in0=PSB[0][o:o + 64, 256 + j * 128:256 + (j + 1) * 128],
                                                                          in1=rkc[o:o + 64, j, :], op=ALU.mult), reads=[PSk[0], K_cn], writes=[K_kin[p]])
                            if _rc <= 1:
                                continue
                            nk = nft * 128
                            ko = 128 if gla else 256
                            if RETV == 30:
                                mmK(PSB[2][:, 0:nk], lambda kt: HT[:, kt, tok], lambda kt: wg[:, kt, ko:ko + nk], PSk[2])
                                mmK(PSB[2][:, nk:nk + 256], lambda kt: HT[:, kt, tok], lambda kt: wg[:, kt, ko + nk:ko + nk + 256], PSk[2])
                            else:
                                mmK(PSB[2][:, 0:nk + 256], lambda kt: HT[:, kt, tok], lambda kt: wg[:, kt, ko:ko + nk + 256], PSk[2])
                            go = 528 if gla else 768
                            mmK(PSB[3][:, 0:256], lambda kt: HT[:, kt, tok], lambda kt: wg[:, kt, go:go + 256], PSk[3])
                            if gla:
                                C.op("dve", lambda e: e.tensor_tensor(out=kend[:, p, :], in0=PSB[2][:, 0:128], in1=EB[:, p, 2, :], op=ALU.mult),
                                     reads=[PSk[2], K_EB[p]], writes=[K_kend[p]])
                            else:
                                C.op("dve", lambda e: e.tensor_tensor(out=kend[:, p, :], in0=PSB[2][:, 0:256], in1=rkend[:], op=ALU.mult),
                                     reads=[PSk[2], K_cn], writes=[K_kend[p]])
                            C.op("act", lambda e: e.copy(out=vv[:, p, :], in_=PSB[2][:, nk:nk + 256]), reads=[PSk[2]], writes=[K_vv[p]])
                            C.op("act", lambda e: e.activation(out=ggr[:, p, :], in_=PSB[3][:, 0:256], func=AF.Silu), reads=[PSk[3]], writes=[K_ggr[p]])
                            C.op("dve", lambda e: e.tensor_tensor(out=ggr[:, p, :], in0=ggr[:, p, :], in1=gbc[:], op=ALU.mult),
                                 reads=[K_ggr[p], K_gbc], writes=[K_ggr[p]])
                            if _rc <= 2:
                                continue
                            for h in range(4 if not (RETV == 9 and tt == 1) else 0):
                                j = 0 if gla else h // 2
                                C.op("pe", lambda e: e.matmul(PSB[4][:, h * 128:(h + 1) * 128], kin[:, p, h, :], qd[:, p, j, :],
                                                              start=True, stop=True),
                                     reads=[K_kin[p], K_qd[p]], writes=[PSk[4]], inc=(h == 3))
                            if not (RETV in (9, 13) and tt == 1):
                                C.op("dve", lambda e: e.tensor_tensor(out=atm[:, p, :, :], in0=PSB[4][:].rearrange("p (h x) -> p h x", h=4),
                                                                      in1=tri[:].unsqueeze(1).to_broadcast([128, 4, 128]), op=ALU.mult),
                                     reads=[PSk[4], K_tri], writes=[K_atm[p]])
                            obank = [5, 6]
                            if tt > 0 and RETV not in (1, 5, 6, 7, 8):
                                for j in range(nft if RETV != 2 else 1):
                                    C.op("pe", lambda e: e.matmul(PSB[obank[j]][:, 0:SW], qd[:, p, j, :], Sb[:, q, j, :], start=True, stop=False),
                                         reads=[K_qd[p], K_Sb[q]], writes=[PSk[obank[j]]], inc=(RETV == 3))
                            for h in range(4):
                                if (tt == 1 and RETV in (5, 13)) or (tt == 0 and RETV == 22) or (tt == 1 and False) or (tt == 1 and (False or (RETV == 6 and h >= 2) or (RETV == 8 and h < 2))):
                                    continue
                                j, oc = (0, h * 64) if gla else (h // 2, (h % 2) * 64)
                                C.op("pe", lambda e: e.matmul(PSB[obank[j]][:, oc:oc + 64], atm[:, p, h, :], vv[:, p, h * 64:(h + 1) * 64],
                                                              start=(tt == 0 or RETV in (1, 5, 6, 7, 8, 9, 13) or (RETV == 2 and h >= 2)), stop=(tt == 0 or h == 3 or (not gla and h == 1))),
                                     reads=[K_atm[p], K_vv[p]], writes=[PSk[obank[j]]], inc=True)
                            if _rc <= 3:
                                continue
                            if tt < NT - 1:
                                kvb = 1 if not gla else 3
                                for j in range(nft):
                                    C.op("pe", lambda e: e.matmul(PSB[kvb][:, 256:256 + SW] if gla else PSB[kvb][:, j * 128:(j + 1) * 128],
                                                                  kend[:, p, j * 128:(j + 1) * 128], vv[:, p, j * SW:(j + 1) * SW] if not gla else vv[:, p, :],
                                                                  start=True, stop=True),
                                         reads=[K_kend[p], K_vv[p]], writes=[PSk[kvb]])
                                    src = PSB[kvb][:, 256:256 + SW] if gla else PSB[kvb][:, j * 128:(j + 1) * 128]
                                    C.op("dve", lambda e: e.tensor_tensor(out=kvt[:, j if not gla else 0, :], in0=src, in1=bdm[:], op=ALU.mult),
                                         reads=[PSk[kvb], K_cn], writes=[K_kvt[j]])
                                    if gla:
                                        C.op("act", lambda e: e.copy(out=dec[:, p:p + 1], in_=EB[:, p, 0, 127:128]), reads=[K_EB[p]], writes=[K_EB[p]])
                                        dsc = dec[:, p:p + 1]
                                    else:
                                        dsc = rdec[:, j:j + 1]
                                    C.op("dve", lambda e: e.scalar_tensor_tensor(out=Sf[:, p, j, :], in0=Sf[:, q, j, :], scalar=dsc,
                                                                                 in1=kvt[:, j if not gla else 0, :], op0=ALU.mult, op1=ALU.add),
                                         reads=[K_Sf[q], K_kvt[j], K_EB[p] if gla else K_cn], writes=[K_Sf[p]])
                                C.op("act", lambda e: e.copy(out=Sb[:, p, :, :], in_=Sf[:, p, :, :]), reads=[K_Sf[p]], writes=[K_Sb[p]])
                            if _rc <= 4:
                                continue
                            osb = t1
                            for j in range(nft):
                                C.op("act", lambda e: e.copy(out=t1[:, p, j * SW:(j + 1) * SW], in_=PSB[obank[j]][:, 0:SW]), reads=[PSk[obank[j]]], writes=[K_t1[p]])
                            ov = t1[:, p, :].rearrange("p (h d) -> p h d", h=4)
                            C.op("act", lambda e: e.activation(out=sq[:, p, :], in_=t1[:, p, :], func=AF.Square), reads=[K_t1[p]], writes=[K_sq[p]])
                            C.op("dve", lambda e: e.tensor_reduce(out=st[:, p, 0, :], in_=sq[:, p, :].rearrange("p (h d) -> p h d", h=4), axis=AX.X, op=ALU.add),
                                 reads=[K_sq[p]], writes=[K_st[p]])
                            if _rc <= 4.2:
                                continue
                            if gla:
                                C.op("dve", lambda e: e.tensor_scalar(out=st[:, p, 0, :], in0=st[:, p, 0, :], scalar1=1.0 / 64, scalar2=EPS, op0=ALU.mult, op1=ALU.add),
                                     reads=[K_st[p]], writes=[K_st[p]])
                            else:
                                C.op("dve", lambda e: e.tensor_reduce(out=st[:, p, 1, :], in_=ov, axis=AX.X, op=ALU.add), reads=[K_t1[p]], writes=[K_st[p]])
                                C.op("dve", lambda e: e.tensor_scalar(out=st[:, p, 1, :], in0=st[:, p, 1, :], scalar1=-1.0 / 64, scalar2=None, op0=ALU.mult),
                                     reads=[K_st[p]], writes=[K_st[p]])
                                C.op("dve", lambda e: e.tensor_tensor(out=st[:, p, 2, :], in0=st[:, p, 1, :], in1=st[:, p, 1, :], op=ALU.mult),
                                     reads=[K_st[p]], writes=[K_st[p]])
                                C.op("dve", lambda e: e.scalar_tensor_tensor(out=st[:, p, 0, :], in0=st[:, p, 0, :], scalar=1.0 / 64, in1=st[:, p, 2, :],
                                                                             op0=ALU.mult, op1=ALU.subtract), reads=[K_st[p]], writes=[K_st[p]])
                                C.op("dve", lambda e: e.tensor_scalar(out=st[:, p, 0, :], in0=st[:, p, 0, :], scalar1=EPS, scalar2=None, op0=ALU.add),
                                     reads=[K_st[p]], writes=[K_st[p]])
                            if _rc <= 4.4:
                                continue
                            C.op("act", lambda e: e.activation(out=st[:, p, 0, :], in_=st[:, p, 0, :], func=AF.Sqrt), reads=[K_st[p]], writes=[K_st[p]])
                            C.op("dve", lambda e: e.reciprocal(out=st[:, p, 0, :], in_=st[:, p, 0, :]), reads=[K_st[p]], writes=[K_st[p]])
                            if _rc <= 4.6:
                                continue
                            t1v = t1[:, p, :].rearrange("p (h d) -> p h d", h=4)
                            if gla:
                                C.op("dve", lambda e: e.tensor_tensor(out=t1v, in0=ov, in1=st[:, p, 0, :].unsqueeze(2).to_broadcast([128, 4, 64]), op=ALU.mult),
                                     reads=[K_t1[p], K_st[p]], writes=[K_t1[p]])
                            else:
                                C.op("dve", lambda e: e.tensor_tensor(out=t1v, in0=ov, in1=st[:, p, 1, :].unsqueeze(2).to_broadcast([128, 4, 64]), op=ALU.add),
                                     reads=[K_t1[p], K_st[p]], writes=[K_t1[p]])
                                C.op("dve", lambda e: e.tensor_tensor(out=t1v, in0=t1v, in1=st[:, p, 0, :].unsqueeze(2).to_broadcast([128, 4, 64]), op=ALU.mult),
                                     reads=[K_t1[p], K_st[p]], writes=[K_t1[p]])
                            if _rc <= 4.8:
                                continue
                            C.op("dve", lambda e: e.tensor_tensor(out=ytile[:, p, :], in0=t1[:, p, :], in1=ggr[:, p, :], op=ALU.mult),
                                 reads=[K_t1[p], K_ggr[p]], writes=[K_yt[p]])
                            if _rc <= 5:
                                continue
                            emit_y(1 if gla else 2, tt, p)
                        C.barrier()
                    if stop_after == f"{br}{l}":
                        break
                    if stop_after == f"retall{l}" and "memsetYT" not in SKIP:
                        pass
                if "retq" not in SKIP:
                    with ExitStack() as ph:
                        VR = sb("VR", [128, NT, 4, 64], BF16, ph)
                        K_VR = tks(NT, "VR")
                        GG = sb("GG", [128, NT, 256], BF16, ph)
                        K_GG = tks(NT, "GG")
                        gbc = sb("rgbc", [128, 256], F32, ph)
                        K_gbc = Tk()
                        C.dma("sp", gbc[:], dr["g_ret_norm"][l].partition_broadcast(128), writes=[K_gbc])
                        gtmp = sb("rgtmp", [128, 2, 256], F32, ph)
                        K_gtmp = tks(2)
                        PTp = sb("rPTp", [128, 8, 512], BF16, ph)
                        PTd = sb("rPTd", [128, 2, 384], BF16, ph)
                        K_PTp = tks(8)
                        K_PTd = tks(2)
                        t1 = sb("rt1", [128, 2, 128], F32, ph)
                        sq = sb("rsq", [128, 2, 128], F32, ph)
                        st = sb("rst", [128, 2, 3, 2], F32, ph)
                        K_t1, K_sq, K_st = tks(2), tks(2), tks(2)
                        QR = [sb(f"QR{i}", [64, S], BF16, ph) for i in range(2)]
                        KR = [sb(f"KR{i}", [64, S], BF16, ph) for i in range(2)]
                        K_QR = [tks(4), tks(4)]
                        K_KR = [tks(4), tks(4)]
                        with ExitStack() as phA:
                            wvg = sb("rwvg", [128, 8, 512], BF16, phA)
                            K_wvg = Tk()
                            wload(wvg[:], K_wvg, WIN[:, O_RV:O_RV + 512])

                            def vr_ev(tt, pb):
                                C.op("act", lambda e: e.copy(out=VR[:, tt, :, :], in_=PSB[pb][:, 0:256].rearrange("p (g d) -> p g d", g=4)),
                                     reads=[PSk[pb]], writes=[K_VR[tt]])
                            proj_tm(vr_ev, wvg, K_wvg, 0, 256)

                            def gg_ev(tt, pb):
                                bi = tt % 2
                                C.op("act", lambda e: e.activation(out=gtmp[:, bi, :], in_=PSB[pb][:, 0:256], func=AF.Silu), reads=[PSk[pb]], writes=[K_gtmp[bi]])
                                C.op("dve", lambda e: e.tensor_tensor(out=GG[:, tt, :], in0=gtmp[:, bi, :], in1=gbc[:], op=ALU.mult),
                                     reads=[K_gtmp[bi], K_gbc], writes=[K_GG[tt]])
                            proj_tm(gg_ev, wvg, K_wvg, 256, 256)
                            C.barrier()
                        for pair in range(2):
                            with ExitStack() as phB:
                                wqk = sb("rwqk", [128, 8, 256], BF16, phB)
                                K_wqk = Tk()
                                wload(wqk[:, :, 0:128], K_wqk, WIN[:, O_RQ + pair * 128:O_RQ + (pair + 1) * 128])
                                wload(wqk[:, :, 128:256], K_wqk, WIN[:, O_RK + pair * 128:O_RK + (pair + 1) * 128])
                                dq = sb("rdq", [64, 1, S], BF16, phB)
                                dk = sb("rdk", [64, 1, S], BF16, phB)
                                K_dqk = Tk()
                                ev = 0
                                for hh in range(2):
                                    C.dma("pool", dq[:, 0, :], cst["ret_qd"][2 * pair + hh].partition_broadcast(64), writes=[K_dqk])
                                    C.dma("pool", dk[:, 0, :], cst["ret_kd"][2 * pair + hh].partition_broadcast(64), writes=[K_dqk])
                                    for which in range(2):
                                        for g in range(4):
                                            pb = ev % 2
                                            ev += 1
                                            c0 = which * 128 + hh * 64
                                            for kt in range(8):
                                                C.op("pe", lambda e: e.matmul(PSB[pb][0:64, :], wqk[:, kt, c0:c0 + 64], HT[:, kt, g * 512:(g + 1) * 512],
                                                                              start=(kt == 0), stop=(kt == 7)),
                                                     reads=[K_wqk] + HTr(g), writes=[PSk[pb]], inc=(kt == 7))
                                            dst = (QR if which == 0 else KR)[hh][:, g * 512:(g + 1) * 512]
                                            dtk = (K_QR if which == 0 else K_KR)[hh][g]
                                            dec_ = (dq if which == 0 else dk)[:, 0, g * 512:(g + 1) * 512]
                                            C.op("dve", lambda e: e.tensor_tensor(out=dst, in0=PSB[pb][0:64, :], in1=dec_, op=ALU.mult),
                                                 reads=[PSk[pb], K_dqk], writes=[dtk])
                                C.barrier()
                            sc = 0
                            for b in range(8):
                                for hh in range(2):
                                    h = 2 * pair + hh
                                    for pr in range(b):
                                        pb = sc % 2
                                        sc += 1
                                        for j in range(2):
                                            kt = 2 * pr + j
                                            C.op("pe", lambda e: e.matmul(PSB[pb][:, j * 256:(j + 1) * 256], KR[hh][:, kt * 128:(kt + 1) * 128],
                                                                          QR[hh][:, b * 256:(b + 1) * 256], start=True, stop=True),
                                                 reads=[K_KR[hh][kt // 4], K_QR[hh][b // 2]], writes=[PSk[pb]], inc=(j == 1))
                                        C.op("act", lambda e: e.activation(out=PTp[:, pr, :], in_=PSB[pb][:], func=AF.Identity, scale=0.125),
                                             reads=[PSk[pb]], writes=[K_PTp[pr]])
                                    pb = sc % 2
                                    sc += 1
                                    bi = hh
                                    kt = 2 * b
                                    C.op("pe", lambda e: e.matmul(PSB[pb][:, 0:256], KR[hh][:, kt * 128:(kt + 1) * 128],
                                                                  QR[hh][:, b * 256:(b + 1) * 256], start=True, stop=True),
                                         reads=[K_KR[hh][kt // 4], K_QR[hh][b // 2]], writes=[PSk[pb]], inc=False)
                                    kt = 2 * b + 1
                                    C.op("pe", lambda e: e.matmul(PSB[pb][:, 256:384], KR[hh][:, kt * 128:(kt + 1) * 128],
                                                                  QR[hh][:, b * 256 + 128:(b + 1) * 256], start=True, stop=True),
                                         reads=[K_KR[hh][kt // 4], K_QR[hh][b // 2]], writes=[PSk[pb]])
                                    C.op("act", lambda e: e.activation(out=PTd[:, bi, :], in_=PSB[pb][:, 0:384], func=AF.Identity, scale=0.125),
                                         reads=[PSk[pb]], writes=[K_PTd[bi]])
                                    C.op("dve", lambda e: e.tensor_tensor(out=PTd[:, bi, 0:128], in0=PTd[:, bi, 0:128], in1=tri[:], op=ALU.mult),
                                         reads=[K_PTd[bi], K_tri], writes=[K_PTd[bi]])
                                    C.op("dve", lambda e: e.tensor_tensor(out=PTd[:, bi, 256:384], in0=PTd[:, bi, 256:384], in1=tri[:], op=ALU.mult),
                                         reads=[K_PTd[bi], K_tri], writes=[K_PTd[bi]])
                                    for qi in range(2):
                                        po = 4 + qi
                                        for pr in range(b):
                                            for j in range(2):
                                                kt = 2 * pr + j
                                                C.op("pe", lambda e: e.matmul(PSB[po][:, hh * 64:(hh + 1) * 64],
                                                                              PTp[:, pr, j * 256 + qi * 128:j * 256 + (qi + 1) * 128],
                                                                              VR[:, kt, h, :], start=(kt == 0), stop=False),
                                                     reads=[K_PTp[pr], K_VR[kt]], writes=[PSk[po]], inc=False)
                                        C.op("pe", lambda e: e.matmul(PSB[po][:, hh * 64:(hh + 1) * 64], PTd[:, bi, qi * 128:(qi + 1) * 128],
                                                                      VR[:, 2 * b, h, :], start=(b == 0), stop=(qi == 0)),
                                             reads=[K_PTd[bi], K_VR[2 * b]], writes=[PSk[po]], inc=(qi == 0))
                                        if qi == 1:
                                            C.op("pe", lambda e: e.matmul(PSB[po][:, hh * 64:(hh + 1) * 64], PTd[:, bi, 256:384],
                                                                          VR[:, 2 * b + 1, h, :], start=False, stop=True),
                                                 reads=[K_PTd[bi], K_VR[2 * b + 1]], writes=[PSk[po]])
                                for qi in range(2):
                                    tt = 2 * b + qi
                                    po = 4 + qi
                                    p = qi
                                    C.op("act", lambda e: e.copy(out=t1[:, p, :], in_=PSB[po][:, 0:128]), reads=[PSk[po]], writes=[K_t1[p]])
                                    C.op("act", lambda e: e.activation(out=sq[:, p, :], in_=t1[:, p, :], func=AF.Square), reads=[K_t1[p]], writes=[K_sq[p]])
                                    ov = t1[:, p, :].rearrange("p (h d) -> p h d", h=2)
                                    C.op("dve", lambda e: e.tensor_reduce(out=st[:, p, 0, :], in_=sq[:, p, :].rearrange("p (h d) -> p h d", h=2), axis=AX.X, op=ALU.add),
                                         reads=[K_sq[p]], writes=[K_st[p]])
                                    C.op("dve", lambda e: e.tensor_reduce(out=st[:, p, 1, :], in_=ov, axis=AX.X, op=ALU.add), reads=[K_t1[p]], writes=[K_st[p]])
                                    C.op("dve", lambda e: e.tensor_scalar(out=st[:, p, 1, :], in0=st[:, p, 1, :], scalar1=-1.0 / 64, scalar2=None, op0=ALU.mult),
                                         reads=[K_st[p]], writes=[K_st[p]])
                                    C.op("dve", lambda e: e.tensor_tensor(out=st[:, p, 2, :], in0=st[:, p, 1, :], in1=st[:, p, 1, :], op=ALU.mult),
                                         reads=[K_st[p]], writes=[K_st[p]])
                                    C.op("dve", lambda e: e.scalar_tensor_tensor(out=st[:, p, 0, :], in0=st[:, p, 0, :], scalar=1.0 / 64, in1=st[:, p, 2, :],
                                                                                 op0=ALU.mult, op1=ALU.subtract), reads=[K_st[p]], writes=[K_st[p]])
                                    C.op("dve", lambda e: e.tensor_scalar(out=st[:, p, 0, :], in0=st[:, p, 0, :], scalar1=EPS, scalar2=None, op0=ALU.add),
                                         reads=[K_st[p]], writes=[K_st[p]])
                                    C.op("act", lambda e: e.activation(out=st[:, p, 0, :], in_=st[:, p, 0, :], func=AF.Sqrt), reads=[K_st[p]], writes=[K_st[p]])
                                    C.op("dve", lambda e: e.reciprocal(out=st[:, p, 0, :], in_=st[:, p, 0, :]), reads=[K_st[p]], writes=[K_st[p]])
                                    C.op("dve", lambda e: e.tensor_tensor(out=ov, in0=ov, in1=st[:, p, 1, :].unsqueeze(2).to_broadcast([128, 2, 64]), op=ALU.add),
                                         reads=[K_t1[p], K_st[p]], writes=[K_t1[p]])
                                    C.op("dve", lambda e: e.tensor_tensor(out=ov, in0=ov, in1=st[:, p, 0, :].unsqueeze(2).to_broadcast([128, 2, 64]), op=ALU.mult),
                                         reads=[K_t1[p], K_st[p]], writes=[K_t1[p]])
                                    C.op("dve", lambda e: e.tensor_tensor(out=ytile[:, p, 0:128], in0=t1[:, p, :], in1=GG[:, tt, pair * 128:(pair + 1) * 128], op=ALU.mult),
                                         reads=[K_t1[p], K_GG[tt]], writes=[K_yt[p]])
                                    C.op("pe", lambda e: e.transpose(ps_bf(7)[:, 0:128], ytile[:, p, 0:128], ident_bf[:]),
                                         reads=[K_yt[p], K_id], writes=[PSk[7]])
                                    C.op("act", lambda e: e.copy(out=YT[:, 4 + pair, tt * 128:(tt + 1) * 128], in_=ps_bf(7)[:, 0:128]),
                                         reads=[PSk[7]], writes=[YTk[4 + pair][tt]])
                            C.barrier()
                if stop_after == f"retq{l}":
                    tap(f"YT{l}", YT[:], [t for r in YTk for t in r], [128, 8, S], BF16)
                    break
                if stop_after in (f"gla{l}", f"ret{l}", f"retonly{l}", f"retall{l}"):
                    tap(f"YT{l}", YT[:], [t for r in YTk for t in r], [128, 8, S], BF16)
                    break

                with ExitStack() as ph:
                    MP = sb("MP", [128, 8, S], BF16, ph)
                    MPk = [tks(4, f"MP{f}_") for f in range(8)]
                    with ExitStack() as ph2:
                        wm = sb("wm", [128, 2, 8, 256], BF16, ph2)
                        wbr = sb("wbr", [128, 2, 2, D], BF16, ph2)
                        K_wm, K_wbr = tks(2), tks(2)
                        sig = sb("sig", [128, 2, 512], BF16, ph2)
                        prod = sb("prod", [128, 2, 512], F32, ph2)
                        K_sig, K_prod = tks(2), tks(2)
                        ci_ = 0
                        it = 0
                        for n in range(4):
                            wb_ = n % 2
                            for hf_ in range(2):
                                C.dma("pool", wbr[:, wb_, :, hf_ * 512:(hf_ + 1) * 512],
                                      dr["w_branch"][l][n].rearrange("(ci p) d -> p ci d", p=128)[:, :, hf_ * 512:(hf_ + 1) * 512], writes=[K_wbr[wb_]])
                            for fp in range(4):
                                w_ = ci_ % 2
                                ci_ += 1
                                c0 = O_MG + n * D + fp * 256
                                wload(wm[:, w_, :, :], K_wm[w_], WIN[:, c0:c0 + 256])
                                for fl in range(2):
                                    ft = fp * 2 + fl
                                    for g in range(4):
                                        b0, b1, bi = it % 2, 2 + it % 2, it % 2
                                        it += 1
                                        for kt in range(8):
                                            C.op("pe", lambda e: e.matmul(PSB[b0][:], wm[:, w_, kt, fl * 128:(fl + 1) * 128], HT[:, kt, g * 512:(g + 1) * 512],
                                                                          start=(kt == 0), stop=(kt == 7)),
                                                 reads=[K_wm[w_]] + HTr(g), writes=[PSk[b0]], inc=(kt == 7))
                                        for ci in range(2):
                                            C.op("pe", lambda e: e.matmul(PSB[b1][:], wbr[:, wb_, ci, ft * 128:(ft + 1) * 128], YT[:, 2 * n + ci, g * 512:(g + 1) * 512],
                                                                          start=(ci == 0), stop=(ci == 1)),
                                                 reads=[K_wbr[wb_]] + YTk[2 * n + ci][4 * g:4 * g + 4], writes=[PSk[b1]], inc=(ci == 1))
                                        C.op("act", lambda e: e.activation(out=sig[:, bi, :], in_=PSB[b0][:], func=AF.Sigmoid), reads=[PSk[b0]], writes=[K_sig[bi]])
                                        mp = MP[:, ft, g * 512:(g + 1) * 512]
                                        if n == 0:
                                            C.op("dve", lambda e: e.tensor_tensor(out=mp, in0=sig[:, bi, :], in1=PSB[b1][:], op=ALU.mult),
                                                 reads=[K_sig[bi], PSk[b1]], writes=[MPk[ft][g]])
                                        else:
                                            C.op("dve", lambda e: e.tensor_tensor(out=prod[:, bi, :], in0=sig[:, bi, :], in1=PSB[b1][:], op=ALU.mult),
                                                 reads=[K_sig[bi], PSk[b1]], writes=[K_prod[bi]])
                                            C.op("dve", lambda e: e.tensor_tensor(out=mp, in0=mp, in1=prod[:, bi, :], op=ALU.add),
                                                 reads=[K_prod[bi], MPk[ft][g]], writes=[MPk[ft][g]])
                        C.barrier()
                    with ExitStack() as ph2:
                        wo = sb("wo", [128, 8, D], BF16, ph2)
                        K_wo = Tk()
                        wload(wo[:], K_wo, dr["w_out"][l])
                        tmp = sb("otmp", [128, 2, 512], F32, ph2)
                        K_tmp = tks(2)
                        it = 0
                        for tt in range(NT):
                            for hf in range(2):
                                pb, bi = 4 + it % 4, it % 2
                                it += 1
                                for ft in range(8):
                                    C.op("pe", lambda e: e.matmul(PSB[pb][:], MP[:, ft, tt * 128:(tt + 1) * 128], wo[:, ft, hf * 512:(hf + 1) * 512],
                                                                  start=(ft == 0), stop=(ft == 7)),
                                         reads=[K_wo, MPk[ft][tt // 4]], writes=[PSk[pb]], inc=(ft == 7))
                                C.op("dve", lambda e: e.tensor_tensor(out=tmp[:, bi, :], in0=PSB[pb][:], in1=G12[:, 0, hf * 512:(hf + 1) * 512], op=ALU.mult),
                                     reads=[PSk[pb], K_G[0]], writes=[K_tmp[bi]])
                                C.op("pool", lambda e: e.tensor_tensor(out=X[:, tt, hf * 512:(hf + 1) * 512], in0=X[:, tt, hf * 512:(hf + 1) * 512],
                                                                       in1=tmp[:, bi, :], op=ALU.add),
                                     reads=[K_tmp[bi], XT[tt]], writes=[XT[tt]])
                        C.barrier()
            tap(f"X1_{l}", X[:], XT, [128, NT, D])
            if stop_after == f"mix{l}":
                break

            norm_to_HT(2)
            with ExitStack() as ph:
                comb = sb("comb", [128, NT, NE], F32, ph)
                K_comb = tks(NT, "comb")
                bguT = sb("bguT", [128, NE * 16], F32, ph)
                K_bgu = Tk()
                with ExitStack() as ph2:
                    wr = sb("wr", [128, 8, NE], BF16, ph2)
                    brt = sb("brt", [128, NE], F32, ph2)
                    bd = sb("bd", [NE, D], F32, ph2)
                    K_r = Tk()
                    C.dma("pool", wr[:], dr["w_router"][l].rearrange("(kt p) e -> p kt e", p=128), writes=[K_r])
                    C.dma("sp", brt[:], dr["b_router"][l].partition_broadcast(128), writes=[K_r])
                    C.dma("sp", bd[:], dr["b_down"][l], writes=[K_r])
                    rows = sb("bgrows", [128, 4, 128], F32, ph2)
                    K_rows = Tk()
                    C.dma("sp", rows[:], dr["b_gate_up"][l].rearrange("e (j p) -> (e j) p", p=128).rearrange("(r q) p -> q r p", q=128), writes=[K_rows])
                    for r_ in range(4):
                        C.op("pe", lambda e: e.transpose(PSB[0][:, r_ * 128:(r_ + 1) * 128], rows[:, r_, :], ident_f[:]),
                             reads=[K_rows, K_id], writes=[PSk[0]], inc=(r_ == 3))
                    C.op("dve", lambda e: e.tensor_copy(out=bguT[:], in_=PSB[0][:]), reads=[PSk[0]], writes=[K_bgu])
                    bv = bguT[:].rearrange("p (e j) -> p e j", j=16)
                    C.op("dve", lambda e: e.tensor_scalar(out=bv[:, :, 8:16], in0=bv[:, :, 8:16], scalar1=1.0, scalar2=None, op0=ALU.add),
                         reads=[K_bgu], writes=[K_bgu])
                    lg = sb("lg", [128, 2, NE], F32, ph2)
                    m8r = sb("m8r", [128, 2, 8], F32, ph2)
                    sel = sb("rsel", [128, 2, NE], F32, ph2)
                    sm = sb("rsm", [128, 2, 2], F32, ph2)
                    cT = sb("rcT", [NE, 2, 128], F32, ph2)
                    tmpb = sb("rtmp", [128, 2, 512], F32, ph2)
                    K_lg, K_m8r, K_sel, K_sm, K_cT, K_tmpb = tks(2), tks(2), tks(2), tks(2), tks(2), tks(2)
                    it = 0
                    for tt in range(NT):
                        p = tt % 2
                        for kt in range(8):
                            C.op("pe", lambda e: e.matmul(PSB[1][:, 0:NE], HT[:, kt, tt * 128:(tt + 1) * 128], wr[:, kt, :], start=(kt == 0), stop=(kt == 7)),
                                 reads=[K_r] + HTr(tt // 4), writes=[PSk[1]], inc=(kt == 7))
                        C.op("dve", lambda e: e.tensor_tensor(out=lg[:, p, :], in0=PSB[1][:, 0:NE], in1=brt[:], op=ALU.add), reads=[PSk[1], K_r], writes=[K_lg[p]])
                        C.op("dve", lambda e: e.max(out=m8r[:, p, :], in_=lg[:, p, :]), reads=[K_lg[p]], writes=[K_m8r[p]])
                        C.op("dve", lambda e: e.tensor_scalar(out=sel[:, p, :], in0=lg[:, p, :], scalar1=m8r[:, p, 3:4], scalar2=None, op0=ALU.is_ge),
                             reads=[K_lg[p], K_m8r[p]], writes=[K_sel[p]])
                        C.op("dve", lambda e: e.tensor_scalar(out=sm[:, p, 0:1], in0=m8r[:, p, 0:1], scalar1=-1.0, scalar2=None, op0=ALU.mult),
                             reads=[K_m8r[p]], writes=[K_sm[p]])
                        C.op("act", lambda e: e.activation(out=lg[:, p, :], in_=lg[:, p, :], func=AF.Exp, bias=sm[:, p, 0:1]), reads=[K_lg[p], K_sm[p]], writes=[K_lg[p]])
                        C.op("dve", lambda e: e.tensor_tensor(out=sel[:, p, :], in0=sel[:, p, :], in1=lg[:, p, :], op=ALU.mult), reads=[K_lg[p], K_sel[p]], writes=[K_sel[p]])
                        C.op("dve", lambda e: e.reduce_sum(out=sm[:, p, 1:2], in_=sel[:, p, :], axis=AX.X), reads=[K_sel[p]], writes=[K_sm[p]])
                        C.op("dve", lambda e: e.reciprocal(out=sm[:, p, 1:2], in_=sm[:, p, 1:2]), reads=[K_sm[p]], writes=[K_sm[p]])
                        C.op("dve", lambda e: e.tensor_scalar(out=comb[:, tt, :], in0=sel[:, p, :], scalar1=sm[:, p, 1:2], scalar2=None, op0=ALU.mult),
                             reads=[K_sel[p], K_sm[p]], writes=[K_comb[tt]])
                        C.op("pe", lambda e: e.transpose(PSB[2][0:NE, 0:128], comb[:, tt, :], ident_f[:]), reads=[K_comb[tt], K_id], writes=[PSk[2]])
                        C.op("act", lambda e: e.copy(out=cT[:, p, :], in_=PSB[2][0:NE, 0:128]), reads=[PSk[2]], writes=[K_cT[p]])
                        for hf in range(2):
                            pb, bi = 4 + it % 4, it % 2
                            it += 1
                            C.op("pe", lambda e: e.matmul(PSB[pb][:], cT[:, p, :], bd[:, hf * 512:(hf + 1) * 512], start=True, stop=True),
                                 reads=[K_cT[p], K_r], writes=[PSk[pb]])
                            C.op("dve", lambda e: e.tensor_tensor(out=tmpb[:, bi, :], in0=PSB[pb][:], in1=G12[:, 1, hf * 512:(hf + 1) * 512], op=ALU.mult),
                                 reads=[PSk[pb], K_G[1]], writes=[K_tmpb[bi]])
                            C.op("pool", lambda e: e.tensor_tensor(out=X[:, tt, hf * 512:(hf + 1) * 512], in0=X[:, tt, hf * 512:(hf + 1) * 512],
                                                                   in1=tmpb[:, bi, :], op=ALU.add),
                                 reads=[K_tmpb[bi], XT[tt]], writes=[XT[tt]])
                    C.barrier()
                tap(f"comb{l}", comb[:], K_comb, [128, NT, NE])
                if stop_after == f"router{l}":
                    break
                RING = 4
                ring = sb("ring", [128, RING, 8, 512], BF16, ph)
                K_ring = tks(RING, "ring")
                actT = sb("actT", [128, 8, S], BF16, ph)
                K_act = [tks(4, f"act{f}_") for f in range(8)]
                xg = sb("xg", [128, 2, 512], F32, ph)
                sg = sb("sg", [128, 2, 512], BF16, ph)
                Al = sb("Al", [128, 2, 512], F32, ph)
                tg_ = sb("tg", [128, 2, 512], F32, ph)
                yt_ = sb("ytmp", [128, 2, 512], F32, ph)
                K_xg, K_sg, K_Al, K_tg, K_ytmp = tks(2), tks(2), tks(2), tks(2), tks(2)
                n_exp = NE if stop_after != f"moe1e{l}" else 1
                chunks = []
                for e_ in range(n_exp):
                    wgu = dr["w_gate_up"][l][e_]
                    wdn = dr["w_down"][l][e_]
                    chunks += [wgu[:, 0:512], wgu[:, 1024:1536], wgu[:, 512:1024], wgu[:, 1536:2048], wdn[:, 0:512], wdn[:, 512:1024]]
                loaded = [0]

                def prefetch(upto):
                    while loaded[0] < min(upto, len(chunks)):
                        i = loaded[0]
                        wload(ring[:, i % RING, :, :], K_ring[i % RING], chunks[i])
                        loaded[0] += 1
                prefetch(RING - 1)
                ci_ = 0
                gi = 0
                yi = 0
                for e_ in range(n_exp):
                    for c in range(2):
                        sl_g, sl_l = ci_ % RING, (ci_ + 1) % RING
                        prefetch(ci_ + RING)
                        for g in range(4):
                            for i in range(4):
                                ft = c * 4 + i
                                pg, pl, bi = (gi % 2) * 2, (gi % 2) * 2 + 1, gi % 2
                                gi += 1
                                for kt in range(8):
                                    C.op("pe", lambda e: e.matmul(PSB[pg][:], ring[:, sl_g, kt, i * 128:(i + 1) * 128], HT[:, kt, g * 512:(g + 1) * 512],
                                                                  start=(kt == 0), stop=(kt == 7)),
                                         reads=[K_ring[sl_g]] + HTr(g), writes=[PSk[pg]], inc=(kt == 7))
                                for kt in range(8):
                                    C.op("pe", lambda e: e.matmul(PSB[pl][:], ring[:, sl_l, kt, i * 128:(i + 1) * 128], HT[:, kt, g * 512:(g + 1) * 512],
                                                                  start=(kt == 0), stop=(kt == 7)),
                                         reads=[K_ring[sl_l]] + HTr(g), writes=[PSk[pl]], inc=(kt == 7))
                                cg = e_ * 16 + ft
                                C.op("dve", lambda e: e.tensor_scalar(out=xg[:, bi, :], in0=PSB[pg][:], scalar1=bguT[:, cg:cg + 1], scalar2=7.0, op0=ALU.add, op1=ALU.min),
                                     reads=[PSk[pg], K_bgu], writes=[K_xg[bi]])
                                C.op("act", lambda e: e.activation(out=sg[:, bi, :], in_=xg[:, bi, :], func=AF.Sigmoid, scale=1.702), reads=[K_xg[bi]], writes=[K_sg[bi]])
                                C.op("dve", lambda e: e.tensor_scalar(out=Al[:, bi, :], in0=PSB[pl][:], scalar1=bguT[:, cg + 8:cg + 9], scalar2=8.0, op0=ALU.add, op1=ALU.min),
                                     reads=[PSk[pl], K_bgu], writes=[K_Al[bi]])
                                C.op("dve", lambda e: e.tensor_tensor(out=tg_[:, bi, :], in0=xg[:, bi, :], in1=sg[:, bi, :], op=ALU.mult),
                                     reads=[K_xg[bi], K_sg[bi]], writes=[K_tg[bi]])
                                C.op("dve", lambda e: e.scalar_tensor_tensor(out=actT[:, ft, g * 512:(g + 1) * 512], in0=Al[:, bi, :], scalar=-6.0, in1=tg_[:, bi, :],
                                                                             op0=ALU.max, op1=ALU.mult),
                                     reads=[K_Al[bi], K_tg[bi]], writes=[K_act[ft][g]])
                        ci_ += 2
                    for hf in range(2):
                        sl_d = ci_ % RING
                        prefetch(ci_ + RING)
                        for tt in range(NT):
                            pb, bi = 4 + yi % 4, yi % 2
                            yi += 1
                            for ft in range(8):
                                C.op("pe", lambda e: e.matmul(PSB[pb][:], actT[:, ft, tt * 128:(tt + 1) * 128], ring[:, sl_d, ft, :], start=(ft == 0), stop=(ft == 7)),
                                     reads=[K_ring[sl_d], K_act[ft][tt // 4]], writes=[PSk[pb]], inc=(ft == 7))
                            C.op("act", lambda e: e.activation(out=yt_[:, bi, :], in_=PSB[pb][:], func=AF.Identity, scale=comb[:, tt, e_:e_ + 1]),
                                 reads=[PSk[pb], K_comb[tt]], writes=[K_ytmp[bi]])
                            C.op("pool", lambda e: e.tensor_tensor(out=yt_[:, bi, :], in0=yt_[:, bi, :], in1=G12[:, 1, hf * 512:(hf + 1) * 512], op=ALU.mult),
                                 reads=[K_ytmp[bi], K_G[1]], writes=[K_ytmp[bi]])
                            C.op("pool", lambda e: e.tensor_tensor(out=X[:, tt, hf * 512:(hf + 1) * 512], in0=X[:, tt, hf * 512:(hf + 1) * 512],
                                                                   in1=yt_[:, bi, :], op=ALU.add),
                                 reads=[K_ytmp[bi], XT[tt]], writes=[XT[tt]])
                        ci_ += 1
                C.barrier()
            tap(f"X2_{l}", X[:], XT, [128, NT, D])
            if stop_after == f"moe{l}" or stop_after == f"moe1e{l}":
                break

        if stop_after is None:
            with ExitStack() as ph:
                gf = sb("gf", [128, D], F32, ph)
                K_gf = Tk()
                C.dma("sp", gf[:], dr["g_final"][0].partition_broadcast(128), writes=[K_gf])
                ot = sb("ot", [128, 2, D], F32, ph)
                K_ot = tks(2)
                ov_ = out_d.rearrange("(t p) d -> p t d", p=128)
                for tt in range(NT):
                    p = tt % 2
                    g = tt // 4
                    C.op("act", lambda e: e.activation(out=junk[:, p, :], in_=X[:, tt, :], func=AF.Square, accum_out=ss[:, tt:tt + 1]),
                         reads=[XT[tt]], writes=[K_junk[p], K_ss[g]])
                    C.op("dve", lambda e: e.tensor_scalar(out=rstd[:, tt:tt + 1], in0=ss[:, tt:tt + 1], scalar1=1.0 / D, scalar2=EPS, op0=ALU.mult, op1=ALU.add),
                         reads=[K_ss[g]], writes=[K_rstd[g]])
                    C.op("act", lambda e: e.activation(out=rstd[:, tt:tt + 1], in_=rstd[:, tt:tt + 1], func=AF.Sqrt), reads=[K_rstd[g]], writes=[K_rstd[g]])
                    C.op("dve", lambda e: e.reciprocal(out=rstd[:, tt:tt + 1], in_=rstd[:, tt:tt + 1]), reads=[K_rstd[g]], writes=[K_rstd[g]])
                    C.op("dve", lambda e: e.scalar_tensor_tensor(out=ot[:, p, :], in0=X[:, tt, :], scalar=rstd[:, tt:tt + 1], in1=gf[:], op0=ALU.mult, op1=ALU.mult),
                         reads=[XT[tt], K_rstd[g], K_gf], writes=[K_ot[p]])
                    C.dma("sp", ov_[:, tt, :], ot[:, p, :], reads=[K_ot[p]])
        C.barrier()
    return nc, list(tap_d.keys())


def kernel(**inputs):
    n = 8
    nc, _ = build()
    consts = make_consts()
    shared = {}
    for k in IN_SPECS:
        if k in ("x", "c"):
            continue
        a = np.ascontiguousarray(np.asarray(inputs[k], dtype=np.float32))
        if k == "g_final":
            a = a.reshape(1, D)
        shared[k] = a
    for k, v in consts.items():
        shared["k_" + k] = v
    x = np.asarray(inputs["x"], dtype=np.float32)
    c = np.asarray(inputs["c"], dtype=np.float32)
    in_maps = []
    for b in range(n):
        m = dict(shared)
        m["x"] = np.ascontiguousarray(x[b])
        m["c"] = np.ascontiguousarray(c[b:b + 1])
        in_maps.append(m)
    res = run_bass_kernel_spmd(nc, in_maps, core_ids=list(range(n)))
    return np.stack([np.asarray(r["out"], dtype=np.float32) for r in res.results], axis=0)
```

```python
import numpy as np
import ml_dtypes
from contextlib import ExitStack
import concourse.bass as bass
import concourse.mybir as mybir
from concourse.bass_utils import run_bass_kernel_spmd

F32 = mybir.dt.float32
BF16 = mybir.dt.bfloat16
AF = mybir.ActivationFunctionType
ALU = mybir.AluOpType
AX = mybir.AxisListType

D = 1024
S = 2048
NT = 16
DEPTH = 2
NE = 32
EPS = 1e-5
D_IN = 7184
O_MQ, O_MK, O_MV = 0, 256, 512
O_GQ, O_GK, O_GV, O_GA, O_GR = 768, 896, 1024, 1280, 1296
O_RQ, O_RK, O_RV, O_RG = 1552, 1808, 2064, 2320
O_SQ, O_SK, O_SV = 2576, 2832, 2960
O_MG = 3088
NEG = -30000.0
SKIP = {"ret"}
RETCUT = 99
RETTILES = 16
RETV = 0


class Tk:
    __slots__ = ("w", "r", "name")

    def __init__(self, name=""):
        self.w = None
        self.r = {}
        self.name = name


def tks(n, name=""):
    return [Tk(f"{name}{i}") for i in range(n)]


class Ctx:
    ENG = ("pe", "act", "dve", "pool", "sp")

    def __init__(self, nc, es, n_dsem=24):
        self.nc = nc
        self.E = {"pe": nc.tensor, "act": nc.scalar, "dve": nc.vector, "pool": nc.gpsimd, "sp": nc.sync}
        self.sem = {e: es.enter_context(nc.semaphore("s_" + e)) for e in self.ENG}
        self.cnt = {e: 0 for e in self.ENG}
        self.seen = {e: {} for e in self.ENG}
        self.dsem = [es.enter_context(nc.semaphore(f"s_d{i}")) for i in range(n_dsem)]
        self.dcnt = [0] * n_dsem
        half = n_dsem // 2
        self.dpool = {"sp": list(range(half)), "pool": list(range(half, n_dsem))}
        self.dnext = {"sp": 0, "pool": 0}
        self.nins = 0

    def _need(self, eng, reads, writes):
        need = {}

        def add(key, val):
            if need.get(key, 0) < val:
                need[key] = val

        for t in reads:
            if t.w is not None:
                for k, v in t.w.items():
                    add(k, v)
        for t in writes:
            if t.w is not None:
                for k, v in t.w.items():
                    add(k, v)
            for k, v in t.r.items():
                add(k, v)
        for key, val in need.items():
            if key == ("e", "pe") and eng == "pe":
                continue
            if self.seen[eng].get(key, 0) >= val:
                continue
            sem = self.sem[key[1]] if key[0] == "e" else self.dsem[key[1]]
            self.E[eng].wait_ge(sem, val)
            self.seen[eng][key] = val
            self.nins += 1

    def _mark(self, key, val, reads, writes):
        for t in reads:
            if t.r.get(key, 0) < val:
                t.r[key] = val
        for t in writes:
            if key[0] == "d" and t.w is not None:
                t.w[key] = val
            else:
                t.w = {key: val}
            t.r = {}

    def op(self, eng, fn, reads=(), writes=(), inc=True):
        self._need(eng, reads, writes)
        ins = fn(self.E[eng])
        self.nins += 1
        if inc:
            ins.then_inc(self.sem[eng], 1)
            self.cnt[eng] += 1
            val = self.cnt[eng]
        else:
            assert eng == "pe"
            val = self.cnt[eng] + 1
        self._mark(("e", eng), val, reads, writes)
        return ins

    def dma(self, q, out, in_, reads=(), writes=(), **kw):
        self._need(q, reads, writes)
        i = self.dpool[q][self.dnext[q]]
        self.dnext[q] = (self.dnext[q] + 1) % len(self.dpool[q])
        key = ("d", i)
        if self.dcnt[i] > 0 and self.seen[q].get(key, 0) < self.dcnt[i] * 16:
            self.E[q].wait_ge(self.dsem[i], self.dcnt[i] * 16)
            self.seen[q][key] = self.dcnt[i] * 16
        ins = self.E[q].dma_start(out=out, in_=in_, **kw)
        ins.then_inc(self.dsem[i], 16)
        self.nins += 1
        self.dcnt[i] += 1
        self._mark(key, self.dcnt[i] * 16, reads, writes)
        return ins

    def barrier(self):
        for i in range(len(self.dsem)):
            if self.dcnt[i] > 0 and self.seen["sp"].get(("d", i), 0) < self.dcnt[i] * 16:
                self.E["sp"].wait_ge(self.dsem[i], self.dcnt[i] * 16)
                self.seen["sp"][("d", i)] = self.dcnt[i] * 16
        ins = self.E["sp"].nop() if False else None
        for e in self.ENG:
            for f in self.ENG:
                key = ("e", f)
                if self.cnt[f] > 0 and self.seen[e].get(key, 0) < self.cnt[f]:
                    self.E[e].wait_ge(self.sem[f], self.cnt[f])
                    self.seen[e][key] = self.cnt[f]
            for i in range(len(self.dsem)):
                if self.dcnt[i] > 0 and self.seen[e].get(("d", i), 0) < self.dcnt[i] * 16:
                    self.E[e].wait_ge(self.dsem[i], self.dcnt[i] * 16)
                    self.seen[e][("d", i)] = self.dcnt[i] * 16


def make_consts():
    bf = ml_dtypes.bfloat16
    c = {}
    c["ident_bf"] = np.eye(128, dtype=np.float32).astype(bf)
    c["ident_f"] = np.eye(128, dtype=np.float32)
    k = np.arange(128)[:, None]
    q = np.arange(128)[None, :]
    c["tri_bf"] = (k <= q).astype(np.float32).astype(bf)
    slopes = 2.0 ** (-(np.arange(8, dtype=np.float64) + 1.0))
    swa = np.zeros((128, 4, 2, 128), np.float64)
    for h in range(4):
        sl = slopes[h]
        dist_prev = 128 + q - k
        swa[:, h, 0, :] = np.where(k > q, np.exp(-sl * dist_prev), 0.0)
        dist_own = q - k
        swa[:, h, 1, :] = np.where(k <= q, np.exp(-sl * dist_own), 0.0)
    c["swa_mask"] = swa.astype(np.float32).astype(bf)
    t = np.arange(S)
    a = (t // 128).astype(np.float64)
    r = (t % 128).astype(np.float64)
    qa = np.zeros((4, 12, S), np.float64)
    ka = np.zeros((4, 12, S), np.float64)
    for h in range(4):
        sl = slopes[4 + h] * 8.0
        for j in range(8):
            ka[h, j] = (t // 256 == j)
        qa[h, 8] = -sl * 128.0 * a
        ka[h, 8] = 1.0
        qa[h, 9] = -sl * r
        ka[h, 9] = 1.0
        qa[h, 10] = 1.0
        ka[h, 10] = sl * 128.0 * a
        qa[h, 11] = 1.0
        ka[h, 11] = sl * r
    c["moba_qa"] = qa.astype(np.float32).astype(bf)
    c["moba_ka"] = ka.astype(np.float32).astype(bf)
    m = np.arange(128)[:, None]
    l = np.arange(128)[None, :]
    c["cum_incl"] = ((m <= l) * (-1.0 / 16.0)).astype(np.float32)
    c["cum_after"] = ((m > l) * (-1.0 / 16.0)).astype(np.float32)
    gam = 1.0 - 2.0 ** (-5.0 - np.arange(4, dtype=np.float64))
    pos = np.arange(128, dtype=np.float64)
    rq = np.zeros((128, 2, 128)); rk = np.zeros((128, 2, 128))
    rke = np.zeros((128, 256)); rdec = np.zeros((128, 2))
    for h in range(4):
        j, o = h // 2, (h % 2) * 64
        rq[o:o + 64, j, :] = gam[h] ** (pos + 1.0)
        rk[o:o + 64, j, :] = gam[h] ** (-(pos + 1.0)) * 64 ** -0.5
        rke[:, h * 64:(h + 1) * 64] = (gam[h] ** (127.0 - pos))[:, None] * 64 ** -0.5
        rdec[o:o + 64, j] = gam[h] ** 128.0
    tt_ = np.arange(16)[:, None]; nn_ = np.arange(8)[None, :]
    c["moba_past"] = np.where(nn_ < tt_ // 2, 0.0, -1e30).astype(np.float32).reshape(1, 128)
    c["moba_notown"] = np.where(nn_ == tt_ // 2, 0.0, 1.0).astype(np.float32).reshape(1, 128)
    bd = np.zeros((128, 256), np.float32)
    for h in range(4):
        bd[32 * h:32 * h + 32, 64 * h:64 * h + 64] = 1.0
    c["bd_gla"] = bd
    bd = np.zeros((128, 128), np.float32)
    for h in range(2):
        bd[64 * h:64 * h + 64, 64 * h:64 * h + 64] = 1.0
    c["bd_ret"] = bd
    tpos = np.arange(S, dtype=np.float64) - 1024.0
    c["ret_qd"] = np.stack([gam[h] ** tpos for h in range(4)]).astype(np.float32)
    c["ret_kd"] = np.stack([gam[h] ** (-tpos) for h in range(4)]).astype(np.float32)
    c["ret_q"] = rq.astype(np.float32)
    c["ret_k"] = rk.astype(np.float32)
    c["ret_kend"] = rke.astype(np.float32)
    c["ret_dec"] = rdec.astype(np.float32)
    return c


CONST_SPECS = {
    "ident_bf": ([128, 128], BF16), "ident_f": ([128, 128], F32), "tri_bf": ([128, 128], BF16),
    "swa_mask": ([128, 4, 2, 128], BF16), "moba_qa": ([4, 12, S], BF16), "moba_ka": ([4, 12, S], BF16),
    "cum_incl": ([128, 128], F32), "cum_after": ([128, 128], F32),
    "ret_q": ([128, 2, 128], F32), "ret_k": ([128, 2, 128], F32), "ret_kend": ([128, 256], F32),
    "ret_dec": ([128, 2], F32), "moba_past": ([1, 128], F32), "ret_qd": ([4, S], F32), "ret_kd": ([4, S], F32), "bd_gla": ([128, 256], F32), "bd_ret": ([128, 128], F32), "moba_notown": ([1, 128], F32),
}

IN_SPECS = {
    "x": [S, D], "c": [1, D], "w_ada": [DEPTH, D, 6 * D], "b_ada": [DEPTH, 6 * D], "g_norm_mix": [DEPTH, D],
    "w_in": [DEPTH, D, D_IN], "w_gla_gate": [DEPTH, 16, 128], "b_gla_gate": [DEPTH, 128],
    "g_gla_norm": [DEPTH, 256], "g_ret_norm": [DEPTH, 256], "attn_sinks": [DEPTH, 4],
    "w_branch": [DEPTH, 4, 256, D], "w_out": [DEPTH, D, D], "g_norm_ffn": [DEPTH, D],
    "w_router": [DEPTH, D, NE], "b_router": [DEPTH, NE], "w_gate_up": [DEPTH, NE, D, 2 * D],
    "b_gate_up": [DEPTH, NE, 2 * D], "w_down": [DEPTH, NE, D, D], "b_down": [DEPTH, NE, D],
    "g_final": [1, D],
}


def build(n_layers=DEPTH, stop_after=None, taps=()):
    nc = bass.Bass("TRN2", target_bir_lowering=False)
    small = stop_after is not None and not stop_after.startswith(("moe", "router"))
    dr = {k: nc.dram_tensor(k, shp, F32, kind="ExternalInput").ap() for k, shp in IN_SPECS.items()
          if not (small and k in ("w_gate_up", "w_down"))}
    cst = {k: nc.dram_tensor("k_" + k, shp, dt, kind="ExternalInput").ap() for k, (shp, dt) in CONST_SPECS.items()}
    out_d = nc.dram_tensor("out", [S, D], F32, kind="ExternalOutput").ap()
    tap_d = {}
    es = ExitStack()
    with es:
        C = Ctx(nc, es)

        uniq = [0]

        def sb(name, shape, dt, stack=es):
            uniq[0] += 1
            return stack.enter_context(nc.sbuf_tensor(f"{name}_{uniq[0]}", shape, dt))

        X = sb("X", [128, NT, D], F32)
        XT = tks(NT, "X")
        HT = sb("HT", [128, 8, S], BF16)
        HTk = [[Tk(f"HT{f}_{g}") for g in range(4)] for f in range(8)]
        ident_bf = sb("ident_bf", [128, 128], BF16)
        ident_f = sb("ident_f", [128, 128], F32)
        K_id = Tk("ident")
        PSB = [es.enter_context(nc.psum_tensor(f"ps{i}", [128, 512], F32)) for i in range(8)]
        PSk = tks(8, "ps")
        colsT = sb("colsT", [128, 64], F32)
        K_cols = Tk("colsT")
        modT = sb("modT", [128, 48], F32)
        K_mod = Tk("modT")
        AB = sb("AB", [128, 4, 8], F32)
        K_AB = Tk("AB")
        G12 = sb("G12", [128, 2, D], F32)
        K_G = tks(2, "G")
        cactT = sb("cactT", [128, 8], BF16)
        cactB = sb("cactB", [128, 8, 128], BF16)
        K_cact = Tk("cact")
        ss = sb("ss", [128, NT], F32)
        rstd = sb("rstd", [128, NT], F32)
        K_ss = tks(4, "ss")
        K_rstd = tks(4, "rstd")
        junk = sb("junk", [128, 2, D], BF16)
        K_junk = tks(2, "junk")

        def tap(name, ap, reads, shape, dt=F32):
            if name not in taps:
                return
            d = nc.dram_tensor("tap_" + name, list(shape), dt, kind="ExternalOutput").ap()
            tap_d[name] = d
            C.dma("sp", d, ap, reads=reads)

        def wload(dst, dst_tk, src2d, q="pool"):
            n = src2d.shape[1]
            sv = src2d.rearrange("(kt p) c -> p kt c", p=128)
            for c0 in range(0, n, 512):
                c1 = min(n, c0 + 512)
                C.dma(q, dst[:, :, c0:c1], sv[:, :, c0:c1], writes=[dst_tk])

        def ps_bf(i):
            return PSB[i][:].bitcast(BF16)

        C.dma("sp", ident_bf[:], cst["ident_bf"], writes=[K_id])
        C.dma("sp", ident_f[:], cst["ident_f"], writes=[K_id])
        xv = dr["x"].rearrange("(t p) d -> p t d", p=128)
        for g in range(4):
            C.dma("sp", X[:, 4 * g:4 * g + 4, :], xv[:, 4 * g:4 * g + 4, :], writes=XT[4 * g:4 * g + 4])

        with ExitStack() as ph:
            crow = sb("crow", [8, 128], F32, ph)
            K_crow = Tk()
            ccol = sb("ccol", [128, 8], F32, ph)
            K_ccol = Tk()
            C.dma("sp", crow[:], dr["c"].rearrange("o (kt p) -> (o kt) p", p=128), writes=[K_crow])
            C.op("pe", lambda e: e.transpose(PSB[0][:, 0:8], crow[:], ident_f[0:8, 0:8]),
                 reads=[K_crow, K_id], writes=[PSk[0]])
            C.op("act", lambda e: e.activation(out=ccol[:], in_=PSB[0][:, 0:8], func=AF.Silu),
                 reads=[PSk[0]], writes=[K_ccol])
            C.op("dve", lambda e: e.tensor_copy(out=cactT[:], in_=ccol[:]), reads=[K_ccol], writes=[K_cact])
            C.op("dve", lambda e: e.tensor_copy(out=cactB[:], in_=ccol[:].unsqueeze(2).to_broadcast([128, 8, 128])),
                 reads=[K_ccol], writes=[K_cact])
            C.barrier()

        def adaln(l):
            with ExitStack() as ph:
                rows = sb("rows", [64, 128], F32, ph)
                K_rows = Tk()
                C.dma("sp", rows[0:48, :], dr["b_ada"][l].rearrange("(r p) -> r p", p=128), writes=[K_rows])
                C.dma("sp", rows[48:56, :], dr["g_norm_mix"][l].rearrange("(r p) -> r p", p=128), writes=[K_rows])
                C.dma("sp", rows[56:64, :], dr["g_norm_ffn"][l].rearrange("(r p) -> r p", p=128), writes=[K_rows])
                C.op("pe", lambda e: e.transpose(PSB[0][:, 0:64], rows[:], ident_f[0:64, 0:64]),
                     reads=[K_rows, K_id], writes=[PSk[0]])
                C.op("dve", lambda e: e.tensor_copy(out=colsT[:], in_=PSB[0][:, 0:64]), reads=[PSk[0]], writes=[K_cols])
                wa = [sb(f"wa{i}", [128, 8, 512], BF16, ph) for i in range(2)]
                K_wa = tks(2, "wa")
                bbc = sb("bbc", [128, D], F32, ph)
                K_bbc = Tk()
                ci = 0
                for j in range(6):
                    for half in range(2):
                        w = ci % 2
                        ci += 1
                        c0 = j * D + half * 512
                        wload(wa[w][:], K_wa[w], dr["w_ada"][l][:, c0:c0 + 512])
                        if j in (2, 5):
                            gi = 0 if j == 2 else 1
                            pb = 2 + half
                            for kt in range(8):
                                C.op("pe", lambda e: e.matmul(PSB[pb][:], cactB[:, kt, :], wa[w][:, kt, :],
                                                              start=(kt == 0), stop=(kt == 7)),
                                     reads=[K_cact, K_wa[w]], writes=[PSk[pb]], inc=(kt == 7))
                            if half == 0:
                                C.dma("sp", bbc[:], dr["b_ada"][l][j * D:(j + 1) * D].partition_broadcast(128),
                                      writes=[K_bbc])
                            C.op("dve", lambda e: e.tensor_tensor(out=G12[:, gi, half * 512:(half + 1) * 512],
                                                                  in0=PSB[pb][:], in1=bbc[:, half * 512:(half + 1) * 512],
                                                                  op=ALU.add),
                                 reads=[PSk[pb], K_bbc], writes=[K_G[gi]])
                        else:
                            for fl in range(4):
                                col = j * 8 + half * 4 + fl
                                for kt in range(8):
                                    C.op("pe", lambda e: e.matmul(PSB[1][:, col:col + 1], wa[w][:, kt, fl * 128:(fl + 1) * 128],
                                                                  cactT[:, kt:kt + 1], start=(kt == 0), stop=(kt == 7)),
                                         reads=[K_cact, K_wa[w]], writes=[PSk[1]], inc=(kt == 7))
                for j in (0, 1, 3, 4):
                    C.op("dve", lambda e: e.tensor_tensor(out=modT[:, j * 8:(j + 1) * 8], in0=PSB[1][:, j * 8:(j + 1) * 8],
                                                          in1=colsT[:, j * 8:(j + 1) * 8], op=ALU.add),
                         reads=[PSk[1], K_cols], writes=[K_mod])
                C.op("dve", lambda e: e.scalar_tensor_tensor(out=AB[:, 0, :], in0=modT[:, 8:16], scalar=1.0,
                                                             in1=colsT[:, 48:56], op0=ALU.add, op1=ALU.mult),
                     reads=[K_mod, K_cols], writes=[K_AB])
                C.op("dve", lambda e: e.tensor_copy(out=AB[:, 1, :], in_=modT[:, 0:8]), reads=[K_mod], writes=[K_AB])
                C.op("dve", lambda e: e.scalar_tensor_tensor(out=AB[:, 2, :], in0=modT[:, 32:40], scalar=1.0,
                                                             in1=colsT[:, 56:64], op0=ALU.add, op1=ALU.mult),
                     reads=[K_mod, K_cols], writes=[K_AB])
                C.op("dve", lambda e: e.tensor_copy(out=AB[:, 3, :], in_=modT[:, 24:32]), reads=[K_mod], writes=[K_AB])
                C.barrier()

        def norm_to_HT(ai):
            with ExitStack() as ph:
                xn = sb("xn", [128, 4, D], BF16, ph)
                K_xn = tks(4, "xn")
                ev = 0
                for g in range(4):
                    for i in range(4):
                        tt = 4 * g + i
                        C.op("act", lambda e: e.activation(out=junk[:, i % 2, :], in_=X[:, tt, :], func=AF.Square,
                                                           accum_out=ss[:, tt:tt + 1]),
                             reads=[XT[tt]], writes=[K_junk[i % 2], K_ss[g]])
                    C.op("dve", lambda e: e.tensor_scalar(out=rstd[:, 4 * g:4 * g + 4], in0=ss[:, 4 * g:4 * g + 4],
                                                          scalar1=1.0 / D, scalar2=EPS, op0=ALU.mult, op1=ALU.add),
                         reads=[K_ss[g]], writes=[K_rstd[g]])
                    C.op("act", lambda e: e.activation(out=rstd[:, 4 * g:4 * g + 4], in_=rstd[:, 4 * g:4 * g + 4], func=AF.Sqrt),
                         reads=[K_rstd[g]], writes=[K_rstd[g]])
                    C.op("dve", lambda e: e.reciprocal(out=rstd[:, 4 * g:4 * g + 4], in_=rstd[:, 4 * g:4 * g + 4]),
                         reads=[K_rstd[g]], writes=[K_rstd[g]])
                    for i in range(4):
                        tt = 4 * g + i
                        C.op("dve", lambda e: e.tensor_scalar(out=xn[:, i, :], in0=X[:, tt, :], scalar1=rstd[:, tt:tt + 1],
                                                              scalar2=None, op0=ALU.mult),
                             reads=[XT[tt], K_rstd[g]], writes=[K_xn[i]])
                    for ft in range(8):
                        pb = ft % 4
                        for i in range(4):
                            C.op("pe", lambda e: e.transpose(ps_bf(pb)[:, i * 128:(i + 1) * 128],
                                                             xn[:, i, ft * 128:(ft + 1) * 128], ident_bf[:]),
                                 reads=[K_xn[i], K_id], writes=[PSk[pb]], inc=(i == 3))
                        dst = HT[:, ft, g * 512:(g + 1) * 512]
                        if ev % 2 == 0:
                            C.op("act", lambda e: e.activation(out=dst, in_=ps_bf(pb)[:, 0:512], func=AF.Identity,
                                                               bias=AB[:, ai + 1, ft:ft + 1], scale=AB[:, ai, ft:ft + 1]),
                                 reads=[PSk[pb], K_AB], writes=[HTk[ft][g]])
                        else:
                            C.op("dve", lambda e: e.tensor_scalar(out=dst, in0=ps_bf(pb)[:, 0:512],
                                                                  scalar1=AB[:, ai, ft:ft + 1], scalar2=AB[:, ai + 1, ft:ft + 1],
                                                                  op0=ALU.mult, op1=ALU.add),
                                 reads=[PSk[pb], K_AB], writes=[HTk[ft][g]])
                        ev += 1
                C.barrier()

        for l in range(n_layers):
            adaln(l)
            tap(f"modT{l}", modT[:], [K_mod], [128, 48])
            tap(f"G{l}", G12[:], K_G, [128, 2, D])
            norm_to_HT(0)
            tap(f"HT{l}", HT[:], [t for r in HTk for t in r], [128, 8, S], BF16)
            if stop_after == f"norm{l}":
                break

            with ExitStack() as mx:
                YT = sb("YT", [128, 8, S], BF16, mx)
                YTk = [tks(NT, f"YT{c}_") for c in range(8)]
                tri = sb("tri", [128, 128], BF16, mx)
                K_tri = Tk()
                C.dma("sp", tri[:], cst["tri_bf"], writes=[K_tri])
                ytile = sb("ytile", [128, 2, 256], BF16, mx)
                K_yt = tks(2, "yt")
                WIN = dr["w_in"][l]

                def emit_y(n, tt, par):
                    pb = 7
                    for ci in range(2):
                        C.op("pe", lambda e: e.transpose(ps_bf(pb)[:, ci * 128:(ci + 1) * 128], ytile[:, par, ci * 128:(ci + 1) * 128], ident_bf[:]),
                             reads=[K_yt[par], K_id], writes=[PSk[pb]], inc=(ci == 1))
                    C.op("act", lambda e: e.copy(out=YT[:, 2 * n:2 * n + 2, tt * 128:(tt + 1) * 128],
                                                 in_=ps_bf(pb)[:, 0:256].rearrange("p (c x) -> p c x", c=2)),
                         reads=[PSk[pb]], writes=[YTk[2 * n][tt], YTk[2 * n + 1][tt]])

                def HTr(g):
                    return [HTk[f][g] for f in range(8)]

                def proj_fm(dst_fn, w, K_w, c0, M, evi=[0]):
                    for g in range(4):
                        pb = evi[0] % 2
                        for kt in range(8):
                            C.op("pe", lambda e: e.matmul(PSB[pb][0:M, :], w[:, kt, c0:c0 + M], HT[:, kt, g * 512:(g + 1) * 512],
                                                          start=(kt == 0), stop=(kt == 7)),
                                 reads=[K_w] + HTr(g), writes=[PSk[pb]], inc=(kt == 7))
                        dst, K_dst = dst_fn(g)
                        if evi[0] % 2 == 0:
                            C.op("act", lambda e: e.copy(out=dst, in_=PSB[pb][0:M, :]), reads=[PSk[pb]], writes=[K_dst])
                        else:
                            C.op("dve", lambda e: e.tensor_copy(out=dst, in_=PSB[pb][0:M, :]), reads=[PSk[pb]], writes=[K_dst])
                        evi[0] += 1

                def proj_tm(dst_fn, w, K_w, c0, N, evi=[0]):
                    for tt in range(NT):
                        pb = 2 + evi[0] % 2
                        evi[0] += 1
                        for kt in range(8):
                            C.op("pe", lambda e: e.matmul(PSB[pb][:, 0:N], HT[:, kt, tt * 128:(tt + 1) * 128], w[:, kt, c0:c0 + N],
                                                          start=(kt == 0), stop=(kt == 7)),
                                 reads=[K_w] + HTr(tt // 4), writes=[PSk[pb]], inc=(kt == 7))
                        dst_fn(tt, pb)

                if "swa" not in SKIP:
                    with ExitStack() as ph:
                        QS = [sb(f"QS{h}", [64, S], BF16, ph) for h in range(4)]
                        K_QS = [tks(4, f"QS{h}_") for h in range(4)]
                        KS = [sb(f"KS{g}", [64, S], BF16, ph) for g in range(2)]
                        K_KS = [tks(4, f"KS{g}_") for g in range(2)]
                        VS = sb("VS", [128, NT, 2, 65], BF16, ph)
                        K_VS = tks(NT, "VS")
                        wq = sb("swq", [128, 8, 256], BF16, ph)
                        wkv = sb("swkv", [128, 8, 256], BF16, ph)
                        K_wq, K_wkv = Tk(), Tk()
                        msk = sb("smask", [128, 4, 2, 128], BF16, ph)
                        K_msk = Tk()
                        esink = sb("esink", [128, 4], F32, ph)
                        K_es = Tk()
                        wload(wq[:], K_wq, WIN[:, O_SQ:O_SQ + 256])
                        wload(wkv[:], K_wkv, WIN[:, O_SK:O_SK + 256])
                        C.dma("sp", msk[:], cst["swa_mask"], writes=[K_msk])
                        C.dma("sp", esink[:], dr["attn_sinks"][l].partition_broadcast(128), writes=[K_es])
                        C.op("act", lambda e: e.activation(out=esink[:], in_=esink[:], func=AF.Exp), reads=[K_es], writes=[K_es])
                        C.op("dve", lambda e: e.memset(VS[:, :, :, 64:65], 1.0), writes=K_VS)
                        for h in range(4):
                            proj_fm(lambda g: (QS[h][:, g * 512:(g + 1) * 512], K_QS[h][g]), wq, K_wq, h * 64, 64)
                        for g2 in range(2):
                            proj_fm(lambda g: (KS[g2][:, g * 512:(g + 1) * 512], K_KS[g2][g]), wkv, K_wkv, g2 * 64, 64)

                        def v_ev(tt, pb):
                            C.op("act", lambda e: e.copy(out=VS[:, tt, :, 0:64], in_=PSB[pb][:, 0:128].rearrange("p (g d) -> p g d", g=2)),
                                 reads=[PSk[pb]], writes=[K_VS[tt]])
                        proj_tm(v_ev, wkv, K_wkv, 128, 128)
                        EX = sb("sEX", [128, 2, 512], BF16, ph)
                        PT = sb("sPT", [128, 2, 512], BF16, ph)
                        K_EX, K_PT = tks(2), tks(2)
                        den = sb("sden", [128, 2, 4], F32, ph)
                        K_den = tks(2)
                        it = 0
                        for tt in range(NT):
                            par = tt % 2
                            po = 4 + par
                            for g2 in range(2):
                                pb = it % 2
                                bi = it % 2
                                it += 1
                                pvs = (1,) if tt == 0 else (0, 1)
                                for hh in range(2):
                                    for pv in pvs:
                                        kt = tt - 1 + pv
                                        o = (hh * 2 + pv) * 128
                                        C.op("pe", lambda e: e.matmul(PSB[pb][:, o:o + 128], KS[g2][:, kt * 128:(kt + 1) * 128],
                                                                      QS[2 * g2 + hh][:, tt * 128:(tt + 1) * 128], start=True, stop=True),
                                             reads=[K_KS[g2][kt // 4], K_QS[2 * g2 + hh][tt // 4]], writes=[PSk[pb]],
                                             inc=(hh == 1 and pv == 1))
                                if tt == 0:
                                    exv = EX[:, bi, :].rearrange("p (a b c) -> p a b c", a=2, b=2)[:, :, 1, :]
                                    psv = PSB[pb][:].rearrange("p (a b c) -> p a b c", a=2, b=2)[:, :, 1, :]
                                    ptv = PT[:, bi, :].rearrange("p (a b c) -> p a b c", a=2, b=2)[:, :, 1, :]
                                    C.op("act", lambda e: e.activation(out=exv, in_=psv, func=AF.Exp, scale=0.125),
                                         reads=[PSk[pb]], writes=[K_EX[bi]])
                                    C.op("dve", lambda e: e.tensor_tensor(out=ptv, in0=exv, in1=msk[:, 2 * g2:2 * g2 + 2, 1, :], op=ALU.mult),
                                         reads=[K_EX[bi], K_msk], writes=[K_PT[bi]])
                                else:
                                    C.op("act", lambda e: e.activation(out=EX[:, bi, :], in_=PSB[pb][:], func=AF.Exp, scale=0.125),
                                         reads=[PSk[pb]], writes=[K_EX[bi]])
                                    C.op("dve", lambda e: e.tensor_tensor(out=PT[:, bi, :], in0=EX[:, bi, :],
                                                                          in1=msk[:, 2 * g2:2 * g2 + 2, :, :].rearrange("p a b c -> p (a b c)"),
                                                                          op=ALU.mult),
                                         reads=[K_EX[bi], K_msk], writes=[K_PT[bi]])
                                for hh in range(2):
                                    h = 2 * g2 + hh
                                    for pv in pvs:
                                        kt = tt - 1 + pv
                                        o = (hh * 2 + pv) * 128
                                        C.op("pe", lambda e: e.matmul(PSB[po][:, h * 65:(h + 1) * 65], PT[:, bi, o:o + 128], VS[:, kt, g2, :],
                                                                      start=(pv == pvs[0]), stop=(pv == 1)),
                                             reads=[K_PT[bi], K_VS[kt]], writes=[PSk[po]], inc=(pv == 1))
                            pov = PSB[po][:, 0:260].rearrange("p (h d) -> p h d", h=4)
                            C.op("dve", lambda e: e.tensor_tensor(out=den[:, par, :], in0=pov[:, :, 64], in1=esink[:], op=ALU.add),
                                 reads=[PSk[po], K_es], writes=[K_den[par]])
                            C.op("dve", lambda e: e.reciprocal(out=den[:, par, :], in_=den[:, par, :]), reads=[K_den[par]], writes=[K_den[par]])
                            C.op("dve", lambda e: e.tensor_tensor(out=ytile[:, par, :].rearrange("p (h d) -> p h d", h=4), in0=pov[:, :, 0:64],
                                                                  in1=den[:, par, :].unsqueeze(2).to_broadcast([128, 4, 64]), op=ALU.mult),
                                 reads=[PSk[po], K_den[par]], writes=[K_yt[par]])
                            emit_y(3, tt, par)
                        C.barrier()
                if stop_after == f"swa{l}":
                    tap(f"YT{l}", YT[:], [t for r in YTk for t in r], [128, 8, S], BF16)
                    break

                if "moba" not in SKIP:
                    with ExitStack() as ph:
                        QA = [sb(f"QA{h}", [76, S], BF16, ph) for h in range(4)]
                        K_QA = [tks(4, f"QA{h}_") for h in range(4)]
                        K_QAs = [tks(4, f"QAs{h}_") for h in range(4)]
                        KA = [sb(f"KA{h}", [76, S], BF16, ph) for h in range(4)]
                        K_KA = [tks(4, f"KA{h}_") for h in range(4)]
                        VM = sb("VM", [128, NT, 4, 65], BF16, ph)
                        K_VM = tks(NT, "VM")
                        past = sb("mpast", [128, 128], F32, ph)
                        notown = sb("mnotown", [128, 128], F32, ph)
                        phA = ExitStack()
                        wq = sb("mwq", [128, 8, 256], BF16, phA)
                        wk = sb("mwk", [128, 8, 256], BF16, phA)
                        wv = sb("mwv", [128, 8, 256], BF16, phA)
                        K_wq, K_wk, K_wv = Tk(), Tk(), Tk()
                        wload(wq[:], K_wq, WIN[:, O_MQ:O_MQ + 256])
                        wload(wk[:], K_wk, WIN[:, O_MK:O_MK + 256])
                        wload(wv[:], K_wv, WIN[:, O_MV:O_MV + 256])
                        K_aug = Tk()
                        for h in range(4):
                            C.dma("sp", QA[h][64:76, :], cst["moba_qa"][h], writes=[K_aug])
                            C.dma("sp", KA[h][64:76, :], cst["moba_ka"][h], writes=[K_aug])
                        K_pm = Tk()
                        C.dma("sp", past[:], cst["moba_past"][0].partition_broadcast(128), writes=[K_pm])
                        C.dma("sp", notown[:], cst["moba_notown"][0].partition_broadcast(128), writes=[K_pm])
                        C.op("dve", lambda e: e.memset(VM[:, :, :, 64:65], 1.0), writes=K_VM)
                        for h in range(4):
                            proj_fm(lambda g: (QA[h][0:64, g * 512:(g + 1) * 512], K_QA[h][g]), wq, K_wq, h * 64, 64)
                            proj_fm(lambda g: (KA[h][0:64, g * 512:(g + 1) * 512], K_KA[h][g]), wk, K_wk, h * 64, 64)

                        def vm_ev(tt, pb):
                            C.op("act", lambda e: e.copy(out=VM[:, tt, :, 0:64], in_=PSB[pb][:, 0:256].rearrange("p (g d) -> p g d", g=4)),
                                 reads=[PSk[pb]], writes=[K_VM[tt]])
                        proj_tm(vm_ev, wv, K_wv, 0, 256)
                        C.barrier()
                        phA.close()
                        phB = ExitStack()
                        kms = sb("kms", [64, 4, 8], F32, phB)
                        kmb = sb("kmb", [64, 4, 8], BF16, phB)
                        K_km = Tk()
                        for h in range(4):
                            C.op("dve", lambda e: e.tensor_reduce(out=kms[:, h, :], in_=KA[h][0:64, :].rearrange("p (n s) -> p n s", n=8),
                                                                  axis=AX.X, op=ALU.add),
                                 reads=K_KA[h], writes=[K_km])
                        C.op("dve", lambda e: e.tensor_copy(out=kmb[:], in_=kms[:]), reads=[K_km], writes=[K_km])
                        for h in range(4):
                            for tt in range(NT):
                                o = (h * NT + tt) * 8
                                C.op("pe", lambda e: e.matmul(PSB[0][:, o:o + 8], QA[h][0:64, tt * 128:(tt + 1) * 128], kmb[:, h, :],
                                                              start=True, stop=True),
                                     reads=[K_QA[h][tt // 4], K_km], writes=[PSk[0]], inc=(h == 3 and tt == NT - 1))
                        gm = sb("mgm", [128, 4, 128], F32, phB)
                        m8 = sb("mm8", [128, 64, 8], F32, phB)
                        selb = sb("mselb", [128, 4, 128], F32, phB)
                        SP = sb("mSP", [128, 64, 72], BF16, phB)
                        K_gm, K_m8, K_selb, K_SP = Tk(), Tk(), Tk(), Tk()
                        C.op("dve", lambda e: e.tensor_tensor(out=gm[:], in0=PSB[0][:].rearrange("p (h x) -> p h x", h=4),
                                                              in1=past[:].unsqueeze(1).to_broadcast([128, 4, 128]), op=ALU.add),
                             reads=[PSk[0], K_pm], writes=[K_gm])
                        gmv = gm[:].rearrange("p h (t n) -> p (h t) n", n=8)
                        for gi in range(64):
                            C.op("dve", lambda e: e.max(out=m8[:, gi, :], in_=gmv[:, gi, :]), reads=[K_gm], writes=[K_m8])
                        C.op("dve", lambda e: e.tensor_tensor(out=selb[:].rearrange("p h (t n) -> p (h t) n", n=8), in0=gmv,
                                                              in1=m8[:, :, 2:3].to_broadcast([128, 64, 8]), op=ALU.is_ge),
                             reads=[K_gm, K_m8], writes=[K_selb])
                        C.op("dve", lambda e: e.tensor_scalar(out=selb[:], in0=selb[:], scalar1=-1.0, scalar2=-NEG, op0=ALU.add, op1=ALU.mult),
                             reads=[K_selb], writes=[K_selb])
                        C.op("dve", lambda e: e.tensor_tensor(out=selb[:], in0=selb[:], in1=notown[:].unsqueeze(1).to_broadcast([128, 4, 128]),
                                                              op=ALU.mult),
                             reads=[K_selb, K_pm], writes=[K_selb])
                        C.op("dve", lambda e: e.memset(SP[:], 0.0), writes=[K_SP])
                        C.op("dve", lambda e: e.tensor_copy(out=SP[:, :, 64:72], in_=selb[:].rearrange("p h (t n) -> p (h t) n", n=8)),
                             reads=[K_selb], writes=[K_SP])
                        for h in range(4):
                            for g in range(4):
                                pb = 1 + (h * 4 + g) % 2
                                for i in range(4):
                                    tt = 4 * g + i
                                    C.op("pe", lambda e: e.matmul(PSB[pb][0:72, i * 128:(i + 1) * 128], SP[:, h * NT + tt, :], ident_bf[:],
                                                                  start=True, stop=True),
                                         reads=[K_SP, K_id], writes=[PSk[pb]], inc=(i == 3))
                                C.op("act", lambda e: e.copy(out=QA[h][64:72, g * 512:(g + 1) * 512], in_=PSB[pb][64:72, :]),
                                     reads=[PSk[pb], K_aug], writes=[K_QAs[h][g]])
                        C.barrier()
                        phB.close()
                        PTp = sb("mPTp", [128, 2, 8, 512], BF16, ph)
                        PTd = sb("mPTd", [128, 2, 384], BF16, ph)
                        K_PTp = [tks(8), tks(8)]
                        K_PTd = tks(2)
                        rd = sb("mrd", [128, 2, 4], F32, ph)
                        K_rd = tks(2)
                        it = 0
                        sc = 0
                        for b in range(8):
                            for h in range(4):
                                bi = it % 2
                                it += 1
                                qrd = [K_QA[h][b // 2], K_QAs[h][b // 2]]
                                for pr in range(b):
                                    pb = sc % 2
                                    sc += 1
                                    for j in range(2):
                                        kt = 2 * pr + j
                                        C.op("pe", lambda e: e.matmul(PSB[pb][:, j * 256:(j + 1) * 256], KA[h][:, kt * 128:(kt + 1) * 128],
                                                                      QA[h][:, b * 256:(b + 1) * 256], start=True, stop=True),
                                             reads=[K_KA[h][kt // 4], K_aug] + qrd, writes=[PSk[pb]], inc=(j == 1))
                                    C.op("act", lambda e: e.activation(out=PTp[:, bi, pr, :], in_=PSB[pb][:], func=AF.Exp, scale=0.125),
                                         reads=[PSk[pb]], writes=[K_PTp[bi][pr]])
                                pb = sc % 2
                                sc += 1
                                kt = 2 * b
                                C.op("pe", lambda e: e.matmul(PSB[pb][:, 0:256], KA[h][:, kt * 128:(kt + 1) * 128],
                                                              QA[h][:, b * 256:(b + 1) * 256], start=True, stop=True),
                                     reads=[K_KA[h][kt // 4], K_aug] + qrd, writes=[PSk[pb]], inc=False)
                                kt = 2 * b + 1
                                C.op("pe", lambda e: e.matmul(PSB[pb][:, 256:384], KA[h][:, kt * 128:(kt + 1) * 128],
                                                              QA[h][:, b * 256 + 128:(b + 1) * 256], start=True, stop=True),
                                     reads=[K_KA[h][kt // 4], K_aug] + qrd, writes=[PSk[pb]])
                                C.op("act", lambda e: e.activation(out=PTd[:, bi, :], in_=PSB[pb][:, 0:384], func=AF.Exp, scale=0.125),
                                     reads=[PSk[pb]], writes=[K_PTd[bi]])
                                C.op("dve", lambda e: e.tensor_tensor(out=PTd[:, bi, 0:128], in0=PTd[:, bi, 0:128], in1=tri[:], op=ALU.mult),
                                     reads=[K_PTd[bi], K_tri], writes=[K_PTd[bi]])
                                C.op("dve", lambda e: e.tensor_tensor(out=PTd[:, bi, 256:384], in0=PTd[:, bi, 256:384], in1=tri[:], op=ALU.mult),
                                     reads=[K_PTd[bi], K_tri], writes=[K_PTd[bi]])
                                for qi in range(2):
                                    po = 4 + qi
                                    for pr in range(b):
                                        for j in range(2):
                                            kt = 2 * pr + j
                                            C.op("pe", lambda e: e.matmul(PSB[po][:, h * 65:(h + 1) * 65],
                                                                          PTp[:, bi, pr, j * 256 + qi * 128:j * 256 + (qi + 1) * 128],
                                                                          VM[:, kt, h, :], start=(kt == 0), stop=False),
                                                 reads=[K_PTp[bi][pr], K_VM[kt]], writes=[PSk[po]], inc=False)
                                    C.op("pe", lambda e: e.matmul(PSB[po][:, h * 65:(h + 1) * 65], PTd[:, bi, qi * 128:(qi + 1) * 128],
                                                                  VM[:, 2 * b, h, :], start=(b == 0), stop=(qi == 0)),
                                         reads=[K_PTd[bi], K_VM[2 * b]], writes=[PSk[po]], inc=(qi == 0))
                                    if qi == 1:
                                        C.op("pe", lambda e: e.matmul(PSB[po][:, h * 65:(h + 1) * 65], PTd[:, bi, 256:384],
                                                                      VM[:, 2 * b + 1, h, :], start=False, stop=True),
                                             reads=[K_PTd[bi], K_VM[2 * b + 1]], writes=[PSk[po]])
                            for qi in range(2):
                                tt = 2 * b + qi
                                po = 4 + qi
                                par = qi
                                pov = PSB[po][:, 0:260].rearrange("p (h d) -> p h d", h=4)
                                C.op("dve", lambda e: e.reciprocal(out=rd[:, par, :], in_=pov[:, :, 64]), reads=[PSk[po]], writes=[K_rd[par]])
                                C.op("dve", lambda e: e.tensor_tensor(out=ytile[:, par, :].rearrange("p (h d) -> p h d", h=4), in0=pov[:, :, 0:64],
                                                                      in1=rd[:, par, :].unsqueeze(2).to_broadcast([128, 4, 64]), op=ALU.mult),
                                     reads=[PSk[po], K_rd[par]], writes=[K_yt[par]])
                                emit_y(0, tt, par)
                        C.barrier()
                if stop_after == f"moba{l}":
                    tap(f"YT{l}", YT[:], [t for r in YTk for t in r], [128, 8, S], BF16)
                    break

                for br in (("ret",) if stop_after in (f"retonly{l}", f"retall{l}") else tuple(b_ for b_ in ("gla", "ret") if b_ not in SKIP)):
                    with ExitStack() as ph:
                        gla = br == "gla"
                        ncol = 784 if gla else 1024
                        wg = sb("lw", [128, 8, ncol], BF16, ph)
                        K_wg = Tk()
                        wload(wg[:], K_wg, WIN[:, (O_GQ if gla else O_RQ):(O_GQ if gla else O_RQ) + ncol])
                        gbc = sb("lgbc", [128, 256], F32, ph)
                        K_gbc = Tk()
                        C.dma("sp", gbc[:], dr["g_gla_norm" if gla else "g_ret_norm"][l].partition_broadcast(128), writes=[K_gbc])
                        K_cn = Tk()
                        if gla:
                            wgg = sb("lwgg", [32, 128], BF16, ph)
                            C.dma("pool", wgg[0:16, :], dr["w_gla_gate"][l], writes=[K_cn])
                            C.dma("pool", wgg[16:17, :], dr["b_gla_gate"][l:l + 1, :], writes=[K_cn])
                            cinc = sb("lcinc", [128, 128], F32, ph)
                            caft = sb("lcaft", [128, 128], F32, ph)
                            C.dma("sp", cinc[:], cst["cum_incl"], writes=[K_cn])
                            C.dma("sp", caft[:], cst["cum_after"], writes=[K_cn])
                            gaT = sb("lgaT", [32, 2, 128], BF16, ph)
                            K_gaT = tks(2)
                            C.op("dve", lambda e: e.memset(gaT[:], 1.0), writes=K_gaT)
                            LA = sb("lLA", [128, 2, 128], F32, ph)
                            K_LA = tks(2)
                            EB = sb("lEB", [128, 2, 3, 128], F32, ph)
                            K_EB = tks(2)
                            dec = sb("ldec", [128, 2], F32, ph)
                            nft = 1
                            kd = 32
                        else:
                            rqc = sb("lrqc", [128, 2, 128], F32, ph)
                            rkc = sb("lrkc", [128, 2, 128], F32, ph)
                            rkend = sb("lrkend", [128, 256], F32, ph)
                            rdec = sb("lrdec", [128, 2], F32, ph)
                            C.dma("sp", rqc[:], cst["ret_q"], writes=[K_cn])
                            C.dma("sp", rkc[:], cst["ret_k"], writes=[K_cn])
                            C.dma("sp", rkend[:], cst["ret_kend"], writes=[K_cn])
                            K_rd_ = Tk()
                            for h_ in range(4):
                                gv_ = float((1.0 - 2.0 ** (-5.0 - h_)) ** 128.0)
                                o_ = (h_ % 2) * 64
                                C.op("dve", lambda e: e.memset(rdec[o_:o_ + 64, h_ // 2:h_ // 2 + 1], gv_), writes=[K_rd_])
                            nft = 2
                            kd = 64
                        qd = sb("lqd", [128, 2, nft, 128], BF16, ph)
                        kin = sb("lkin", [128, 2, 4, 128], BF16, ph)
                        K_kin = tks(2)
                        C.op("dve", lambda e: e.memset(kin[:], 0.0), writes=K_kin)
                        SW = 256 if gla else 128
                        bdm = sb("lbdm", [128, SW], F32, ph)
                        C.dma("sp", bdm[:], cst["bd_gla" if gla else "bd_ret"], writes=[K_cn])
                        kvt = sb("lkvt", [128, 2, SW], F32, ph)
                        K_kvt = tks(2)
                        kend = sb("lkend", [128, 2, nft * 128], BF16, ph)
                        vv = sb("lvv", [128, 2, 256], BF16, ph)
                        ggr = sb("lggr", [128, 2, 256], F32, ph)
                        atm = sb("latm", [128, 2, 4, 128], BF16, ph)
                        K_qd, K_kend, K_vv, K_ggr, K_atm = tks(2), tks(2), tks(2), tks(2), tks(2)
                        Sf = sb("lSf", [128, 2, nft, SW], F32, ph)
                        Sb = sb("lSb", [128, 2, nft, SW], BF16, ph)
                        K_Sf, K_Sb = tks(2), tks(2)
                        C.op("dve", lambda e: e.memset(Sf[:], 0.0), writes=K_Sf)
                        sq = sb("lsq", [128, 2, 256], F32, ph)
                        st = sb("lst", [128, 2, 3, 4], F32, ph)
                        K_sq, K_st = tks(2), tks(2)
                        t1 = sb("lt1", [128, 2, 256], F32, ph)
                        K_t1 = tks(2)
                        for tt in range((NT if gla else RETTILES) if stop_after != f"retonly{l}" else 1):
                            if RETV in (32, 33) and tt > 0 and not gla:
                                C.barrier()
                            _rc = RETCUT
                            if RETV in (20, 22, 32, 30):
                                _rc = 3 if tt == 0 else 2
                            if RETV == 21:
                                _rc = 3 if tt == 0 else 1
                            p = tt % 2 if RETV != 4 else 0
                            q = 1 - p
                            hr = HTr(tt // 4)
                            tok = slice(tt * 128, (tt + 1) * 128)

                            def mmK(out, lhs, rhs, wr, inc8=True):
                                for kt in range(8):
                                    C.op("pe", lambda e: e.matmul(out, lhs(kt), rhs(kt), start=(kt == 0), stop=(kt == 7)),
                                         reads=[K_wg] + hr, writes=[wr], inc=(kt == 7))
                            for j in range(nft):
                                mmK(PSB[0][:, j * 128:(j + 1) * 128], lambda kt: wg[:, kt, j * 128:(j + 1) * 128], lambda kt: HT[:, kt, tok], PSk[0])
                                ko = 128 if gla else 256
                                mmK(PSB[0][:, (nft + j) * 128:(nft + j + 1) * 128], lambda kt: wg[:, kt, ko + j * 128:ko + (j + 1) * 128],
                                    lambda kt: HT[:, kt, tok], PSk[0])
                            if gla:
                                mmK(PSB[1][0:16, 0:128], lambda kt: wg[:, kt, 512:528], lambda kt: HT[:, kt, tok], PSk[1])
                                C.op("act", lambda e: e.copy(out=gaT[0:16, p, :], in_=PSB[1][0:16, 0:128]), reads=[PSk[1]], writes=[K_gaT[p]])
                                C.op("pe", lambda e: e.matmul(PSB[1][:, 128:256], gaT[0:17, p, :], wgg[0:17, :], start=True, stop=True),
                                     reads=[K_gaT[p], K_cn], writes=[PSk[1]])
                                C.op("act", lambda e: e.activation(out=LA[:, p, :], in_=PSB[1][:, 128:256], func=AF.Exp, scale=-1.0),
                                     reads=[PSk[1]], writes=[K_LA[p]])
                                C.op("act", lambda e: e.activation(out=LA[:, p, :], in_=LA[:, p, :], func=AF.Ln, bias=1.0),
                                     reads=[K_LA[p]], writes=[K_LA[p]])
                                C.op("pe", lambda e: e.matmul(PSB[1][:, 256:384], LA[:, p, :], cinc[:], start=True, stop=True),
                                     reads=[K_LA[p], K_cn], writes=[PSk[1]])
                                C.op("pe", lambda e: e.matmul(PSB[1][:, 384:512], caft[:], LA[:, p, :], start=True, stop=True),
                                     reads=[K_LA[p], K_cn], writes=[PSk[1]])
                                C.op("act", lambda e: e.activation(out=EB[:, p, 0, :], in_=PSB[1][:, 256:384], func=AF.Exp), reads=[PSk[1]], writes=[K_EB[p]])
                                C.op("act", lambda e: e.activation(out=EB[:, p, 1, :], in_=PSB[1][:, 256:384], func=AF.Exp, scale=-1.0), reads=[PSk[1]], writes=[K_EB[p]])
                                C.op("act", lambda e: e.activation(out=EB[:, p, 2, :], in_=PSB[1][:, 384:512], func=AF.Exp), reads=[PSk[1]], writes=[K_EB[p]])
                                C.op("dve", lambda e: e.scalar_tensor_tensor(out=qd[:, p, 0, :], in0=PSB[0][:, 0:128], scalar=32.0 ** -0.5, in1=EB[:, p, 0, :],
                                                                             op0=ALU.mult, op1=ALU.mult), reads=[PSk[0], K_EB[p]], writes=[K_qd[p]])
                                for h in range(4):
                                    o = 32 * h
                                    C.op("dve", lambda e: e.tensor_tensor(out=kin[o:o + 32, p, h, :], in0=PSB[0][o:o + 32, 128:256], in1=EB[o:o + 32, p, 1, :], op=ALU.mult),
                                         reads=[PSk[0], K_EB[p]], writes=[K_kin[p]])
                            else:
                                if RETV in (10, 12):
                                    mmK(PSB[1][0:16, 256:384], lambda kt: wg[:, kt, 0:16], lambda kt: HT[:, kt, tok], PSk[1])
                                if RETV in (11, 12):
                                    C.op("pe", lambda e: e.matmul(PSB[1][:, 384:512], rqc[:, 0, :], rkc[:, 0, :], start=True, stop=True),
                                         reads=[K_cn], writes=[PSk[1]])
                                C.op("dve", lambda e: e.tensor_tensor(out=qd[:, p, :, :], in0=PSB[0][:, 0:256].rearrange("p (j x) -> p j x", j=2),
                                                                      in1=rqc[:], op=ALU.mult), reads=[PSk[0], K_cn], writes=[K_qd[p]])
                                for h in range(4):
                                    j, o = h // 2, (h % 2) * 64
                                    C.op("dve", lambda e: e.tensor_tensor(out=kin[o:o + 64, p, h, :], in0=PSB[0][o:o + 64, 256 + j * 128:256 + (j + 1) * 128],
                                                                          in1=rkc[o:o + 64, j, :], op=ALU.mult), reads=[PSk[0], K_cn], writes=[K_kin[p]])
                            if _rc <= 1:
                                continue
                            nk = nft * 128
                            ko = 128 if gla else 256
                            if RETV == 30:
                                mmK(PSB[2][:, 0:nk], lambda kt: HT[:, kt, tok], lambda kt: wg[:, kt, ko:ko + nk], PSk[2])
                                mmK(PSB[2][:, nk:nk + 256], lambda kt: HT[:, kt, tok], lambda kt: wg[:, kt, ko + nk:ko + nk + 256], PSk[2])
                            else:
                                mmK(PSB[2][:, 0:nk + 256], lambda kt: HT[:, kt, tok], lambda kt: wg[:, kt, ko:ko + nk + 256], PSk[2])
                            go = 528 if gla else 768
                            mmK(PSB[3][:, 0:256], lambda kt: HT[:, kt, tok], lambda kt: wg[:, kt, go:go + 256], PSk[3])
                            if gla:
                                C.op("dve", lambda e: e.tensor_tensor(out=kend[:, p, :], in0=PSB[2][:, 0:128], in1=EB[:, p, 2, :], op=ALU.mult),
                                     reads=[PSk[2], K_EB[p]], writes=[K_kend[p]])
                            else:
                                C.op("dve", lambda e: e.tensor_tensor(out=kend[:, p, :], in0=PSB[2][:, 0:256], in1=rkend[:], op=ALU.mult),
                                     reads=[PSk[2], K_cn], writes=[K_kend[p]])
                            C.op("act", lambda e: e.copy(out=vv[:, p, :], in_=PSB[2][:, nk:nk + 256]), reads=[PSk[2]], writes=[K_vv[p]])
                            C.op("act", lambda e: e.activation(out=ggr[:, p, :], in_=PSB[3][:, 0:256], func=AF.Silu), reads=[PSk[3]], writes=[K_ggr[p]])
                            C.op("dve", lambda e: e.tensor_tensor(out=ggr[:, p, :], in0=ggr[:, p, :], in1=gbc[:], op=ALU.mult),
                                 reads=[K_ggr[p], K_gbc], writes=[K_ggr[p]])
                            if _rc <= 2:
                                continue
                            for h in range(4 if not (RETV == 9 and tt == 1) else 0):
                                j = 0 if gla else h // 2
                                C.op("pe", lambda e: e.matmul(PSB[4][:, h * 128:(h + 1) * 128], kin[:, p, h, :], qd[:, p, j, :],
                                                              start=True, stop=True),
                                     reads=[K_kin[p], K_qd[p]], writes=[PSk[4]], inc=(h == 3))
                            if not (RETV in (9, 13) and tt == 1):
                                C.op("dve", lambda e: e.tensor_tensor(out=atm[:, p, :, :], in0=PSB[4][:].rearrange("p (h x) -> p h x", h=4),
                                                                      in1=tri[:].unsqueeze(1).to_broadcast([128, 4, 128]), op=ALU.mult),
                                     reads=[PSk[4], K_tri], writes=[K_atm[p]])
                            obank = [5, 6]
                            if tt > 0 and RETV not in (1, 5, 6, 7, 8):
                                for j in range(nft if RETV != 2 else 1):
                                    C.op("pe", lambda e: e.matmul(PSB[obank[j]][:, 0:SW], qd[:, p, j, :], Sb[:, q, j, :], start=True, stop=False),
                                         reads=[K_qd[p], K_Sb[q]], writes=[PSk[obank[j]]], inc=(RETV == 3))
                            for h in range(4):
                                if (tt == 1 and RETV in (5, 13)) or (tt == 0 and RETV == 22) or (tt == 1 and False) or (tt == 1 and (False or (RETV == 6 and h >= 2) or (RETV == 8 and h < 2))):
                                    continue
                                j, oc = (0, h * 64) if gla else (h // 2, (h % 2) * 64)
                                C.op("pe", lambda e: e.matmul(PSB[obank[j]][:, oc:oc + 64], atm[:, p, h, :], vv[:, p, h * 64:(h + 1) * 64],
                                                              start=(tt == 0 or RETV in (1, 5, 6, 7, 8, 9, 13) or (RETV == 2 and h >= 2)), stop=(tt == 0 or h == 3 or (not gla and h == 1))),
                                     reads=[K_atm[p], K_vv[p]], writes=[PSk[obank[j]]], inc=True)
                            if _rc <= 3:
                                continue
                            if tt < NT - 1:
                                kvb = 1 if not gla else 3
                                for j in range(nft):
                                    C.op("pe", lambda e: e.matmul(PSB[kvb][:, 256:256 + SW] if gla else PSB[kvb][:, j * 128:(j + 1) * 128],
                                                                  kend[:, p, j * 128:(j + 1) * 128], vv[:, p, j * SW:(j + 1) * SW] if not gla else vv[:, p, :],
                                                                  start=True, stop=True),
                                         reads=[K_kend[p], K_vv[p]], writes=[PSk[kvb]])
                                    src = PSB[kvb][:, 256:256 + SW] if gla else PSB[kvb][:, j * 128:(j + 1) * 128]
                                    C.op("dve", lambda e: e.tensor_tensor(out=kvt[:, j if not gla else 0, :], in0=src, in1=bdm[:], op=ALU.mult),
                                         reads=[PSk[kvb], K_cn], writes=[K_kvt[j]])
                                    if gla:
                                        C.op("act", lambda e: e.copy(out=dec[:, p:p + 1], in_=EB[:, p, 0, 127:128]), reads=[K_EB[p]], writes=[K_EB[p]])
                                        dsc = dec[:, p:p + 1]
                                    else:
                                        dsc = rdec[:, j:j + 1]
                                    C.op("dve", lambda e: e.scalar_tensor_tensor(out=Sf[:, p, j, :], in0=Sf[:, q, j, :], scalar=dsc,
                                                                                 in1=kvt[:, j if not gla else 0, :], op0=ALU.mult, op1=ALU.add),
                                         reads=[K_Sf[q], K_kvt[j], K_EB[p] if gla else K_cn], writes=[K_Sf[p]])
                                C.op("act", lambda e: e.copy(out=Sb[:, p, :, :], in_=Sf[:, p, :, :]), reads=[K_Sf[p]], writes=[K_Sb[p]])
                            if _rc <= 4:
                                continue
                            osb = t1
                            for j in range(nft):
                                C.op("act", lambda e: e.copy(out=t1[:, p, j * SW:(j + 1) * SW], in_=PSB[obank[j]][:, 0:SW]), reads=[PSk[obank[j]]], writes=[K_t1[p]])
                            ov = t1[:, p, :].rearrange("p (h d) -> p h d", h=4)
                            C.op("act", lambda e: e.activation(out=sq[:, p, :], in_=t1[:, p, :], func=AF.Square), reads=[K_t1[p]], writes=[K_sq[p]])
                            C.op("dve", lambda e: e.tensor_reduce(out=st[:, p, 0, :], in_=sq[:, p, :].rearrange("p (h d) -> p h d", h=4), axis=AX.X, op=ALU.add),
                                 reads=[K_sq[p]], writes=[K_st[p]])
                            if _rc <= 4.2:
                                continue
                            if gla:
                                C.op("dve", lambda e: e.tensor_scalar(out=st[:, p, 0, :], in0=st[:, p, 0, :], scalar1=1.0 / 64, scalar2=EPS, op0=ALU.mult, op1=ALU.add),
                                     reads=[K_st[p]], writes=[K_st[p]])
                            else:
                                C.op("dve", lambda e: e.tensor_reduce(out=st[:, p, 1, :], in_=ov, axis=AX.X, op=ALU.add), reads=[K_t1[p]], writes=[K_st[p]])
                                C.op("dve", lambda e: e.tensor_scalar(out=st[:, p, 1, :], in0=st[:, p, 1, :], scalar1=-1.0 / 64, scalar2=None, op0=ALU.mult),
                                     reads=[K_st[p]], writes=[K_st[p]])
                                C.op("dve", lambda e: e.tensor_tensor(out=st[:, p, 2, :], in0=st[:, p, 1, :], in1=st[:, p, 1, :], op=ALU.mult),
                                     reads=[K_st[p]], writes=[K_st[p]])
                                C.op("dve", lambda e: e.scalar_tensor_tensor(out=st[:, p, 0, :], in0=st[:, p, 0, :], scalar=1.0 / 64, in1=st[:, p, 2, :],
                                                                             op0=ALU.mult, op1=ALU.subtract), reads=[K_st[p]], writes=[K_st[p]])
                                C.op("dve", lambda e: e.tensor_scalar(out=st[:, p, 0, :], in0=st[:, p, 0, :], scalar1=EPS, scalar2=None, op0=ALU.add),
                                     reads=[K_st[p]], writes=[K_st[p]])
                            if _rc <= 4.4:
                                continue
                            C.op("act", lambda e: e.activation(out=st[:, p, 0, :], in_=st[:, p, 0, :], func=AF.Sqrt), reads=[K_st[p]], writes=[K_st[p]])
                            C.op("dve", lambda e: e.reciprocal(out=st[:, p, 0, :], in_=st[:, p, 0, :]), reads=[K_st[p]], writes=[K_st[p]])
                            if _rc <= 4.6:
                                continue
                            t1v = t1[:, p, :].rearrange("p (h d) -> p h d", h=4)
                            if gla:
                                C.op("dve", lambda e: e.tensor_tensor(out=t1v, in0=ov, in1=st[:, p, 0, :].unsqueeze(2).to_broadcast([128, 4, 64]), op=ALU.mult),
                                     reads=[K_t1[p], K_st[p]], writes=[K_t1[p]])
                            else:
                                C.op("dve", lambda e: e.tensor_tensor(out=t1v, in0=ov, in1=st[:, p, 1, :].unsqueeze(2).to_broadcast([128, 4, 64]), op=ALU.add),
                                     reads=[K_t1[p], K_st[p]], writes=[K_t1[p]])
                                C.op("dve", lambda e: e.tensor_tensor(out=t1v, in0=t1v, in1=st[:, p, 0, :].unsqueeze(2).to_broadcast([128, 4, 64]), op=ALU.mult),
                                     reads=[K_t1[p], K_st[p]], writes=[K_t1[p]])
                            if _rc <= 4.8:
                                continue
                            C.op("dve", lambda e: e.tensor_tensor(out=ytile[:, p, :], in0=t1[:, p, :], in1=ggr[:, p, :], op=ALU.mult),
                                 reads=[K_t1[p], K_ggr[p]], writes=[K_yt[p]])
                            if _rc <= 5:
                                continue
                            emit_y(1 if gla else 2, tt, p)
                        C.barrier()
                    if stop_after == f"{br}{l}":
                        break
                    if stop_after == f"retall{l}" and "memsetYT" not in SKIP:
                        pass
                if "retq" not in SKIP:
                    with ExitStack() as ph:
                        VR = sb("VR", [128, NT, 4, 64], BF16, ph)
                        K_VR = tks(NT, "VR")
                        GG = sb("GG", [128, NT, 256], BF16, ph)
                        K_GG = tks(NT, "GG")
                        gbc = sb("rgbc", [128, 256], F32, ph)
                        K_gbc = Tk()
                        C.dma("sp", gbc[:], dr["g_ret_norm"][l].partition_broadcast(128), writes=[K_gbc])
                        gtmp = sb("rgtmp", [128, 2, 256], F32, ph)
                        K_gtmp = tks(2)
                        PTp = sb("rPTp", [128, 8, 512], BF16, ph)
                        PTd = sb("rPTd", [128, 2, 384], BF16, ph)
                        K_PTp = tks(8)
                        K_PTd = tks(2)
                        t1 = sb("rt1", [128, 4, 128], F32, ph)
                        sq = sb("rsq", [128, 4, 128], F32, ph)
                        st = sb("rst", [128, 4, 3, 2], F32, ph)
                        K_t1, K_sq, K_st = tks(4), tks(4), tks(4)
                        QR = [sb(f"QR{i}", [64, S], BF16, ph) for i in range(2)]
                        KR = [sb(f"KR{i}", [64, S], BF16, ph) for i in range(2)]
                        K_QR = [tks(4), tks(4)]
                        K_KR = [tks(4), tks(4)]
                        with ExitStack() as phA:
                            wvg = sb("rwvg", [128, 8, 512], BF16, phA)
                            K_wvg = Tk()
                            wload(wvg[:], K_wvg, WIN[:, O_RV:O_RV + 512])

                            def vr_ev(tt, pb):
                                C.op("act", lambda e: e.copy(out=VR[:, tt, :, :], in_=PSB[pb][:, 0:256].rearrange("p (g d) -> p g d", g=4)),
                                     reads=[PSk[pb]], writes=[K_VR[tt]])
                            proj_tm(vr_ev, wvg, K_wvg, 0, 256)

                            def gg_ev(tt, pb):
                                bi = tt % 2
                                C.op("act", lambda e: e.activation(out=gtmp[:, bi, :], in_=PSB[pb][:, 0:256], func=AF.Silu), reads=[PSk[pb]], writes=[K_gtmp[bi]])
                                C.op("dve", lambda e: e.tensor_tensor(out=GG[:, tt, :], in0=gtmp[:, bi, :], in1=gbc[:], op=ALU.mult),
                                     reads=[K_gtmp[bi], K_gbc], writes=[K_GG[tt]])
                            proj_tm(gg_ev, wvg, K_wvg, 256, 256)
                            C.barrier()
                        for pair in range(2):
                            with ExitStack() as phB:
                                wqk = sb("rwqk", [128, 8, 256], BF16, phB)
                                K_wqk = Tk()
                                wload(wqk[:, :, 0:128], K_wqk, WIN[:, O_RQ + pair * 128:O_RQ + (pair + 1) * 128])
                                wload(wqk[:, :, 128:256], K_wqk, WIN[:, O_RK + pair * 128:O_RK + (pair + 1) * 128])
                                dq = sb("rdq", [64, 1, S], BF16, phB)
                                dk = sb("rdk", [64, 1, S], BF16, phB)
                                K_dqk = Tk()
                                ev = 0
                                for hh in range(2):
                                    C.dma("pool", dq[:, 0, :], cst["ret_qd"][2 * pair + hh].partition_broadcast(64), writes=[K_dqk])
                                    C.dma("pool", dk[:, 0, :], cst["ret_kd"][2 * pair + hh].partition_broadcast(64), writes=[K_dqk])
                                    for which in range(2):
                                        for g in range(4):
                                            pb = ev % 2
                                            ev += 1
                                            c0 = which * 128 + hh * 64
                                            for kt in range(8):
                                                C.op("pe", lambda e: e.matmul(PSB[pb][0:64, :], wqk[:, kt, c0:c0 + 64], HT[:, kt, g * 512:(g + 1) * 512],
                                                                              start=(kt == 0), stop=(kt == 7)),
                                                     reads=[K_wqk] + HTr(g), writes=[PSk[pb]], inc=(kt == 7))
                                            dst = (QR if which == 0 else KR)[hh][:, g * 512:(g + 1) * 512]
                                            dtk = (K_QR if which == 0 else K_KR)[hh][g]
                                            dec_ = (dq if which == 0 else dk)[:, 0, g * 512:(g + 1) * 512]
                                            C.op("dve", lambda e: e.tensor_tensor(out=dst, in0=PSB[pb][0:64, :], in1=dec_, op=ALU.mult),
                                                 reads=[PSk[pb], K_dqk], writes=[dtk])
                                C.barrier()
                            sc = 0
                            for b in range(8):
                                for hh in range(2):
                                    h = 2 * pair + hh
                                    for pr in range(b):
                                        pb = sc % 2
                                        sc += 1
                                        for j in range(2):
                                            kt = 2 * pr + j
                                            C.op("pe", lambda e: e.matmul(PSB[pb][:, j * 256:(j + 1) * 256], KR[hh][:, kt * 128:(kt + 1) * 128],
                                                                          QR[hh][:, b * 256:(b + 1) * 256], start=True, stop=True),
                                                 reads=[K_KR[hh][kt // 4], K_QR[hh][b // 2]], writes=[PSk[pb]], inc=(j == 1))
                                        C.op("act", lambda e: e.activation(out=PTp[:, pr, :], in_=PSB[pb][:], func=AF.Identity, scale=0.125),
                                             reads=[PSk[pb]], writes=[K_PTp[pr]])
                                    pb = sc % 2
                                    sc += 1
                                    bi = hh
                                    kt = 2 * b
                                    C.op("pe", lambda e: e.matmul(PSB[pb][:, 0:256], KR[hh][:, kt * 128:(kt + 1) * 128],
                                                                  QR[hh][:, b * 256:(b + 1) * 256], start=True, stop=True),
                                         reads=[K_KR[hh][kt // 4], K_QR[hh][b // 2]], writes=[PSk[pb]], inc=False)
                                    kt = 2 * b + 1
                                    C.op("pe", lambda e: e.matmul(PSB[pb][:, 256:384], KR[hh][:, kt * 128:(kt + 1) * 128],
                                                                  QR[hh][:, b * 256 + 128:(b + 1) * 256], start=True, stop=True),
                                         reads=[K_KR[hh][kt // 4], K_QR[hh][b // 2]], writes=[PSk[pb]])
                                    C.op("act", lambda e: e.activation(out=PTd[:, bi, :], in_=PSB[pb][:, 0:384], func=AF.Identity, scale=0.125),
                                         reads=[PSk[pb]], writes=[K_PTd[bi]])
                                    C.op("dve", lambda e: e.tensor_tensor(out=PTd[:, bi, 0:128], in0=PTd[:, bi, 0:128], in1=tri[:], op=ALU.mult),
                                         reads=[K_PTd[bi], K_tri], writes=[K_PTd[bi]])
                                    C.op("dve", lambda e: e.tensor_tensor(out=PTd[:, bi, 256:384], in0=PTd[:, bi, 256:384], in1=tri[:], op=ALU.mult),
                                         reads=[K_PTd[bi], K_tri], writes=[K_PTd[bi]])
                                    for qi in range(2):
                                        po = 4 + qi
                                        for pr in range(b):
                                            for j in range(2):
                                                kt = 2 * pr + j
                                                C.op("pe", lambda e: e.matmul(PSB[po][:, hh * 64:(hh + 1) * 64],
                                                                              PTp[:, pr, j * 256 + qi * 128:j * 256 + (qi + 1) * 128],
                                                                              VR[:, kt, h, :], start=(kt == 0), stop=False),
                                                     reads=[K_PTp[pr], K_VR[kt]], writes=[PSk[po]], inc=False)
                                        C.op("pe", lambda e: e.matmul(PSB[po][:, hh * 64:(hh + 1) * 64], PTd[:, bi, qi * 128:(qi + 1) * 128],
                                                                      VR[:, 2 * b, h, :], start=(b == 0), stop=(qi == 0)),
                                             reads=[K_PTd[bi], K_VR[2 * b]], writes=[PSk[po]], inc=(qi == 0))
                                        if qi == 1:
                                            C.op("pe", lambda e: e.matmul(PSB[po][:, hh * 64:(hh + 1) * 64], PTd[:, bi, 256:384],
                                                                          VR[:, 2 * b + 1, h, :], start=False, stop=True),
                                                 reads=[K_PTd[bi], K_VR[2 * b + 1]], writes=[PSk[po]])
                                def chain_head(b):
                                    for qi in range(2):
                                        po = 4 + qi
                                        p = (b % 2) * 2 + qi
                                        C.op("act", lambda e: e.copy(out=t1[:, p, :], in_=PSB[po][:, 0:128]), reads=[PSk[po]], writes=[K_t1[p]])
                                        C.op("act", lambda e: e.activation(out=sq[:, p, :], in_=t1[:, p, :], func=AF.Square), reads=[K_t1[p]], writes=[K_sq[p]])

                                def chain_tail(b):
                                    for qi in range(2):
                                        tt = 2 * b + qi
                                        p = (b % 2) * 2 + qi
                                        yp = qi
                                        ov = t1[:, p, :].rearrange("p (h d) -> p h d", h=2)
                                        C.op("dve", lambda e: e.tensor_reduce(out=st[:, p, 0, :], in_=sq[:, p, :].rearrange("p (h d) -> p h d", h=2), axis=AX.X, op=ALU.add),
                                             reads=[K_sq[p]], writes=[K_st[p]])
                                        C.op("dve", lambda e: e.tensor_reduce(out=st[:, p, 1, :], in_=ov, axis=AX.X, op=ALU.add), reads=[K_t1[p]], writes=[K_st[p]])
                                        C.op("dve", lambda e: e.tensor_scalar(out=st[:, p, 1, :], in0=st[:, p, 1, :], scalar1=-1.0 / 64, scalar2=None, op0=ALU.mult),
                                             reads=[K_st[p]], writes=[K_st[p]])
                                        C.op("dve", lambda e: e.tensor_tensor(out=st[:, p, 2, :], in0=st[:, p, 1, :], in1=st[:, p, 1, :], op=ALU.mult),
                                             reads=[K_st[p]], writes=[K_st[p]])
                                        C.op("dve", lambda e: e.scalar_tensor_tensor(out=st[:, p, 0, :], in0=st[:, p, 0, :], scalar=1.0 / 64, in1=st[:, p, 2, :],
                                                                                     op0=ALU.mult, op1=ALU.subtract), reads=[K_st[p]], writes=[K_st[p]])
                                        C.op("dve", lambda e: e.tensor_scalar(out=st[:, p, 0, :], in0=st[:, p, 0, :], scalar1=EPS, scalar2=None, op0=ALU.add),
                                             reads=[K_st[p]], writes=[K_st[p]])
                                        C.op("act", lambda e: e.activation(out=st[:, p, 0, :], in_=st[:, p, 0, :], func=AF.Sqrt), reads=[K_st[p]], writes=[K_st[p]])
                                        C.op("dve", lambda e: e.reciprocal(out=st[:, p, 0, :], in_=st[:, p, 0, :]), reads=[K_st[p]], writes=[K_st[p]])
                                        C.op("dve", lambda e: e.tensor_tensor(out=ov, in0=ov, in1=st[:, p, 1, :].unsqueeze(2).to_broadcast([128, 2, 64]), op=ALU.add),
                                             reads=[K_t1[p], K_st[p]], writes=[K_t1[p]])
                                        C.op("dve", lambda e: e.tensor_tensor(out=ov, in0=ov, in1=st[:, p, 0, :].unsqueeze(2).to_broadcast([128, 2, 64]), op=ALU.mult),
                                             reads=[K_t1[p], K_st[p]], writes=[K_t1[p]])
                                        C.op("dve", lambda e: e.tensor_tensor(out=ytile[:, yp, 0:128], in0=t1[:, p, :], in1=GG[:, tt, pair * 128:(pair + 1) * 128], op=ALU.mult),
                                             reads=[K_t1[p], K_GG[tt]], writes=[K_yt[yp]])
                                        C.op("pe", lambda e: e.transpose(ps_bf(7)[:, 0:128], ytile[:, yp, 0:128], ident_bf[:]),
                                             reads=[K_yt[yp], K_id], writes=[PSk[7]])
                                        C.op("act", lambda e: e.copy(out=YT[:, 4 + pair, tt * 128:(tt + 1) * 128], in_=ps_bf(7)[:, 0:128]),
                                             reads=[PSk[7]], writes=[YTk[4 + pair][tt]])

                                if b > 0:
                                    chain_tail(b - 1)
                                chain_head(b)
                            chain_tail(7)
                            C.barrier()
                if stop_after == f"retq{l}":
                    tap(f"YT{l}", YT[:], [t for r in YTk for t in r], [128, 8, S], BF16)
                    break
                if stop_after in (f"gla{l}", f"ret{l}", f"retonly{l}", f"retall{l}"):
                    tap(f"YT{l}", YT[:], [t for r in YTk for t in r], [128, 8, S], BF16)
                    break

                with ExitStack() as ph:
                    MP = sb("MP", [128, 8, S], BF16, ph)
                    MPk = [tks(4, f"MP{f}_") for f in range(8)]
                    with ExitStack() as ph2:
                        wm = sb("wm", [128, 2, 8, 256], BF16, ph2)
                        wbr = sb("wbr", [128, 2, 2, D], BF16, ph2)
                        K_wm, K_wbr = tks(2), tks(2)
                        sig = sb("sig", [128, 2, 512], BF16, ph2)
                        prod = sb("prod", [128, 2, 512], F32, ph2)
                        K_sig, K_prod = tks(2), tks(2)
                        ci_ = 0
                        it = 0
                        for n in range(4):
                            wb_ = n % 2
                            for hf_ in range(2):
                                C.dma("pool", wbr[:, wb_, :, hf_ * 512:(hf_ + 1) * 512],
                                      dr["w_branch"][l][n].rearrange("(ci p) d -> p ci d", p=128)[:, :, hf_ * 512:(hf_ + 1) * 512], writes=[K_wbr[wb_]])
                            for fp in range(4):
                                w_ = ci_ % 2
                                ci_ += 1
                                c0 = O_MG + n * D + fp * 256
                                wload(wm[:, w_, :, :], K_wm[w_], WIN[:, c0:c0 + 256])
                                for fl in range(2):
                                    ft = fp * 2 + fl
                                    for g in range(4):
                                        b0, b1, bi = it % 2, 2 + it % 2, it % 2
                                        it += 1
                                        for kt in range(8):
                                            C.op("pe", lambda e: e.matmul(PSB[b0][:], wm[:, w_, kt, fl * 128:(fl + 1) * 128], HT[:, kt, g * 512:(g + 1) * 512],
                                                                          start=(kt == 0), stop=(kt == 7)),
                                                 reads=[K_wm[w_]] + HTr(g), writes=[PSk[b0]], inc=(kt == 7))
                                        for ci in range(2):
                                            C.op("pe", lambda e: e.matmul(PSB[b1][:], wbr[:, wb_, ci, ft * 128:(ft + 1) * 128], YT[:, 2 * n + ci, g * 512:(g + 1) * 512],
                                                                          start=(ci == 0), stop=(ci == 1)),
                                                 reads=[K_wbr[wb_]] + YTk[2 * n + ci][4 * g:4 * g + 4], writes=[PSk[b1]], inc=(ci == 1))
                                        C.op("act", lambda e: e.activation(out=sig[:, bi, :], in_=PSB[b0][:], func=AF.Sigmoid), reads=[PSk[b0]], writes=[K_sig[bi]])
                                        mp = MP[:, ft, g * 512:(g + 1) * 512]
                                        if n == 0:
                                            C.op("dve", lambda e: e.tensor_tensor(out=mp, in0=sig[:, bi, :], in1=PSB[b1][:], op=ALU.mult),
                                                 reads=[K_sig[bi], PSk[b1]], writes=[MPk[ft][g]])
                                        else:
                                            C.op("dve", lambda e: e.tensor_tensor(out=prod[:, bi, :], in0=sig[:, bi, :], in1=PSB[b1][:], op=ALU.mult),
                                                 reads=[K_sig[bi], PSk[b1]], writes=[K_prod[bi]])
                                            C.op("dve", lambda e: e.tensor_tensor(out=mp, in0=mp, in1=prod[:, bi, :], op=ALU.add),
                                                 reads=[K_prod[bi], MPk[ft][g]], writes=[MPk[ft][g]])
                        C.barrier()
                    with ExitStack() as ph2:
                        wo = sb("wo", [128, 8, D], BF16, ph2)
                        K_wo = Tk()
                        wload(wo[:], K_wo, dr["w_out"][l])
                        tmp = sb("otmp", [128, 2, 512], F32, ph2)
                        K_tmp = tks(2)
                        it = 0
                        for tt in range(NT):
                            for hf in range(2):
                                pb, bi = 4 + it % 4, it % 2
                                it += 1
                                for ft in range(8):
                                    C.op("pe", lambda e: e.matmul(PSB[pb][:], MP[:, ft, tt * 128:(tt + 1) * 128], wo[:, ft, hf * 512:(hf + 1) * 512],
                                                                  start=(ft == 0), stop=(ft == 7)),
                                         reads=[K_wo, MPk[ft][tt // 4]], writes=[PSk[pb]], inc=(ft == 7))
                                C.op("dve", lambda e: e.tensor_tensor(out=tmp[:, bi, :], in0=PSB[pb][:], in1=G12[:, 0, hf * 512:(hf + 1) * 512], op=ALU.mult),
                                     reads=[PSk[pb], K_G[0]], writes=[K_tmp[bi]])
                                C.op("pool", lambda e: e.tensor_tensor(out=X[:, tt, hf * 512:(hf + 1) * 512], in0=X[:, tt, hf * 512:(hf + 1) * 512],
                                                                       in1=tmp[:, bi, :], op=ALU.add),
                                     reads=[K_tmp[bi], XT[tt]], writes=[XT[tt]])
                        C.barrier()
            tap(f"X1_{l}", X[:], XT, [128, NT, D])
            if stop_after == f"mix{l}":
                break

            norm_to_HT(2)
            with ExitStack() as ph:
                comb = sb("comb", [128, NT, NE], F32, ph)
                K_comb = tks(NT, "comb")
                bguT = sb("bguT", [128, NE * 16], F32, ph)
                K_bgu = Tk()
                with ExitStack() as ph2:
                    wr = sb("wr", [128, 8, NE], BF16, ph2)
                    brt = sb("brt", [128, NE], F32, ph2)
                    bd = sb("bd", [NE, D], F32, ph2)
                    K_r = Tk()
                    C.dma("pool", wr[:], dr["w_router"][l].rearrange("(kt p) e -> p kt e", p=128), writes=[K_r])
                    C.dma("sp", brt[:], dr["b_router"][l].partition_broadcast(128), writes=[K_r])
                    C.dma("sp", bd[:], dr["b_down"][l], writes=[K_r])
                    rows = sb("bgrows", [128, 4, 128], F32, ph2)
                    K_rows = Tk()
                    C.dma("sp", rows[:], dr["b_gate_up"][l].rearrange("e (j p) -> (e j) p", p=128).rearrange("(r q) p -> q r p", q=128), writes=[K_rows])
                    for r_ in range(4):
                        C.op("pe", lambda e: e.transpose(PSB[0][:, r_ * 128:(r_ + 1) * 128], rows[:, r_, :], ident_f[:]),
                             reads=[K_rows, K_id], writes=[PSk[0]], inc=(r_ == 3))
                    C.op("dve", lambda e: e.tensor_copy(out=bguT[:], in_=PSB[0][:]), reads=[PSk[0]], writes=[K_bgu])
                    bv = bguT[:].rearrange("p (e j) -> p e j", j=16)
                    C.op("dve", lambda e: e.tensor_scalar(out=bv[:, :, 8:16], in0=bv[:, :, 8:16], scalar1=1.0, scalar2=None, op0=ALU.add),
                         reads=[K_bgu], writes=[K_bgu])
                    lg = sb("lg", [128, 2, NE], F32, ph2)
                    m8r = sb("m8r", [128, 2, 8], F32, ph2)
                    sel = sb("rsel", [128, 2, NE], F32, ph2)
                    sm = sb("rsm", [128, 2, 2], F32, ph2)
                    cT = sb("rcT", [NE, 2, 128], F32, ph2)
                    tmpb = sb("rtmp", [128, 2, 512], F32, ph2)
                    K_lg, K_m8r, K_sel, K_sm, K_cT, K_tmpb = tks(2), tks(2), tks(2), tks(2), tks(2), tks(2)
                    it = 0
                    for tt in range(NT):
                        p = tt % 2
                        for kt in range(8):
                            C.op("pe", lambda e: e.matmul(PSB[1][:, 0:NE], HT[:, kt, tt * 128:(tt + 1) * 128], wr[:, kt, :], start=(kt == 0), stop=(kt == 7)),
                                 reads=[K_r] + HTr(tt // 4), writes=[PSk[1]], inc=(kt == 7))
                        C.op("dve", lambda e: e.tensor_tensor(out=lg[:, p, :], in0=PSB[1][:, 0:NE], in1=brt[:], op=ALU.add), reads=[PSk[1], K_r], writes=[K_lg[p]])
                        C.op("dve", lambda e: e.max(out=m8r[:, p, :], in_=lg[:, p, :]), reads=[K_lg[p]], writes=[K_m8r[p]])
                        C.op("dve", lambda e: e.tensor_scalar(out=sel[:, p, :], in0=lg[:, p, :], scalar1=m8r[:, p, 3:4], scalar2=None, op0=ALU.is_ge),
                             reads=[K_lg[p], K_m8r[p]], writes=[K_sel[p]])
                        C.op("dve", lambda e: e.tensor_scalar(out=sm[:, p, 0:1], in0=m8r[:, p, 0:1], scalar1=-1.0, scalar2=None, op0=ALU.mult),
                             reads=[K_m8r[p]], writes=[K_sm[p]])
                        C.op("act", lambda e: e.activation(out=lg[:, p, :], in_=lg[:, p, :], func=AF.Exp, bias=sm[:, p, 0:1]), reads=[K_lg[p], K_sm[p]], writes=[K_lg[p]])
                        C.op("dve", lambda e: e.tensor_tensor(out=sel[:, p, :], in0=sel[:, p, :], in1=lg[:, p, :], op=ALU.mult), reads=[K_lg[p], K_sel[p]], writes=[K_sel[p]])
                        C.op("dve", lambda e: e.reduce_sum(out=sm[:, p, 1:2], in_=sel[:, p, :], axis=AX.X), reads=[K_sel[p]], writes=[K_sm[p]])
                        C.op("dve", lambda e: e.reciprocal(out=sm[:, p, 1:2], in_=sm[:, p, 1:2]), reads=[K_sm[p]], writes=[K_sm[p]])
                        C.op("dve", lambda e: e.tensor_scalar(out=comb[:, tt, :], in0=sel[:, p, :], scalar1=sm[:, p, 1:2], scalar2=None, op0=ALU.mult),
                             reads=[K_sel[p], K_sm[p]], writes=[K_comb[tt]])
                        C.op("pe", lambda e: e.transpose(PSB[2][0:NE, 0:128], comb[:, tt, :], ident_f[:]), reads=[K_comb[tt], K_id], writes=[PSk[2]])
                        C.op("act", lambda e: e.copy(out=cT[:, p, :], in_=PSB[2][0:NE, 0:128]), reads=[PSk[2]], writes=[K_cT[p]])
                        for hf in range(2):
                            pb, bi = 4 + it % 4, it % 2
                            it += 1
                            C.op("pe", lambda e: e.matmul(PSB[pb][:], cT[:, p, :], bd[:, hf * 512:(hf + 1) * 512], start=True, stop=True),
                                 reads=[K_cT[p], K_r], writes=[PSk[pb]])
                            C.op("dve", lambda e: e.tensor_tensor(out=tmpb[:, bi, :], in0=PSB[pb][:], in1=G12[:, 1, hf * 512:(hf + 1) * 512], op=ALU.mult),
                                 reads=[PSk[pb], K_G[1]], writes=[K_tmpb[bi]])
                            C.op("pool", lambda e: e.tensor_tensor(out=X[:, tt, hf * 512:(hf + 1) * 512], in0=X[:, tt, hf * 512:(hf + 1) * 512],
                                                                   in1=tmpb[:, bi, :], op=ALU.add),
                                 reads=[K_tmpb[bi], XT[tt]], writes=[XT[tt]])
                    C.barrier()
                tap(f"comb{l}", comb[:], K_comb, [128, NT, NE])
                if stop_after == f"router{l}":
                    break
                RING = 5
                ring = sb("ring", [128, RING, 8, 512], BF16, ph)
                K_ring = tks(RING, "ring")
                actT = sb("actT", [128, 8, S], BF16, ph)
                K_act = [tks(4, f"act{f}_") for f in range(8)]
                xg = sb("xg", [128, 2, 512], F32, ph)
                sg = sb("sg", [128, 2, 512], BF16, ph)
                Al = sb("Al", [128, 2, 512], F32, ph)
                tg_ = sb("tg", [128, 2, 512], F32, ph)
                yt_ = sb("ytmp", [128, 2, 512], F32, ph)
                K_xg, K_sg, K_Al, K_tg, K_ytmp = tks(2), tks(2), tks(2), tks(2), tks(2)
                n_exp = NE if stop_after != f"moe1e{l}" else 1
                chunks = []
                for e_ in range(n_exp):
                    wgu = dr["w_gate_up"][l][e_]
                    wdn = dr["w_down"][l][e_]
                    chunks += [wgu[:, 0:512], wgu[:, 1024:1536], wgu[:, 512:1024], wgu[:, 1536:2048], wdn[:, 0:512], wdn[:, 512:1024]]
                loaded = [0]

                def prefetch(upto):
                    while loaded[0] < min(upto, len(chunks)):
                        i = loaded[0]
                        wload(ring[:, i % RING, :, :], K_ring[i % RING], chunks[i])
                        loaded[0] += 1
                prefetch(RING - 1)
                ci_ = 0
                gi = 0
                yi = 0
                for e_ in range(n_exp):
                    for c in range(2):
                        sl_g, sl_l = ci_ % RING, (ci_ + 1) % RING
                        prefetch(ci_ + RING)
                        for g in range(4):
                            for i in range(4):
                                ft = c * 4 + i
                                pg, pl, bi = (gi % 2) * 2, (gi % 2) * 2 + 1, gi % 2
                                gi += 1
                                for kt in range(8):
                                    C.op("pe", lambda e: e.matmul(PSB[pg][:], ring[:, sl_g, kt, i * 128:(i + 1) * 128], HT[:, kt, g * 512:(g + 1) * 512],
                                                                  start=(kt == 0), stop=(kt == 7)),
                                         reads=[K_ring[sl_g]] + HTr(g), writes=[PSk[pg]], inc=(kt == 7))
                                for kt in range(8):
                                    C.op("pe", lambda e: e.matmul(PSB[pl][:], ring[:, sl_l, kt, i * 128:(i + 1) * 128], HT[:, kt, g * 512:(g + 1) * 512],
                                                                  start=(kt == 0), stop=(kt == 7)),
                                         reads=[K_ring[sl_l]] + HTr(g), writes=[PSk[pl]], inc=(kt == 7))
                                cg = e_ * 16 + ft
                                C.op("dve", lambda e: e.tensor_scalar(out=xg[:, bi, :], in0=PSB[pg][:], scalar1=bguT[:, cg:cg + 1], scalar2=7.0, op0=ALU.add, op1=ALU.min),
                                     reads=[PSk[pg], K_bgu], writes=[K_xg[bi]])
                                C.op("act", lambda e: e.activation(out=sg[:, bi, :], in_=xg[:, bi, :], func=AF.Sigmoid, scale=1.702), reads=[K_xg[bi]], writes=[K_sg[bi]])
                                C.op("dve", lambda e: e.tensor_scalar(out=Al[:, bi, :], in0=PSB[pl][:], scalar1=bguT[:, cg + 8:cg + 9], scalar2=8.0, op0=ALU.add, op1=ALU.min),
                                     reads=[PSk[pl], K_bgu], writes=[K_Al[bi]])
                                C.op("dve", lambda e: e.tensor_tensor(out=tg_[:, bi, :], in0=xg[:, bi, :], in1=sg[:, bi, :], op=ALU.mult),
                                     reads=[K_xg[bi], K_sg[bi]], writes=[K_tg[bi]])
                                C.op("dve", lambda e: e.scalar_tensor_tensor(out=actT[:, ft, g * 512:(g + 1) * 512], in0=Al[:, bi, :], scalar=-6.0, in1=tg_[:, bi, :],
                                                                             op0=ALU.max, op1=ALU.mult),
                                     reads=[K_Al[bi], K_tg[bi]], writes=[K_act[ft][g]])
                        ci_ += 2
                    for hf in range(2):
                        sl_d = ci_ % RING
                        prefetch(ci_ + RING)
                        for tt in range(NT):
                            pb, bi = 4 + yi % 4, yi % 2
                            yi += 1
                            for ft in range(8):
                                C.op("pe", lambda e: e.matmul(PSB[pb][:], actT[:, ft, tt * 128:(tt + 1) * 128], ring[:, sl_d, ft, :], start=(ft == 0), stop=(ft == 7)),
                                     reads=[K_ring[sl_d], K_act[ft][tt // 4]], writes=[PSk[pb]], inc=(ft == 7))
                            C.op("act", lambda e: e.activation(out=yt_[:, bi, :], in_=PSB[pb][:], func=AF.Identity, scale=comb[:, tt, e_:e_ + 1]),
                                 reads=[PSk[pb], K_comb[tt]], writes=[K_ytmp[bi]])
                            C.op("pool", lambda e: e.tensor_tensor(out=yt_[:, bi, :], in0=yt_[:, bi, :], in1=G12[:, 1, hf * 512:(hf + 1) * 512], op=ALU.mult),
                                 reads=[K_ytmp[bi], K_G[1]], writes=[K_ytmp[bi]])
                            C.op("pool", lambda e: e.tensor_tensor(out=X[:, tt, hf * 512:(hf + 1) * 512], in0=X[:, tt, hf * 512:(hf + 1) * 512],
                                                                   in1=yt_[:, bi, :], op=ALU.add),
                                 reads=[K_ytmp[bi], XT[tt]], writes=[XT[tt]])
                        ci_ += 1
                C.barrier()
            tap(f"X2_{l}", X[:], XT, [128, NT, D])
            if stop_after == f"moe{l}" or stop_after == f"moe1e{l}":
                break

        if stop_after is None:
            with ExitStack() as ph:
                gf = sb("gf", [128, D], F32, ph)
                K_gf = Tk()
                C.dma("sp", gf[:], dr["g_final"][0].partition_broadcast(128), writes=[K_gf])
                ot = sb("ot", [128, 2, D], F32, ph)
                K_ot = tks(2)
                ov_ = out_d.rearrange("(t p) d -> p t d", p=128)
                for tt in range(NT):
                    p = tt % 2
                    g = tt // 4
                    C.op("act", lambda e: e.activation(out=junk[:, p, :], in_=X[:, tt, :], func=AF.Square, accum_out=ss[:, tt:tt + 1]),
                         reads=[XT[tt]], writes=[K_junk[p], K_ss[g]])
                    C.op("dve", lambda e: e.tensor_scalar(out=rstd[:, tt:tt + 1], in0=ss[:, tt:tt + 1], scalar1=1.0 / D, scalar2=EPS, op0=ALU.mult, op1=ALU.add),
                         reads=[K_ss[g]], writes=[K_rstd[g]])
                    C.op("act", lambda e: e.activation(out=rstd[:, tt:tt + 1], in_=rstd[:, tt:tt + 1], func=AF.Sqrt), reads=[K_rstd[g]], writes=[K_rstd[g]])
                    C.op("dve", lambda e: e.reciprocal(out=rstd[:, tt:tt + 1], in_=rstd[:, tt:tt + 1]), reads=[K_rstd[g]], writes=[K_rstd[g]])
                    C.op("dve", lambda e: e.scalar_tensor_tensor(out=ot[:, p, :], in0=X[:, tt, :], scalar=rstd[:, tt:tt + 1], in1=gf[:], op0=ALU.mult, op1=ALU.mult),
                         reads=[XT[tt], K_rstd[g], K_gf], writes=[K_ot[p]])
                    C.dma("sp", ov_[:, tt, :], ot[:, p, :], reads=[K_ot[p]])
        C.barrier()
    return nc, list(tap_d.keys())


def kernel(**inputs):
    n = 8
    nc, _ = build()
    consts = make_consts()
    shared = {}
    for k in IN_SPECS:
        if k in ("x", "c"):
            continue
        a = np.ascontiguousarray(np.asarray(inputs[k], dtype=np.float32))
        if k == "g_final":
            a = a.reshape(1, D)
        shared[k] = a
    for k, v in consts.items():
        shared["k_" + k] = v
    x = np.asarray(inputs["x"], dtype=np.float32)
    c = np.asarray(inputs["c"], dtype=np.float32)
    in_maps = []
    for b in range(n):
        m = dict(shared)
        m["x"] = np.ascontiguousarray(x[b])
        m["c"] = np.ascontiguousarray(c[b:b + 1])
        in_maps.append(m)
    res = run_bass_kernel_spmd(nc, in_maps, core_ids=list(range(n)))
    return np.stack([np.asarray(r["out"], dtype=np.float32) for r in res.results], axis=0)
```
